# Optimizing a Trainium2 kernel written in Bass

```python
import jax, jax.numpy as jnp
from jax import lax
import numpy as np

D_MODEL = 2048
BATCH = 2
SEQ = 4096
DEPTH = 2

GLA_HEADS = 4
GLA_DK = 128
GLA_DV = 256
GLA_GATE_RANK = 16
GLA_GATE_TAU = 16.0
GLA_CHUNK = 64
FOX_HEADS = 8
FOX_DH = 128
DSA_Q_HEADS = 16
DSA_KV_HEADS = 4
DSA_DH = 128
IDX_HEADS = 16
IDX_DH = 64
DSA_TOPK_MAX = 256
N_GROUPS = 4
EXPERTS_PER_GROUP = 8
EXPERT_FF = 512
TOP_GROUPS = 1
TOP_EXPERTS = 2
Q_BLOCK = 128
ROPE_THETA = 10000.0
LN_EPS = 1e-5
DN_ALPHA = (2 * DEPTH) ** 0.25
DN_BETA = (8 * DEPTH) ** -0.25
MIX_WIDTH = GLA_HEADS * GLA_DV + FOX_HEADS * FOX_DH
DSA_WIDTH = DSA_Q_HEADS * DSA_DH

kernel_name = "hybrid_gla_fox_dsa_hmoe"

F32 = jnp.float32


def _even_sizes():
    return [GLA_HEADS * GLA_DK, GLA_HEADS * GLA_DK, GLA_HEADS * GLA_DV, GLA_HEADS * GLA_DV,
            GLA_GATE_RANK, FOX_HEADS * FOX_DH, FOX_HEADS * FOX_DH, FOX_HEADS * FOX_DH, FOX_HEADS]


def _odd_sizes():
    return [DSA_Q_HEADS * DSA_DH, DSA_KV_HEADS * DSA_DH, DSA_KV_HEADS * DSA_DH,
            IDX_HEADS * IDX_DH, IDX_DH, IDX_HEADS]


def _split(t, sizes):
    return jnp.split(t, np.cumsum(sizes)[:-1].tolist(), axis=-1)


def layer_norm(x, g, b):
    xf = x.astype(F32)
    mu = jnp.mean(xf, axis=-1, keepdims=True)
    var = jnp.mean(jnp.square(xf - mu), axis=-1, keepdims=True)
    y = (xf - mu) * lax.rsqrt(var + LN_EPS) * g.astype(F32) + b.astype(F32)
    return y.astype(x.dtype)


def rms_norm(x, g):
    xf = x.astype(F32)
    return xf * lax.rsqrt(jnp.mean(jnp.square(xf), axis=-1, keepdims=True) + LN_EPS) * g.astype(F32)


def rope(x, pos):
    half = x.shape[-1] // 2
    inv = ROPE_THETA ** (-jnp.arange(half, dtype=F32) / half)
    ang = pos.astype(F32)[:, None] * inv[None, :]
    cos = jnp.cos(ang)[None, :, None, :]
    sin = jnp.sin(ang)[None, :, None, :]
    x1, x2 = x[..., :half], x[..., half:]
    return jnp.concatenate([x1 * cos - x2 * sin, x2 * cos + x1 * sin], axis=-1)


def gla_chunked(q, k, v, log_a):
    B, S, H, DK = q.shape
    DV = v.shape[-1]
    C = GLA_CHUNK
    N = S // C

    def chunks(t):
        return t.reshape(B, N, C, H, t.shape[-1]).transpose(0, 3, 1, 2, 4)

    q, k, v, log_a = chunks(q * DK ** -0.5), chunks(k), chunks(v), chunks(log_a)
    b = jnp.cumsum(log_a, axis=3)
    b_last = b[:, :, :, -1:, :]
    q_dec = q * jnp.exp(b)
    k_inv = k * jnp.exp(-b)
    k_end = k * jnp.exp(b_last - b)
    causal = jnp.tril(jnp.ones((C, C), dtype=bool))
    scores = jnp.where(causal, jnp.einsum('bhnid,bhnjd->bhnij', q_dec, k_inv), 0.0)
    o_intra = jnp.einsum('bhnij,bhnjv->bhniv', scores, v)
    chunk_state = jnp.einsum('bhncd,bhncv->bhndv', k_end, v)
    chunk_decay = jnp.exp(b_last[:, :, :, 0, :])

    def step(state, inp):
        ds, dec = inp
        return dec[..., None] * state + ds, state

    init = jnp.zeros((B, H, DK, DV), F32)
    _, prev = lax.scan(step, init, (jnp.moveaxis(chunk_state, 2, 0), jnp.moveaxis(chunk_decay, 2, 0)))
    prev = jnp.moveaxis(prev, 0, 2)
    o = o_intra + jnp.einsum('bhncd,bhndv->bhncv', q_dec, prev)
    return o.transpose(0, 2, 3, 1, 4).reshape(B, S, H, DV)


def fox_attention(q, k, v, log_f):
    B, S, H, D = q.shape
    c = jnp.cumsum(log_f, axis=1).transpose(0, 2, 1)
    key_pos = jnp.arange(S)
    scale = D ** -0.5

    def block(i):
        start = i * Q_BLOCK
        qb = lax.dynamic_slice_in_dim(q, start, Q_BLOCK, axis=1)
        cb = lax.dynamic_slice_in_dim(c, start, Q_BLOCK, axis=2)
        q_pos = start + jnp.arange(Q_BLOCK)
        logits = jnp.einsum('bqhd,bshd->bhqs', qb, k) * scale + cb[..., None] - c[:, :, None, :]
        logits = jnp.where(key_pos[None, :] <= q_pos[:, None], logits, -jnp.inf)
        p = jax.nn.softmax(logits, axis=-1)
        return jnp.einsum('bhqs,bshd->bqhd', p, v)

    out = lax.map(block, jnp.arange(S // Q_BLOCK))
    return jnp.moveaxis(out, 0, 1).reshape(B, S, H, D)


def dsa_attention(q, k, v, iq, ik, iw):
    B, S, HQ, D = q.shape
    HKV = k.shape[2]
    G = HQ // HKV
    top_k = min(DSA_TOPK_MAX, S // 4)
    key_pos = jnp.arange(S)
    gather = jax.vmap(lambda seq, idx: seq[idx])

    def block(i):
        start = i * Q_BLOCK
        q_pos = start + jnp.arange(Q_BLOCK)
        iqb = lax.dynamic_slice_in_dim(iq, start, Q_BLOCK, axis=1)
        iwb = lax.dynamic_slice_in_dim(iw, start, Q_BLOCK, axis=1)
        rel = jax.nn.relu(jnp.einsum('bqhd,bsd->bqhs', iqb, ik)) * IDX_DH ** -0.5
        score = jnp.einsum('bqh,bqhs->bqs', iwb, rel)
        score = jnp.where(key_pos[None, :] <= q_pos[:, None], score, -jnp.inf)
        _, idx = lax.top_k(score, top_k)
        valid = idx <= q_pos[None, :, None]
        kg = gather(k, idx)
        vg = gather(v, idx)
        qb = lax.dynamic_slice_in_dim(q, start, Q_BLOCK, axis=1).reshape(B, Q_BLOCK, HKV, G, D)
        logits = jnp.einsum('bqhgd,bqkhd->bhgqk', qb, kg) * D ** -0.5
        logits = jnp.where(valid[:, None, None], logits, -jnp.inf)
        p = jax.nn.softmax(logits, axis=-1)
        o = jnp.einsum('bhgqk,bqkhd->bqhgd', p, vg)
        return o.reshape(B, Q_BLOCK, HQ, D)

    out = lax.map(block, jnp.arange(S // Q_BLOCK))
    return jnp.moveaxis(out, 0, 1).reshape(B, S, HQ, D)


def even_mixer(x, w_in, gate_w2, gate_b, gla_norm_g, fox_gate_b, w_out):
    B, S, _ = x.shape
    gq, gk, gv, gr, glr, fq, fk, fv, ff = _split(x @ w_in, _even_sizes())
    log_a = jax.nn.log_sigmoid((glr @ gate_w2).astype(F32) + gate_b.astype(F32)) / GLA_GATE_TAU
    hk = lambda t, d: t.reshape(B, S, -1, d).astype(F32)
    o_a = gla_chunked(hk(gq, GLA_DK), hk(gk, GLA_DK), hk(gv, GLA_DV), hk(log_a, GLA_DK))
    o_a = rms_norm(o_a, gla_norm_g) * jax.nn.silu(hk(gr, GLA_DV))
    log_f = jax.nn.log_sigmoid(ff.astype(F32) + fox_gate_b.astype(F32))
    o_b = fox_attention(hk(fq, FOX_DH), hk(fk, FOX_DH), hk(fv, FOX_DH), log_f)
    o = jnp.concatenate([o_a.reshape(B, S, -1), o_b.reshape(B, S, -1)], axis=-1)
    return o.astype(x.dtype) @ w_out


def odd_mixer(x, w_in, w_out):
    B, S, _ = x.shape
    q, k, v, iq, ik, iw = _split(x @ w_in, _odd_sizes())
    pos = jnp.arange(S)
    q = rope(q.reshape(B, S, DSA_Q_HEADS, DSA_DH).astype(F32), pos)
    k = rope(k.reshape(B, S, DSA_KV_HEADS, DSA_DH).astype(F32), pos)
    v = v.reshape(B, S, DSA_KV_HEADS, DSA_DH).astype(F32)
    iq = rope(iq.reshape(B, S, IDX_HEADS, IDX_DH).astype(F32), pos)
    ik = rope(ik.reshape(B, S, 1, IDX_DH).astype(F32), pos)[:, :, 0]
    iw = iw.astype(F32) * IDX_HEADS ** -0.5
    o = dsa_attention(q, k, v, iq, ik, iw)
    return o.reshape(B, S, -1).astype(x.dtype) @ w_out


def hier_moe(x, wg, bg, we, be, w_gate, w_up, w_down):
    B, S, D = x.shape
    t = x.reshape(B * S, D)
    group_prob = jax.nn.softmax((t @ wg).astype(F32) + bg.astype(F32), axis=-1)
    g_prob, g_idx = lax.top_k(group_prob, TOP_GROUPS)
    expert_logits = jnp.einsum('td,gde->tge', t, we).astype(F32) + be.astype(F32)
    chosen = jnp.take_along_axis(expert_logits, g_idx[:, :, None], axis=1)[:, 0]
    e_val, e_idx = lax.top_k(chosen, TOP_EXPERTS)
    e_w = jax.nn.softmax(e_val, axis=-1) * g_prob
    within = jnp.sum(jax.nn.one_hot(e_idx, EXPERTS_PER_GROUP, dtype=F32) * e_w[..., None], axis=1)
    combine = jax.nn.one_hot(g_idx[:, 0], N_GROUPS, dtype=F32)[:, :, None] * within[:, None, :]
    out = jnp.zeros((B * S, D), F32)
    for g in range(N_GROUPS):
        h = jax.nn.silu(jnp.einsum('td,edf->tef', t, w_gate[g])) * jnp.einsum('td,edf->tef', t, w_up[g])
        h = h * combine[:, g, :, None].astype(h.dtype)
        out = out + jnp.einsum('tef,efd->td', h, w_down[g]).astype(F32)
    return out.reshape(B, S, D).astype(x.dtype)


def setup_inputs(seed: int = 0) -> dict:
    key = jax.random.key(seed)
    ks = iter(jax.random.split(key, 32))
    nrm = lambda shape, scale: jax.random.normal(next(ks), shape, F32) * scale
    ne, no = (DEPTH + 1) // 2, DEPTH // 2
    even_cols = int(sum(_even_sizes()))
    odd_cols = int(sum(_odd_sizes()))
    G, E, F, D = N_GROUPS, EXPERTS_PER_GROUP, EXPERT_FF, D_MODEL
    return {
        'x': nrm((BATCH, SEQ, D), 1.0),
        'a_w_in': nrm((ne, D, even_cols), D ** -0.5),
        'a_gla_gate_w2': nrm((ne, GLA_GATE_RANK, GLA_HEADS * GLA_DK), GLA_GATE_RANK ** -0.5),
        'a_gla_gate_b': nrm((ne, GLA_HEADS * GLA_DK), 0.1),
        'a_gla_norm_g': 1.0 + nrm((ne, GLA_DV), 0.02),
        'a_fox_gate_b': jax.random.uniform(next(ks), (ne, FOX_HEADS), F32, 1.0, 4.0),
        'a_w_out': nrm((ne, MIX_WIDTH, D), MIX_WIDTH ** -0.5 * DN_BETA),
        'c_w_in': nrm((no, D, odd_cols), D ** -0.5),
        'c_w_out': nrm((no, DSA_WIDTH, D), DSA_WIDTH ** -0.5 * DN_BETA),
        'ln_mix_g': 1.0 + nrm((DEPTH, D), 0.02),
        'ln_mix_b': nrm((DEPTH, D), 0.02),
        'ln_ffn_g': 1.0 + nrm((DEPTH, D), 0.02),
        'ln_ffn_b': nrm((DEPTH, D), 0.02),
        'moe_group_w': nrm((DEPTH, D, G), D ** -0.5),
        'moe_group_b': nrm((DEPTH, G), 0.01),
        'moe_expert_w': nrm((DEPTH, G, D, E), D ** -0.5),
        'moe_expert_b': nrm((DEPTH, G, E), 0.01),
        'moe_w_gate': nrm((DEPTH, G, E, D, F), D ** -0.5),
        'moe_w_up': nrm((DEPTH, G, E, D, F), D ** -0.5),
        'moe_w_down': nrm((DEPTH, G, E, F, D), F ** -0.5 * DN_BETA),
    }


def reference(x, a_w_in, a_gla_gate_w2, a_gla_gate_b, a_gla_norm_g, a_fox_gate_b, a_w_out,
              c_w_in, c_w_out, ln_mix_g, ln_mix_b, ln_ffn_g, ln_ffn_b,
              moe_group_w, moe_group_b, moe_expert_w, moe_expert_b,
              moe_w_gate, moe_w_up, moe_w_down):
    for layer in range(DEPTH):
        j = layer // 2
        if layer % 2 == 0:
            h = even_mixer(x, a_w_in[j], a_gla_gate_w2[j], a_gla_gate_b[j], a_gla_norm_g[j],
                           a_fox_gate_b[j], a_w_out[j])
        else:
            h = odd_mixer(x, c_w_in[j], c_w_out[j])
        x = layer_norm(DN_ALPHA * x + h, ln_mix_g[layer], ln_mix_b[layer])
        h = hier_moe(x, moe_group_w[layer], moe_group_b[layer], moe_expert_w[layer], moe_expert_b[layer],
                     moe_w_gate[layer], moe_w_up[layer], moe_w_down[layer])
        x = layer_norm(DN_ALPHA * x + h, ln_ffn_g[layer], ln_ffn_b[layer])
    return x
```

```python
import contextlib
import numpy as np
import concourse.bass as bass
import concourse.mybir as mybir
from concourse.bass_utils import run_bass_kernel_spmd

F32 = mybir.dt.float32
BF16 = mybir.dt.bfloat16
AF = mybir.ActivationFunctionType
ALU = mybir.AluOpType
AX = mybir.AxisListType

D = 2048
NCH = 16
NT = 1024
S = 4096
ALPHA = 4.0 ** 0.25
EPS = 1e-5
NEXP = 32
FF = 512
BIG = 1.0e30


class Buf:
    __slots__ = ("name", "lw", "rd", "dsem", "persist", "depoch")

    def __init__(self, name="", persist=False):
        self.name = name
        self.lw = None
        self.rd = []
        self.dsem = None
        self.persist = persist
        self.depoch = -1


class Prog:
    ENG = ("pe", "act", "dve", "pool", "sp")

    def __init__(self, nc, safe_same_engine=True):
        self.nc = nc
        self.safe = safe_same_engine
        self.ins = []
        self.perq = {e: [] for e in self.ENG}
        self.ndsem = 0
        self.pending = {e: set() for e in self.ENG}
        self.last_dma = {}
        self.epoch = 0
        self.NPERSIST = 6
        self.npersist = 0
        self.next_edsem = self.NPERSIST

    def _add(self, eng, fn, reads, writes, kind, dsem=None):
        iid = len(self.ins)
        deps = set(self.pending[eng])
        self.pending[eng] = set()
        for b in reads:
            if b.lw is not None:
                deps.add(b.lw)
        for b in writes:
            if b.lw is not None:
                deps.add(b.lw)
            last = {}
            for r in b.rd:
                rr = self.ins[r]
                if rr["kind"] == "dma":
                    deps.add(r)
                else:
                    last[rr["eng"]] = max(last.get(rr["eng"], -1), r)
            deps.update(last.values())
        self.ins.append(dict(eng=eng, fn=fn, deps=deps, kind=kind, dsem=dsem))
        self.perq[eng].append(iid)
        for b in reads:
            b.rd.append(iid)
        for b in writes:
            b.lw = iid
            b.rd = []
        if kind == "dma":
            self.last_dma[dsem] = iid
        return iid

    def op(self, eng, fn, reads=(), writes=()):
        return self._add(eng, fn, list(reads), list(writes), "op")

    def dma(self, q, fn, reads=(), writes=(), sembuf=None):
        if sembuf is None:
            sembuf = (list(writes) + list(reads))[0]
        if sembuf.persist:
            if sembuf.dsem is None:
                assert self.npersist < self.NPERSIST
                sembuf.dsem = self.npersist
                self.npersist += 1
        elif sembuf.dsem is None or sembuf.depoch != self.epoch:
            sembuf.dsem = self.next_edsem
            sembuf.depoch = self.epoch
            self.next_edsem += 1
        self.ndsem = max(self.ndsem, sembuf.dsem + 1)
        return self._add(q, fn, list(reads), list(writes), "dma", dsem=sembuf.dsem)

    def new_epoch(self):
        self.barrier()
        self.epoch += 1
        self.next_edsem = self.NPERSIST

    def barrier(self):
        dset = set()
        for e in self.ENG:
            if self.perq[e]:
                dset.add(self.perq[e][-1])
        dset.update(self.last_dma.values())
        for e in self.ENG:
            self.pending[e] |= dset

    def emit(self, final_wait_eng="sp"):
        nc = self.nc
        ins = self.ins
        marked = set()
        for it in ins:
            for d in it["deps"]:
                dd = ins[d]
                if dd["kind"] == "dma":
                    continue
                if dd["eng"] == it["eng"]:
                    if not self.safe:
                        continue
                    if dd["eng"] == "pe" and it["kind"] == "op":
                        continue
                marked.add(d)
        rank = {}
        for e in self.ENG:
            c = 0
            for iid in self.perq[e]:
                if ins[iid]["kind"] == "op" and iid in marked:
                    c += 1
                    rank[iid] = c
        dval = {}
        dcount = [0] * self.ndsem
        for i, it in enumerate(ins):
            if it["kind"] == "dma":
                dcount[it["dsem"]] += 16
                dval[i] = dcount[it["dsem"]]
        with contextlib.ExitStack() as st:
            esem = {e: st.enter_context(nc.semaphore("s_" + e)) for e in self.ENG}
            dsems = [st.enter_context(nc.semaphore("d%d" % k)) for k in range(self.ndsem)]
            block = st.enter_context(nc.Block())

            def make(e):
                def body(engobj):
                    seen = {}
                    for iid in self.perq[e]:
                        it = ins[iid]
                        need = {}
                        for d in it["deps"]:
                            dd = ins[d]
                            if dd["kind"] == "dma":
                                key = ("d", dd["dsem"])
                                val = dval[d]
                            else:
                                if d not in rank:
                                    continue
                                key = ("e", dd["eng"])
                                val = rank[d]
                            if need.get(key, 0) < val:
                                need[key] = val
                        for key, val in need.items():
                            if seen.get(key, 0) >= val:
                                continue
                            seen[key] = val
                            sem = dsems[key[1]] if key[0] == "d" else esem[key[1]]
                            engobj.wait_ge(sem, val)
                        r = it["fn"](engobj)
                        if it["kind"] == "dma":
                            r.then_inc(dsems[it["dsem"]], 16)
                        elif iid in rank:
                            r.then_inc(esem[e], 1)
                    if e == final_wait_eng:
                        for k in range(self.ndsem):
                            if dcount[k] and seen.get(("d", k), 0) < dcount[k]:
                                engobj.wait_ge(dsems[k], dcount[k])
                return body

            block.tensor(make("pe"))
            block.scalar(make("act"))
            block.vector(make("dve"))
            block.gpsimd(make("pool"))
            block.sync(make("sp"))


class Ctx:
    def __init__(self, nc, st):
        self.nc = nc
        import os as _os
        self.P = Prog(nc, safe_same_engine=not _os.environ.get("UNSAFE"))
        self.arena_words = 206 * 1024 // 4
        self.arena = st.enter_context(nc.sbuf_tensor("arena", [128, self.arena_words], F32))
        self.ps = [st.enter_context(nc.psum_tensor("ps%d" % i, [128, 512], F32)) for i in range(8)]
        self.Bps = [Buf("ps%d" % i) for i in range(8)]

    def f32(self, off, n, parts=128):
        assert off % 4 == 0 and off // 4 + n <= self.arena_words, (off, n)
        return self.arena[0:parts, off // 4: off // 4 + n]

    def bf(self, off, n, parts=128):
        assert off % 4 == 0 and n % 2 == 0 and off // 4 + n // 2 <= self.arena_words, (off, n)
        return self.arena[0:parts, off // 4: off // 4 + n // 2].bitcast(BF16)


KB = 1024
OFF_ACC = 0
OFF_XB = 64 * KB
OFF_OT = 96 * KB
OFF_RING = 128 * KB
OFF_STAGE = 160 * KB
OFF_MISC = 184 * KB
NRING = 8
NSTAGE = 3


class WStream:
    def __init__(self, C):
        self.C = C
        self.stage = [C.f32(OFF_STAGE + i * 8 * KB, 2048) for i in range(NSTAGE)]
        self.Bst = [Buf("st%d" % i) for i in range(NSTAGE)]
        self.ring = [C.bf(OFF_RING + i * 4 * KB, 2048) for i in range(NRING)]
        self.Brg = [Buf("rg%d" % i) for i in range(NRING)]
        self.n = 0

    def push(self, src_ap):
        P = self.C.P
        i = self.n
        self.n += 1
        s = i % NSTAGE
        r = i % NRING
        st, bst, rg, brg = self.stage[s], self.Bst[s], self.ring[r], self.Brg[r]
        q = "sp"
        P.dma(q, lambda e: e.dma_start(out=st, in_=src_ap), writes=[bst])
        ce = "act"
        if ce == "pool":
            P.op("pool", lambda e: e.tensor_copy(out=rg, in_=st), reads=[bst], writes=[brg])
        else:
            P.op("act", lambda e: e.activation(out=rg, in_=st, func=AF.Copy), reads=[bst], writes=[brg])
        return rg, brg


def layer_norm_fm(C, zoff, Bz, gcol, bcol, outs, tmp_off):
    P = C.P
    nc = C.nc
    ones = C.ones32
    Bones = C.Bones
    z = lambda m, h: C.f32(zoff + (m * NT + h * 512) * 4, 512)
    sq = [C.f32(tmp_off + i * 2 * KB, 512) for i in range(2)]
    Bsq = [Buf("sq0"), Buf("sq1")]
    mean = C.f32(tmp_off + 4 * KB, 512)
    rstd = C.f32(tmp_off + 6 * KB, 512)
    var = C.f32(tmp_off + 8 * KB, 512)
    Bmean, Brstd, Bvar = Buf("mean"), Buf("rstd"), Buf("var")
    for h in range(2):
        pa, pb = C.ps[6], C.ps[7]
        Bpa, Bpb = C.Bps[6], C.Bps[7]
        for m in range(NCH):
            zz = z(m, h)
            P.op("pe", lambda e, zz=zz, m=m: e.matmul(pa[:], lhsT=ones, rhs=zz, start=(m == 0), stop=(m == NCH - 1)),
                 reads=[Bones, Bz[m][h]], writes=[Bpa])
        for m in range(NCH):
            zz = z(m, h)
            s_, bs_ = sq[m % 2], Bsq[m % 2]
            P.op("act", lambda e, zz=zz, s_=s_: e.activation(out=s_, in_=zz, func=AF.Square), reads=[Bz[m][h]], writes=[bs_])
            P.op("pe", lambda e, s_=s_, m=m: e.matmul(pb[:], lhsT=ones, rhs=s_, start=(m == 0), stop=(m == NCH - 1)),
                 reads=[Bones, bs_], writes=[Bpb])
        P.op("dve", lambda e: e.tensor_scalar(out=mean, in0=pa[:], scalar1=1.0 / D, scalar2=None, op0=ALU.mult),
             reads=[Bpa], writes=[Bmean])
        P.op("dve", lambda e: e.tensor_tensor(out=var, in0=mean, in1=mean, op=ALU.mult), reads=[Bmean], writes=[Bvar])
        P.op("dve", lambda e: e.scalar_tensor_tensor(out=var, in0=pb[:], scalar=1.0 / D, in1=var, op0=ALU.mult, op1=ALU.subtract),
             reads=[Bpb, Bvar], writes=[Bvar])
        P.op("dve", lambda e: e.tensor_scalar(out=var, in0=var, scalar1=EPS, scalar2=None, op0=ALU.add), reads=[Bvar], writes=[Bvar])
        P.op("act", lambda e: e.activation(out=var, in_=var, func=AF.Sqrt), reads=[Bvar], writes=[Bvar])
        P.op("dve", lambda e: e.reciprocal(out=rstd, in_=var), reads=[Bvar], writes=[Brstd])
        for m in range(NCH):
            zz = z(m, h)
            P.op("pool", lambda e, zz=zz: e.tensor_tensor(out=zz, in0=zz, in1=mean, op=ALU.subtract),
                 reads=[Bz[m][h], Bmean], writes=[Bz[m][h]])
            P.op("dve", lambda e, zz=zz: e.tensor_tensor(out=zz, in0=zz, in1=rstd, op=ALU.mult),
                 reads=[Bz[m][h], Brstd], writes=[Bz[m][h]])
            for o in outs:
                if o[0] == "bf16":
                    _, off, Bo, sc, bi = o
                    dst = C.bf(off + (m * NT + h * 512) * 2, 512)
                    P.op("act", lambda e, zz=zz, dst=dst, m=m, sc=sc, bi=bi: e.activation(out=dst, in_=zz, func=AF.Identity, scale=sc(m), bias=bi(m)),
                         reads=[Bz[m][h]], writes=[Bo[m][h]])
            for o in outs:
                if o[0] == "f32":
                    _, sc, bi = o
                    P.op("act", lambda e, zz=zz, m=m, sc=sc, bi=bi: e.activation(out=zz, in_=zz, func=AF.Identity, scale=sc(m), bias=bi(m)),
                         reads=[Bz[m][h]], writes=[Bz[m][h]])


def phase_R(C, dr, Bz, Bot):
    P = C.P
    nc = C.nc
    ws = WStream(C)
    z = lambda m, h: C.f32(OFF_ACC + (m * NT + h * 512) * 4, 512)
    ot = lambda k, h: C.bf(OFF_OT + (k * NT + h * 512) * 2, 512)
    xb = lambda k, h: C.bf(OFF_XB + (k * NT + h * 512) * 2, 512)
    Bxb = [[Buf("xb%d_%d" % (m, h)) for h in range(2)] for m in range(NCH)]
    lnp = C.lnp
    col = lambda w: (lambda m: lnp[:, w * NCH + m: w * NCH + m + 1])

    for m in range(NCH):
        wr, bwr = ws.push(dr["woutp"][m])
        for h in range(2):
            pt, bpt = C.ps[(m * 2 + h) % 4], C.Bps[(m * 2 + h) % 4]
            for k in range(NCH):
                P.op("pe", lambda e, pt=pt, wr=wr, k=k, h=h: e.matmul(pt[:], lhsT=wr[:, k * 128:(k + 1) * 128], rhs=ot(k, h),
                                                                  start=(k == 0), stop=(k == NCH - 1)),
                     reads=[bwr, Bot[k][h]], writes=[bpt])
            P.op("dve", lambda e, pt=pt, m=m, h=h: e.scalar_tensor_tensor(out=z(m, h), in0=z(m, h), scalar=ALPHA, in1=pt[:],
                                                                        op0=ALU.mult, op1=ALU.add),
                 reads=[bpt, Bz[m][h]], writes=[Bz[m][h]])
    if getattr(C, 'stop', None) == 'R1':
        return store_z(C, dr, Bz)
    layer_norm_fm(C, OFF_ACC, Bz, None, None,
                  [("bf16", OFF_XB, Bxb, col(0), col(1)), ("f32", col(4), col(5))], OFF_MISC + 8 * KB)

    if getattr(C, 'stop', None) == 'R2':
        return store_z(C, dr, Bz)
    MO = OFF_OT
    P.barrier()
    wrt = C.f32(MO, NCH * 36)
    Bwrt = Buf("wrt")
    rbb = C.f32(MO + 4 * KB, 36)
    Brbb = Buf("rbb")
    P.dma("sp", lambda e: e.dma_start(out=wrt, in_=dr["wr"]), writes=[Bwrt])
    P.dma("sp", lambda e: e.dma_start(out=rbb, in_=dr["rb"].broadcast_to([128, 36])), writes=[Brbb])
    lg = C.f32(MO + 5 * KB, 8 * 36)
    Blg = Buf("lg")
    comb = C.f32(MO + 7 * KB, 8 * 32)
    Bcomb = Buf("comb")
    sc = C.f32(MO + 9 * KB, 64)
    Bsc = Buf("sc")
    tmp32 = C.f32(MO + 10 * KB, 64)
    Btmp = Buf("tmp32")
    prt, bprt = C.ps[4], C.Bps[4]
    for tt in range(8):
        h, o = tt // 4, (tt % 4) * 128
        for m in range(NCH):
            P.op("pe", lambda e, tt=tt, m=m, h=h, o=o: e.matmul(prt[:, tt * 36:(tt + 1) * 36], lhsT=z(m, h)[:, o:o + 128],
                                                              rhs=wrt[:, m * 36:(m + 1) * 36], start=(m == 0), stop=(m == NCH - 1)),
                 reads=[Bz[m][h], Bwrt], writes=[bprt])
    for tt in range(8):
        l = lg[:, tt * 36:(tt + 1) * 36]
        P.op("dve", lambda e, tt=tt, l=l: e.scalar_tensor_tensor(out=l, in0=prt[:, tt * 36:(tt + 1) * 36], scalar=1.0 / ALPHA, in1=rbb,
                                                               op0=ALU.mult, op1=ALU.add), reads=[bprt, Brbb], writes=[Blg])
        gl = lg[:, tt * 36: tt * 36 + 4]
        el = lg[:, tt * 36 + 4: tt * 36 + 36]
        s = lambda i: sc[:, i:i + 1]
        cm = comb[:, tt * 32:(tt + 1) * 32]
        t32 = tmp32[:, 0:32]
        t4 = tmp32[:, 32:36]
        t4b = tmp32[:, 36:40]
        V = lambda fn, r=(Blg, Bsc, Btmp, Bcomb), w=(Blg, Bsc, Btmp, Bcomb): P.op("dve", fn, reads=list(r), writes=list(w))
        A = lambda fn: P.op("act", fn, reads=[Blg, Bsc, Btmp, Bcomb], writes=[Blg, Bsc, Btmp, Bcomb])
        V(lambda e, gl=gl: e.reduce_max(out=s(0), in_=gl, axis=AX.X))
        V(lambda e, gl=gl: e.tensor_scalar(out=t4, in0=gl, scalar1=s(0), scalar2=None, op0=ALU.subtract))
        A(lambda e: e.activation(out=t4b, in_=t4, func=AF.Exp))
        V(lambda e: e.reduce_sum(out=s(1), in_=t4b, axis=AX.X))
        V(lambda e: e.reciprocal(out=s(1), in_=s(1)))
        V(lambda e: e.tensor_scalar(out=t4, in0=t4, scalar1=0.0, scalar2=-BIG, op0=ALU.is_lt, op1=ALU.mult))
        for g in range(4):
            V(lambda e, g=g, el=el: e.tensor_scalar(out=el[:, g * 8:(g + 1) * 8], in0=el[:, g * 8:(g + 1) * 8],
                                                   scalar1=t4[:, g:g + 1], scalar2=None, op0=ALU.add))
        V(lambda e, el=el: e.reduce_max(out=s(2), in_=el, axis=AX.X))
        V(lambda e, el=el: e.tensor_scalar(out=t32, in0=el, scalar1=s(2), scalar2=None, op0=ALU.is_equal))
        V(lambda e, el=el: e.scalar_tensor_tensor(out=el, in0=t32, scalar=-BIG, in1=el, op0=ALU.mult, op1=ALU.add))
        V(lambda e, el=el: e.reduce_max(out=s(3), in_=el, axis=AX.X))
        V(lambda e: e.tensor_tensor(out=s(4), in0=s(3), in1=s(2), op=ALU.subtract))
        A(lambda e: e.activation(out=s(4), in_=s(4), func=AF.Exp))
        V(lambda e: e.tensor_scalar(out=s(4), in0=s(4), scalar1=1.0, scalar2=None, op0=ALU.add))
        V(lambda e: e.reciprocal(out=s(5), in_=s(4)))
        V(lambda e: e.tensor_scalar(out=s(6), in0=s(5), scalar1=-1.0, scalar2=1.0, op0=ALU.mult, op1=ALU.add))
        V(lambda e: e.tensor_tensor(out=s(5), in0=s(5), in1=s(1), op=ALU.mult))
        V(lambda e: e.tensor_tensor(out=s(6), in0=s(6), in1=s(1), op=ALU.mult))
        V(lambda e, cm=cm: e.tensor_scalar(out=cm, in0=t32, scalar1=s(5), scalar2=None, op0=ALU.mult))
        V(lambda e, el=el: e.tensor_scalar(out=t32, in0=el, scalar1=s(3), scalar2=None, op0=ALU.is_equal))
        V(lambda e, cm=cm: e.scalar_tensor_tensor(out=cm, in0=t32, scalar=s(6), in1=cm, op0=ALU.mult, op1=ALU.add))
    combT = C.f32(MO + 12 * KB, 1024)
    BcombT = Buf("combT")
    pct, bpct = C.ps[5], C.Bps[5]
    for tt in range(8):
        hh, o = tt // 4, (tt % 4) * 128
        P.op("pe", lambda e, tt=tt, o=o: e.transpose(pct[0:32, o:o + 128], comb[:, tt * 32:(tt + 1) * 32], C.ident32),
             reads=[Bcomb, C.Bident], writes=[bpct])
        if tt % 4 == 3:
            P.op("dve", lambda e, hh=hh: e.tensor_copy(out=combT[0:32, hh * 512:(hh + 1) * 512], in_=pct[0:32, :]),
                 reads=[bpct], writes=[BcombT])
    Bcwd = Buf("cwd")
    P.dma("sp", lambda e: e.dma_start(out=dr["cwd"], in_=combT[0:32, :]), reads=[BcombT], writes=[Bcwd])

    if getattr(C, 'stop', None) == 'R3':
        P.dma('sp', lambda e: e.dma_start(out=dr['outT'][0:32, :], in_=combT[0:32, :]), reads=[BcombT], sembuf=C.Bout)
        P.dma('sp', lambda e: e.dma_start(out=dr['outT'][128:256, 0:288], in_=lg), reads=[Blg], sembuf=C.Bout)
        P.dma('sp', lambda e: e.dma_start(out=dr['outT'][256:384, 0:64], in_=sc), reads=[Bsc], sembuf=C.Bout)
        P.dma('sp', lambda e: e.dma_start(out=dr['outT'][512:640, 0:36], in_=rbb), reads=[Brbb], sembuf=C.Bout)
        P.dma('sp', lambda e: e.dma_start(out=dr['outT'][640:768, 0:576], in_=wrt), reads=[Bwrt], sembuf=C.Bout)
        P.op('dve', lambda e: e.tensor_copy(out=tmp32[:, 0:36], in_=prt[:, 0:36]), reads=[bprt], writes=[Btmp])
        P.dma('sp', lambda e: e.dma_start(out=dr['outT'][768:896, 0:36], in_=tmp32[:, 0:36]), reads=[Btmp], sembuf=C.Bout)
        P.dma('sp', lambda e: e.dma_start(out=dr['outT'][384:512, 0:256], in_=comb), reads=[Bcomb], sembuf=C.Bout)
        return
    cwb = [C.f32(MO + 16 * KB + i * 4 * KB, 1024) for i in range(2)]
    Bcwb = [Buf("cwb0"), Buf("cwb1")]
    hT = [[C.bf(MO + 24 * KB + (i * 4 + fc) * 2 * KB, 1024) for fc in range(4)] for i in range(1)]
    BhT = [[Buf("h%d" % fc) for fc in range(4)] for i in range(1)]
    sg = [C.f32(OFF_MISC + i * 2 * KB, 512) for i in range(2)]
    Bsg = [Buf("sg0"), Buf("sg1")]
    tu = [C.f32(OFF_MISC + 4 * KB + i * 2 * KB, 512) for i in range(2)]
    Btu = [Buf("tu0"), Buf("tu1")]
    cnt = 0
    dcnt = 0
    for ex in (C.expert_list if getattr(C, 'expert_list', None) is not None else range(NEXP)):
        cw, bcw = cwb[ex % 2], Bcwb[ex % 2]
        P.dma("pool", lambda e, cw=cw, ex=ex: e.dma_start(out=cw, in_=dr["cwd"][ex:ex + 1, :].broadcast_to([128, 1024])),
              reads=[Bcwd], writes=[bcw])
        for fc in range(4):
            wg, bwg = ws.push(dr["moew"][ex, 2 * fc])
            wu, bwu = ws.push(dr["moew"][ex, 2 * fc + 1])
            for h in range(2):
                pg, bpg = C.ps[(cnt % 2) * 2], C.Bps[(cnt % 2) * 2]
                pu, bpu = C.ps[(cnt % 2) * 2 + 1], C.Bps[(cnt % 2) * 2 + 1]
                sgi, bsgi = sg[cnt % 2], Bsg[cnt % 2]
                tui, btui = tu[cnt % 2], Btu[cnt % 2]
                cnt += 1
                for k in range(NCH):
                    P.op("pe", lambda e, pg=pg, wg=wg, k=k, h=h: e.matmul(pg[:], lhsT=wg[:, k * 128:(k + 1) * 128], rhs=xb(k, h),
                                                                      start=(k == 0), stop=(k == NCH - 1)),
                         reads=[bwg, Bxb[k][h]], writes=[bpg])
                for k in range(NCH):
                    P.op("pe", lambda e, pu=pu, wu=wu, k=k, h=h: e.matmul(pu[:], lhsT=wu[:, k * 128:(k + 1) * 128], rhs=xb(k, h),
                                                                      start=(k == 0), stop=(k == NCH - 1)),
                         reads=[bwu, Bxb[k][h]], writes=[bpu])
                P.op("act", lambda e, pg=pg, sgi=sgi: e.activation(out=sgi, in_=pg[:], func=AF.Silu), reads=[bpg], writes=[bsgi])
                P.op("dve", lambda e, pu=pu, tui=tui, cw=cw, h=h: e.tensor_tensor(out=tui, in0=pu[:], in1=cw[:, h * 512:(h + 1) * 512], op=ALU.mult),
                     reads=[bpu, bcw], writes=[btui])
                hh = hT[0][fc][:, h * 512:(h + 1) * 512]
                P.op("dve", lambda e, hh=hh, sgi=sgi, tui=tui: e.tensor_tensor(out=hh, in0=sgi, in1=tui, op=ALU.mult),
                     reads=[bsgi, btui], writes=[BhT[0][fc]])
        wd = [ws.push(dr["moew"][ex, 8 + fc]) for fc in range(4)]
        for m in range(NCH):
            for h in range(2):
                pd, bpd = C.ps[4 + dcnt % 2], C.Bps[4 + dcnt % 2]
                dcnt += 1
                for fc in range(4):
                    P.op("pe", lambda e, pd=pd, fc=fc, m=m, h=h, w=wd[fc][0]: e.matmul(pd[:], lhsT=w[:, m * 128:(m + 1) * 128],
                                                                                  rhs=hT[0][fc][:, h * 512:(h + 1) * 512],
                                                                                  start=(fc == 0), stop=(fc == 3)),
                         reads=[wd[fc][1], BhT[0][fc]], writes=[bpd])
                P.op("dve", lambda e, pd=pd, m=m, h=h: e.tensor_tensor(out=z(m, h), in0=z(m, h), in1=pd[:], op=ALU.add),
                     reads=[bpd, Bz[m][h]], writes=[Bz[m][h]])
    if getattr(C, 'stop', None) == 'R4':
        return store_z(C, dr, Bz)
    layer_norm_fm(C, OFF_ACC, Bz, None, None, [("f32", col(2), col(3))], OFF_MISC + 8 * KB)
    store_z(C, dr, Bz)


def store_z(C, dr, Bz):
    for m in range(NCH):
        C.P.dma("sp", lambda e, m=m: e.dma_start(out=dr["outT"][m * 128:(m + 1) * 128, :], in_=C.f32(OFF_ACC + m * NT * 4, NT)),
                reads=[Bz[m][0], Bz[m][1]], sembuf=C.Bout)


def setup_consts(C, dr):
    P = C.P
    base = 202 * KB
    C.ones32 = C.f32(base, 128)
    C.ident32 = C.f32(base + 512, 128)
    C.lnp = C.f32(base + 1024, 6 * NCH)
    C.Bones, C.Bident, C.Blnp, C.Bout = Buf("ones"), Buf("ident", persist=True), Buf("lnp", persist=True), Buf("out", persist=True)
    P.op("dve", lambda e: e.memset(C.ones32, 1.0), writes=[C.Bones])
    P.dma("sp", lambda e: e.dma_start(out=C.ident32, in_=dr["ident"]), writes=[C.Bident])
    if "lnp" in dr:
        load_lnp(C, dr)


def load_lnp(C, dr):
    P = C.P
    P.dma("sp", lambda e: e.dma_start(out=C.lnp[:, 0:4 * NCH], in_=dr["lnp"]), writes=[C.Blnp])
    P.op("dve", lambda e: e.tensor_scalar(out=C.lnp[:, 4 * NCH:6 * NCH], in0=C.lnp[:, 0:2 * NCH], scalar1=ALPHA, scalar2=None, op0=ALU.mult),
         reads=[C.Blnp], writes=[C.Blnp])


def declare_R_dram(nc):
    dr = {}
    dr["xT"] = nc.dram_tensor("xT", [D, NT], F32, kind="ExternalInput").ap()
    dr["woutp"] = nc.dram_tensor("woutp", [NCH, 128, 2048], F32, kind="ExternalInput").ap()
    dr["moew"] = nc.dram_tensor("moew", [NEXP, 12, 128, 2048], F32, kind="ExternalInput").ap()
    dr["wr"] = nc.dram_tensor("wr", [128, NCH * 36], F32, kind="ExternalInput").ap()
    dr["rb"] = nc.dram_tensor("rb", [1, 36], F32, kind="ExternalInput").ap()
    dr["lnp"] = nc.dram_tensor("lnp", [128, 4 * NCH], F32, kind="ExternalInput").ap()
    dr["ident"] = nc.dram_tensor("ident", [128, 128], F32, kind="ExternalInput").ap()
    dr["cwd"] = nc.dram_tensor("cwd", [NEXP, NT], F32, kind="Internal").ap()
    dr["outT"] = nc.dram_tensor("outT", [D, NT], F32, kind="ExternalOutput").ap()
    return dr


def load_z(C, dr):
    Bz = [[Buf("z%d_%d" % (m, h)) for h in range(2)] for m in range(NCH)]
    for m in range(NCH):
        C.P.dma("sp", lambda e, m=m: e.dma_start(out=C.f32(OFF_ACC + m * NT * 4, NT), in_=dr["xT"][m * 128:(m + 1) * 128, :]),
                writes=[Bz[m][0], Bz[m][1]], sembuf=Bz[m][0])
    return Bz


def host_R_inputs(layer, w_out, ln_mix_g, ln_mix_b, ln_ffn_g, ln_ffn_b, moe_group_w, moe_group_b, moe_expert_w,
                  moe_expert_b, moe_w_gate, moe_w_up, moe_w_down):
    f = np.float32
    woutp = np.ascontiguousarray(w_out.reshape(NCH, 128, NCH, 128).transpose(2, 1, 0, 3).reshape(NCH, 128, 2048), dtype=f)
    wg = moe_w_gate[layer].reshape(NEXP, NCH, 128, 4, 128).transpose(0, 3, 2, 1, 4).reshape(NEXP, 4, 128, 2048)
    wu = moe_w_up[layer].reshape(NEXP, NCH, 128, 4, 128).transpose(0, 3, 2, 1, 4).reshape(NEXP, 4, 128, 2048)
    wd = moe_w_down[layer].reshape(NEXP, 4, 128, 2048)
    moew = np.empty((NEXP, 12, 128, 2048), dtype=f)
    moew[:, 0:8:2] = wg
    moew[:, 1:8:2] = wu
    moew[:, 8:12] = wd
    wr_full = np.concatenate([moe_group_w[layer], moe_expert_w[layer].transpose(1, 0, 2).reshape(D, 32)], axis=1)
    wr = np.ascontiguousarray(wr_full.reshape(NCH, 128, 36).transpose(1, 0, 2).reshape(128, NCH * 36), dtype=f)
    rb = np.concatenate([moe_group_b[layer], moe_expert_b[layer].reshape(32)])[None, :].astype(f)
    lnp = np.stack([ln_mix_g[layer], ln_mix_b[layer], ln_ffn_g[layer], ln_ffn_b[layer]], 0)
    lnp = np.ascontiguousarray(lnp.reshape(4, NCH, 128).transpose(2, 0, 1).reshape(128, 4 * NCH), dtype=f)
    return dict(woutp=woutp, moew=moew, wr=wr, rb=rb, lnp=lnp, ident=np.eye(128, dtype=f))


NBLK = 8
NTILE = 32
NSLOT = 8


class Stager:
    def __init__(self, C, tag):
        self.C = C
        self.stage = [C.f32(OFF_STAGE + i * 8 * KB, 2048) for i in range(NSTAGE)]
        self.B = [Buf("%s_st%d" % (tag, i)) for i in range(NSTAGE)]
        self.n = 0

    def load_cast(self, src, dst, Bdst, n, parts=128, q="sp"):
        P = self.C.P
        assert n <= 2048
        i = self.n
        self.n += 1
        s = i % NSTAGE
        st, bst = self.stage[s][0:parts, 0:n], self.B[s]
        P.dma(q, lambda e: e.dma_start(out=st, in_=src), writes=[bst])
        if i % 2 == 0:
            P.op("dve", lambda e: e.tensor_copy(out=dst, in_=st), reads=[bst], writes=[Bdst])
        else:
            P.op("act", lambda e: e.activation(out=dst, in_=st, func=AF.Copy), reads=[bst], writes=[Bdst])

    def load_w(self, src, dst_fn, Bdst, ntot, parts=128):
        for c0 in range(0, ntot, 2048):
            n = min(2048, ntot - c0)
            self.load_cast(src[:, c0:c0 + n], dst_fn(c0, n), Bdst, n, parts)


def make_xbf_scratch(C, dr, stg, Bx_prev=None):
    P = C.P
    tmp = [C.bf(i * 4 * KB, 2048) for i in range(4)]
    Bt = [Buf("xc%d" % i) for i in range(4)]
    Bx = Buf("xTb") if Bx_prev is None else Bx_prev
    Bxo = Buf("xTob")
    n = 0
    jobs = ((dr["xTfull"], dr["xTb"], S, Bx), (dr["xT"], dr["xTob"], NT, Bxo))
    if Bx_prev is not None:
        jobs = jobs[1:]
    for (src, dst, ncol, B) in jobs:
        for k in range(NCH):
            for c0 in range(0, ncol, 2048):
                w = min(2048, ncol - c0)
                t, bt = tmp[n % 4][:, 0:w], Bt[n % 4]
                n += 1
                stg.load_cast(src[k * 128:(k + 1) * 128, c0:c0 + w], t, bt, w)
                P.dma("pool", lambda e, t=t, dst=dst, k=k, c0=c0, w=w: e.dma_start(out=dst[k * 128:(k + 1) * 128, c0:c0 + w], in_=t),
                      reads=[bt], writes=[B], sembuf=bt)
    return Bx, Bxo


def load_xblk(C, src, Bsrc, c0, ncol, dst_off, Bdst, q="sp"):
    dst = C.bf(dst_off, NCH * ncol).rearrange("p (k n) -> p k n", k=NCH)
    s = src[:, c0:c0 + ncol].rearrange("(k p) n -> p k n", p=128)
    for g in range(4):
        C.P.dma(q, lambda e, g=g: e.dma_start(out=dst[:, g * 4:(g + 1) * 4, :], in_=s[:, g * 4:(g + 1) * 4, :]), reads=[Bsrc], writes=[Bdst])
    return lambda k, a=0, b=None: C.bf(dst_off + (k * ncol + a) * 2, (ncol if b is None else b) - a)


def proj_fm(C, ps_ap, Bps, w_ap, Bw, xk, Bx, M=128):
    for k in range(NCH):
        xa = xk(k)
        C.P.op("pe", lambda e, k=k, xa=xa: e.matmul(ps_ap, lhsT=w_ap[:, k * M:(k + 1) * M], rhs=xa, start=(k == 0), stop=(k == NCH - 1)),
               reads=[Bw, Bx], writes=[Bps])


def proj_tm(C, ps_ap, Bps, w_ap, Bw, xk, Bx, ncols):
    for k in range(NCH):
        xa = xk(k)
        C.P.op("pe", lambda e, k=k, xa=xa: e.matmul(ps_ap, lhsT=xa, rhs=w_ap[:, k * ncols:(k + 1) * ncols], start=(k == 0), stop=(k == NCH - 1)),
               reads=[Bw, Bx], writes=[Bps])


def setup_mix_consts(C, dr, names):
    P = C.P
    out = {}
    off = OFF_MISC
    for (nm, n, dt_) in names:
        if dt_ == "f32":
            ap = C.f32(off, n)
            off += n * 4
        else:
            ap = C.bf(off, n)
            off += n * 2
        off = (off + 3) // 4 * 4
        b = Buf(nm)
        P.dma("sp", lambda e, ap=ap, nm=nm: e.dma_start(out=ap, in_=dr[nm]), writes=[b])
        out[nm] = (ap, b)
    assert off <= 202 * KB, off
    return out


def phase_M0(C, dr, Bx_prev=None, vq=None):
    P = C.P
    cached = vq is not None and vq > 0
    store = vq == 0
    nc = C.nc
    stg = Stager(C, "m0")
    Bot = [[Buf("ot%d_%d" % (k, h)) for h in range(2)] for k in range(NCH)]
    otslot = lambda ch, u: C.bf(OFF_OT + (ch * NT + u * 128) * 2, 128)
    K = setup_mix_consts(C, dr, [("triL32", 128, "f32"), ("triU32", 128, "f32"), ("cind32", 2, "f32"), ("sel64", 128, "f32"),
                                 ("glamask", 128, "bf16"), ("selt", 32, "f32"), ("pent", 32, "f32"), ("dmask", 32 * 128, "bf16"),
                                 ("onesbf", 128, "bf16"), ("g_ng", 2, "f32"), ("f_gb", 8, "f32"), ("triF32", 128, "f32")])
    Bx, Bxo = make_xbf_scratch(C, dr, stg, Bx_prev)
    C.last_Bx = Bx
    P.barrier()
    ones32, Bones = C.ones32, C.Bones
    stop = getattr(C, 'stop', None)
    if stop == 'A':
        return Bot
    A0 = 0
    for hd in (range(4) if not getattr(C, 'skip_gla', False) else []):
        P.barrier()
        wq = C.bf(A0, 2048); wk = C.bf(A0 + 4 * KB, 2048); wkv = C.bf(A0 + 8 * KB, 6144)
        wgr = [C.bf(A0 + 20 * KB + i * 4 * KB, 2048) for i in range(2)]
        wglr = C.bf(A0 + 28 * KB, 256)
        w2 = C.bf(A0 + 29 * KB, 128, parts=17)
        Bw = Buf("gw")
        stg.load_w(dr["g_wq"][hd], lambda c0, n: wq[:, c0:c0 + n], Bw, 2048)
        stg.load_w(dr["g_wk"][hd], lambda c0, n: wk[:, c0:c0 + n], Bw, 2048)
        stg.load_w(dr["g_wkv"][hd], lambda c0, n: wkv[:, c0:c0 + n], Bw, 6144)
        for i in range(2):
            stg.load_w(dr["g_wgr"][hd, i], lambda c0, n, i=i: wgr[i][:, c0:c0 + n], Bw, 2048)
        stg.load_w(dr["g_wglr"], lambda c0, n: wglr[:, c0:c0 + n], Bw, 256)
        stg.load_cast(dr["g_w2"][hd], w2, Bw, 128, parts=17)
        if stop == 'G0a':
            return Bot
        XO = A0 + 32 * KB
        Bxb = [Buf("xblk0"), Buf("xblk1")]
        T0 = A0 + 64 * KB
        glrT = C.bf(T0, 512, parts=17); BglrT = Buf("glrT")
        kv = C.bf(T0 + 1 * KB, 384); Bkv = Buf("kv")
        e1 = C.f32(T0 + 2 * KB, 128); Be1 = Buf("e1")
        lap = C.f32(T0 + 3 * KB, 128); Blap = Buf("lap")
        ek = C.f32(T0 + 4 * KB, 128); Bek = Buf("ek")
        kend = C.bf(T0 + 5 * KB, 256); Bkend = Buf("kend")
        dec = C.f32(T0 + 6 * KB, 2); Bdec = Buf("dec")
        Sst = C.f32(T0 + 7 * KB, 256); BS = Buf("S")
        Ssel = [C.f32(T0 + 8 * KB + u * KB, 256) for u in range(NSLOT)]; BSsel = [Buf("Ssel%d" % u) for u in range(NSLOT)]
        selt, Bselt = K["selt"]
        P.op("pool", lambda e: e.memset(glrT[0:17, :], 1.0), writes=[BglrT])
        P.op("dve", lambda e: e.memset(Sst, 0.0), writes=[BS])
        for u in range(NSLOT):
            P.op("dve", lambda e, u=u: e.memset(Ssel[u], 0.0), writes=[BSsel[u]])
        p_kv, b_kv = C.ps[0], C.Bps[0]
        p_gl, b_gl = C.ps[1], C.Bps[1]
        p_m, b_m = C.ps[2], C.Bps[2]
        p_cs, b_cs = C.ps[3], C.Bps[3]

        def gate_and_la(xk, Bxk, t0, w, with_kv=True):
            xs = lambda k: xk(k, t0, t0 + 128)
            proj_tm(C, p_kv[:, 0:384], b_kv, wkv, Bw, xs, Bxk, 384)
            P.op("act", lambda e: e.activation(out=kv, in_=p_kv[:, 0:384], func=AF.Copy), reads=[b_kv], writes=[Bkv])
            P.op("pe", lambda e: e.matmul(p_m[:, 0:128], lhsT=glrT[0:17, t0:t0 + 128], rhs=w2[0:17, :], start=True, stop=True),
                 reads=[BglrT, Bw], writes=[b_m])
            P.op("act", lambda e: e.activation(out=e1, in_=p_m[:, 0:128], func=AF.Exp, scale=-1.0), reads=[b_m], writes=[Be1])
            P.op("act", lambda e: e.activation(out=lap, in_=e1, func=AF.Ln, bias=1.0), reads=[Be1], writes=[Blap])

        def glr_block(xk, Bxk, ntok):
            proj_fm(C, p_gl[0:16, 0:ntok], b_gl, wglr, Bw, lambda k: xk(k), Bxk, M=16)
            P.op("dve", lambda e: e.tensor_copy(out=glrT[0:16, 0:ntok], in_=p_gl[0:16, 0:ntok]), reads=[b_gl], writes=[BglrT])

        def state_steps(upd_sel_tile=None):
            triU, BtriU = K["triU32"]
            cind, Bcind = K["cind32"]
            P.op("pe", lambda e: e.matmul(p_m[:, 128:256], lhsT=triU, rhs=lap, start=True, stop=True), reads=[BtriU, Blap], writes=[b_m])
            P.op("act", lambda e: e.activation(out=ek, in_=p_m[:, 128:256], func=AF.Exp, scale=-1.0 / 16), reads=[b_m], writes=[Bek])
            if stop == 'G0d1':
                return
            P.op("pe", lambda e: e.matmul(p_m[:, 256:258], lhsT=lap, rhs=cind, start=True, stop=True), reads=[Bcind, Blap], writes=[b_m])
            P.op("act", lambda e: e.activation(out=dec, in_=p_m[:, 256:258], func=AF.Exp, scale=-1.0 / 16), reads=[b_m], writes=[Bdec])
            if stop == 'G0d2':
                return
            for c in range(2):
                P.op("dve", lambda e, c=c: e.scalar_tensor_tensor(out=kend[:, c * 128:(c + 1) * 128], in0=kv[:, 0:128], scalar=cind[:, c:c + 1], in1=ek,
                                                                 op0=ALU.mult, op1=ALU.mult), reads=[Bkv, Bek, Bcind], writes=[Bkend])
            for c in range(2):
                P.op("pe", lambda e, c=c: e.matmul(p_cs[:, c * 256:(c + 1) * 256], lhsT=kend[:, c * 128:(c + 1) * 128], rhs=kv[:, 128:384],
                                                   start=True, stop=True), reads=[Bkend, Bkv], writes=[b_cs])

        snapb = [C.f32(T0 + 26 * KB + i * KB, 256) for i in range(4)]
        Bsn = [Buf("snapb%d" % i) for i in range(4)]
        Bsnapd = Buf("snapd")
        if cached:
            ot_ = own_tiles(vq)
            for u in range(NSLOT):
                P.dma("sp", lambda e, u=u, tl=ot_[u], hd=hd, Ssel=Ssel: e.dma_start(out=Ssel[u], in_=dr["snap"][hd * NTILE + tl]), writes=[BSsel[u]])
        for blk in (range(NBLK) if not cached else []):
            xk = load_xblk(C, dr["xTb"], Bx, blk * 512, 512, XO + (blk % 2) * 16 * KB, Bxb[blk % 2])
            if stop == 'G0b0':
                return Bot
            glr_block(xk, Bxb[blk % 2], 512)
            if stop == 'G0b':
                return Bot
            for tt in range(4):
                tile_i = blk * 4 + tt
                gate_and_la(xk, Bxb[blk % 2], tt * 128, None)
                if stop == 'G0c':
                    return Bot
                state_steps()
                if stop and stop.startswith('G0d'):
                    return Bot
                u = tile_i // 4
                P.op("dve", lambda e, u=u, tile_i=tile_i: e.scalar_tensor_tensor(out=Ssel[u], in0=Sst, scalar=selt[:, tile_i:tile_i + 1], in1=Ssel[u],
                                                                               op0=ALU.mult, op1=ALU.add), reads=[BS, Bselt, BSsel[u]], writes=[BSsel[u]])
                if store:
                    sn, bsn = snapb[tile_i % 4], Bsn[tile_i % 4]
                    P.op("act", lambda e, sn=sn: e.activation(out=sn, in_=Sst, func=AF.Copy), reads=[BS], writes=[bsn])
                    P.dma("pool", lambda e, sn=sn, tile_i=tile_i, hd=hd: e.dma_start(out=dr["snap"][hd * NTILE + tile_i], in_=sn), reads=[bsn], writes=[Bsnapd], sembuf=bsn)
                for c in range(2):
                    P.op("dve", lambda e, c=c: e.scalar_tensor_tensor(out=Sst, in0=Sst, scalar=dec[:, c:c + 1], in1=p_cs[:, c * 256:(c + 1) * 256],
                                                                     op0=ALU.mult, op1=ALU.add), reads=[BS, Bdec, b_cs], writes=[BS])
        if stop == 'G1':
            return Bot
        O0 = T0 + 16 * KB
        eb = C.f32(O0, 128); enb = C.f32(O0 + 512, 128); Beb = Buf("eb")
        qd = C.bf(O0 + 1 * KB, 128); kin = C.bf(O0 + 1 * KB + 256, 128); Bqk = Buf("qk")
        sT = C.bf(O0 + 1 * KB + 512, 128); BsT = Buf("sT")
        Sa = C.bf(O0 + 2 * KB, 256); Sb32 = C.f32(O0 + 3 * KB, 256); Sb = C.bf(O0 + 4 * KB, 256); BSab = Buf("Sab")
        sq = [C.f32(O0 + 5 * KB + i * 512, 128) for i in range(2)]; Bsq = Buf("gsq")
        rs = C.f32(O0 + 6 * KB, 128); Brs = Buf("grs")
        sgr = [C.f32(O0 + 7 * KB + i * 512, 128) for i in range(2)]; Bsgr = Buf("sgr")
        tt_ = C.f32(O0 + 8 * KB, 128); Btt = Buf("gtt")
        triL, BtriL = K["triL32"]
        gmask, Bgmask = K["glamask"]
        ng, Bng = K["g_ng"]
        p_q, b_q = C.ps[4], C.Bps[4]
        p_k, b_k = C.ps[5], C.Bps[5]
        p_o, b_o = C.ps[6], C.Bps[6]
        p_g, b_g = C.ps[7], C.Bps[7]
        for half in range(2):
            xk = load_xblk(C, dr["xTob"], Bxo, half * 512, 512, XO + half * 16 * KB, Bxb[half])
            glr_block(xk, Bxb[half], 512)
            for tt in range(4):
                u = half * 4 + tt
                t0 = tt * 128
                gate_and_la(xk, Bxb[half], t0, None)
                state_steps()
                xs = lambda k, xk=xk, t0=t0: xk(k, t0, t0 + 128)
                P.op("pe", lambda e: e.matmul(p_m[:, 258:386], lhsT=lap, rhs=triL, start=True, stop=True), reads=[Blap, BtriL], writes=[b_m])
                P.op("act", lambda e: e.activation(out=eb, in_=p_m[:, 258:386], func=AF.Exp, scale=-1.0 / 16), reads=[b_m], writes=[Beb])
                P.op("act", lambda e: e.activation(out=enb, in_=p_m[:, 258:386], func=AF.Exp, scale=1.0 / 16), reads=[b_m], writes=[Beb])
                proj_fm(C, p_q[:, 0:128], b_q, wq, Bw, xs, Bxb[half])
                proj_fm(C, p_k[:, 0:128], b_k, wk, Bw, xs, Bxb[half])
                P.op("dve", lambda e: e.scalar_tensor_tensor(out=qd, in0=p_q[:, 0:128], scalar=128.0 ** -0.5, in1=eb, op0=ALU.mult, op1=ALU.mult),
                     reads=[b_q, Beb], writes=[Bqk])
                P.op("dve", lambda e: e.tensor_tensor(out=kin, in0=p_k[:, 0:128], in1=enb, op=ALU.mult), reads=[b_k, Beb], writes=[Bqk])
                P.op("act", lambda e, u=u: e.activation(out=Sa, in_=Ssel[u], func=AF.Copy), reads=[BSsel[u]], writes=[BSab])
                P.op("dve", lambda e, u=u: e.scalar_tensor_tensor(out=Sb32, in0=Ssel[u], scalar=dec[:, 0:1], in1=p_cs[:, 0:256], op0=ALU.mult, op1=ALU.add),
                     reads=[BSsel[u], Bdec, b_cs], writes=[BSab])
                P.op("act", lambda e: e.activation(out=Sb, in_=Sb32, func=AF.Copy), reads=[BSab], writes=[BSab])
                P.op("pe", lambda e: e.matmul(p_o[:, 0:128], lhsT=kin, rhs=qd, start=True, stop=True), reads=[Bqk], writes=[b_o])
                P.op("dve", lambda e: e.tensor_tensor(out=sT, in0=p_o[:, 0:128], in1=gmask, op=ALU.mult), reads=[b_o, Bgmask], writes=[BsT])
                for vc in range(2):
                    po = p_o[:, 128 + vc * 128: 256 + vc * 128]
                    P.op("pe", lambda e, vc=vc, po=po: e.matmul(po, lhsT=kv[:, 128 + vc * 128: 256 + vc * 128], rhs=sT, start=True, stop=False),
                         reads=[Bkv, BsT], writes=[b_o])
                    P.op("pe", lambda e, vc=vc, po=po: e.matmul(po[:, 0:64], lhsT=Sa[:, vc * 128:(vc + 1) * 128], rhs=qd[:, 0:64], start=False, stop=False),
                         reads=[BSab, Bqk], writes=[b_o])
                    P.op("pe", lambda e, vc=vc, po=po: e.matmul(po[:, 64:128], lhsT=Sb[:, vc * 128:(vc + 1) * 128], rhs=qd[:, 64:128], start=False, stop=True),
                         reads=[BSab, Bqk], writes=[b_o])
                    P.op("act", lambda e, vc=vc, po=po: e.activation(out=sq[vc], in_=po, func=AF.Square), reads=[b_o], writes=[Bsq])
                for vc in range(2):
                    P.op("pe", lambda e, vc=vc: e.matmul(p_k[:, 128:256], lhsT=ones32, rhs=sq[vc], start=(vc == 0), stop=(vc == 1)),
                         reads=[Bones, Bsq], writes=[b_k])
                P.op("dve", lambda e: e.tensor_scalar(out=rs, in0=p_k[:, 128:256], scalar1=1.0 / 256, scalar2=EPS, op0=ALU.mult, op1=ALU.add),
                     reads=[b_k], writes=[Brs])
                P.op("act", lambda e: e.activation(out=rs, in_=rs, func=AF.Sqrt), reads=[Brs], writes=[Brs])
                P.op("dve", lambda e: e.reciprocal(out=rs, in_=rs), reads=[Brs], writes=[Brs])
                for vc in range(2):
                    proj_fm(C, p_g[:, vc * 128:(vc + 1) * 128], b_g, wgr[vc], Bw, xs, Bxb[half])
                    P.op("act", lambda e, vc=vc: e.activation(out=sgr[vc], in_=p_g[:, vc * 128:(vc + 1) * 128], func=AF.Silu), reads=[b_g], writes=[Bsgr])
                    po = p_o[:, 128 + vc * 128: 256 + vc * 128]
                    P.op("dve", lambda e, po=po: e.tensor_tensor(out=tt_, in0=po, in1=rs, op=ALU.mult), reads=[b_o, Brs], writes=[Btt])
                    ch = hd * 2 + vc
                    P.op("dve", lambda e, vc=vc, ch=ch, u=u: e.scalar_tensor_tensor(out=otslot(ch, u), in0=tt_, scalar=ng[:, vc:vc + 1], in1=sgr[vc],
                                                                                  op0=ALU.mult, op1=ALU.mult),
                         reads=[Btt, Bng, Bsgr], writes=[Bot[ch][u // 4]])
    if stop == 'G':
        return Bot
    for pr in range(4):
        P.barrier()
        wq = [C.bf(A0 + i * 4 * KB, 2048) for i in range(2)]
        wk = [C.bf(A0 + 8 * KB + i * 4 * KB, 2048) for i in range(2)]
        wvf = C.bf(A0 + 16 * KB, NCH * 258)
        Bw = Buf("fw")
        for i in range(2):
            stg.load_w(dr["f_wq"][pr * 2 + i], lambda c0, n, i=i: wq[i][:, c0:c0 + n], Bw, 2048)
            stg.load_w(dr["f_wk"][pr * 2 + i], lambda c0, n, i=i: wk[i][:, c0:c0 + n], Bw, 2048)
        stg.load_w(dr["f_wvf"][pr], lambda c0, n: wvf[:, c0:c0 + n], Bw, NCH * 258)
        XO = A0 + 26 * KB
        Bxb = [Buf("fxblk0"), Buf("fxblk1")]
        KT = [C.bf(A0 + 58 * KB + i * 8 * KB, S) for i in range(2)]
        BKT = [[Buf("KT%d_%d" % (i, b)) for b in range(NBLK)] for i in range(2)]
        VV = C.bf(A0 + 74 * KB, NTILE * 256)
        BVV = [Buf("VV%d" % t) for t in range(NTILE)]
        FB = 128 * KB
        QT = [C.bf(FB + i * 2 * KB, NT) for i in range(2)]
        BQT = [Buf("QT0"), Buf("QT1")]
        ffv = C.f32(FB + 4 * KB, 64); Bffv = Buf("ffv")
        cc = C.f32(FB + 4 * KB + 256, 64); Bcc = Buf("cc")
        cbc = C.f32(FB + 4 * KB + 512, 64); Bcbc = Buf("cbc")
        tot = C.f32(FB + 4 * KB + 768, 64); Btot = Buf("tot")
        cref = C.f32(FB + 5 * KB, 16); Bcref = Buf("cref")
        bias = C.f32(FB + 6 * KB, 2 * NTILE * NSLOT); Bbias = Buf("bias")
        PT = [C.bf(FB + 8 * KB + i * KB, 512) for i in range(4)]; BPT = [Buf("PT%d" % i) for i in range(4)]
        rin = C.f32(FB + 12 * KB, 128); Brin = Buf("rin")
        gb, Bgb = K["f_gb"]
        selt, Bselt = K["selt"]
        pent, Bpent = K["pent"]
        dmask, Bdmask = K["dmask"]
        onesbf, Bonesbf = K["onesbf"]
        triL, BtriL = K["triL32"]
        sel64, Bsel64 = K["sel64"]
        p_a, b_a = C.ps[0], C.Bps[0]
        p_b, b_b = C.ps[1], C.Bps[1]
        Bfsc = Buf("fscr")
        if cached:
            for i in range(2):
                P.dma("sp", lambda e, i=i, pr=pr, KT=KT: e.dma_start(out=KT[i], in_=dr["fk_s"][pr][:, i * S:(i + 1) * S]), writes=BKT[i], sembuf=BKT[i][0])
            P.dma("sp", lambda e, pr=pr, VV=VV: e.dma_start(out=VV, in_=dr["fv_s"][pr]), writes=BVV, sembuf=BVV[0])
            P.dma("sp", lambda e, pr=pr, cc=cc: e.dma_start(out=cc, in_=dr["fc_s"][pr][:, 0:64]), writes=[Bcc])
            P.dma("sp", lambda e, pr=pr, cbc=cbc: e.dma_start(out=cbc, in_=dr["fc_s"][pr][:, 64:128]), writes=[Bcbc])
        for blk in (range(NBLK) if not cached else []):
            xk = load_xblk(C, dr["xTb"], Bx, blk * 512, 512, XO + (blk % 2) * 16 * KB, Bxb[blk % 2])
            for i in range(2):
                proj_fm(C, p_a[:, :], b_a, wk[i], Bw, lambda k: xk(k), Bxb[blk % 2])
                P.op("act", lambda e, i=i, blk=blk: e.activation(out=KT[i][:, blk * 512:(blk + 1) * 512], in_=p_a[:, :], func=AF.Copy),
                     reads=[b_a], writes=[BKT[i][blk]])
            for tt in range(4):
                t = blk * 4 + tt
                proj_tm(C, p_b[:, 0:258], b_b, wvf, Bw, lambda k, tt=tt: xk(k, tt * 128, tt * 128 + 128), Bxb[blk % 2], 258)
                P.op("dve", lambda e, t=t: e.tensor_copy(out=VV[:, t * 256:(t + 1) * 256], in_=p_b[:, 0:256]), reads=[b_b], writes=[BVV[t]])
                for i in range(2):
                    P.op("dve", lambda e, t=t, i=i: e.tensor_copy(out=ffv[:, i * 32 + t: i * 32 + t + 1], in_=p_b[:, 256 + i:257 + i]),
                         reads=[b_b], writes=[Bffv])
        for half in range(2):
            xk = load_xblk(C, dr["xTob"], Bxo, half * 512, 512, XO + half * 16 * KB, Bxb[half])
            for i in range(2):
                proj_fm(C, p_a[:, :], b_a, wq[i], Bw, lambda k: xk(k), Bxb[half])
                P.op("act", lambda e, i=i, half=half: e.activation(out=QT[i][:, half * 512:(half + 1) * 512], in_=p_a[:, :], func=AF.Copy),
                     reads=[b_a], writes=[BQT[i]])
        if stop == 'F0':
            return Bot
        for i in (range(2) if not cached else []):
            hidx = pr * 2 + i
            fs = ffv[:, i * 32:(i + 1) * 32]
            P.op("dve", lambda e, fs=fs, hidx=hidx: e.tensor_scalar(out=fs, in0=fs, scalar1=gb[:, hidx:hidx + 1], scalar2=None, op0=ALU.add),
                 reads=[Bffv, Bgb], writes=[Bffv])
            P.op("act", lambda e, fs=fs: e.activation(out=fs, in_=fs, func=AF.Exp, scale=-1.0), reads=[Bffv], writes=[Bffv])
            P.op("act", lambda e, fs=fs: e.activation(out=fs, in_=fs, func=AF.Ln, bias=1.0), reads=[Bffv], writes=[Bffv])
            P.op("pe", lambda e, fs=fs: e.matmul(p_a[:, 0:32], lhsT=triL_full(K), rhs=fs, start=True, stop=True), reads=[Bffv, K["triF32"][1]], writes=[b_a])
            P.op("pe", lambda e, fs=fs: e.matmul(p_a[:, 32:64], lhsT=ones32, rhs=fs, start=True, stop=True), reads=[Bffv, Bones], writes=[b_a])
            ci = cc[:, i * 32:(i + 1) * 32]
            ti = tot[:, i * 32:(i + 1) * 32]
            P.op("dve", lambda e, ti=ti: e.tensor_copy(out=ti, in_=p_a[:, 32:64]), reads=[b_a], writes=[Btot])
            P.op("dve", lambda e, ci=ci: e.tensor_copy(out=ci, in_=p_a[:, 0:32]), reads=[b_a], writes=[Bcc])
            for j in range(1, NTILE):
                if j >= 2:
                    P.op("dve", lambda e, ti=ti, j=j: e.tensor_tensor(out=ti[:, j - 1:j], in0=ti[:, j - 1:j], in1=ti[:, j - 2:j - 1], op=ALU.add),
                         reads=[Btot], writes=[Btot])
                P.op("dve", lambda e, ci=ci, ti=ti, j=j: e.tensor_tensor(out=ci[:, j:j + 1], in0=ci[:, j:j + 1], in1=ti[:, j - 1:j], op=ALU.add),
                     reads=[Btot, Bcc], writes=[Bcc])
            P.op("pe", lambda e, ci=ci: e.matmul(p_a[:, 64:96], lhsT=sel64, rhs=ci, start=True, stop=True), reads=[Bcc, Bsel64], writes=[b_a])
            cb = cbc[:, i * 32:(i + 1) * 32]
            P.op("dve", lambda e, cb=cb: e.tensor_copy(out=cb, in_=p_a[:, 64:96]), reads=[b_a], writes=[Bcbc])
        if store:
            for i in range(2):
                P.dma("pool", lambda e, i=i, pr=pr, KT=KT: e.dma_start(out=dr["fk_s"][pr][:, i * S:(i + 1) * S], in_=KT[i]), reads=BKT[i], writes=[Bfsc], sembuf=BKT[i][0])
            P.dma("pool", lambda e, pr=pr, VV=VV: e.dma_start(out=dr["fv_s"][pr], in_=VV), reads=BVV, writes=[Bfsc], sembuf=BVV[0])
            P.dma("pool", lambda e, pr=pr, cc=cc: e.dma_start(out=dr["fc_s"][pr][:, 0:64], in_=cc), reads=[Bcc], writes=[Bfsc], sembuf=Bcc)
            P.dma("pool", lambda e, pr=pr, cbc=cbc: e.dma_start(out=dr["fc_s"][pr][:, 64:128], in_=cbc), reads=[Bcbc], writes=[Bfsc], sembuf=Bcbc)
        for i in range(2):
            ci = cc[:, i * 32:(i + 1) * 32]
            cb = cbc[:, i * 32:(i + 1) * 32]
            for u in range(NSLOT):
                cr = cref[:, i * 8 + u: i * 8 + u + 1]
                for d_ in range(4):
                    j = 4 * u + d_
                    if d_ == 0:
                        P.op("dve", lambda e, cr=cr, cb=cb, j=j: e.tensor_scalar(out=cr, in0=cb[:, j:j + 1], scalar1=selt[:, j:j + 1], scalar2=None, op0=ALU.mult),
                             reads=[Bcbc, Bselt], writes=[Bcref])
                    else:
                        P.op("dve", lambda e, cr=cr, cb=cb, j=j: e.scalar_tensor_tensor(out=cr, in0=cb[:, j:j + 1], scalar=selt[:, j:j + 1], in1=cr,
                                                                                     op0=ALU.mult, op1=ALU.add), reads=[Bcbc, Bselt, Bcref], writes=[Bcref])
            for j in range(NTILE):
                bj = bias[:, (i * NTILE + j) * NSLOT:(i * NTILE + j + 1) * NSLOT]
                P.op("dve", lambda e, bj=bj, ci=ci, j=j, i=i: e.tensor_scalar(out=bj, in0=cref[:, i * 8:(i + 1) * 8], scalar1=-1.0, scalar2=ci[:, j:j + 1],
                                                                        op0=ALU.mult, op1=ALU.add), reads=[Bcref, Bcc], writes=[Bbias])
                u = j // 4
                P.op("dve", lambda e, bj=bj, j=j, u=u: e.tensor_tensor(out=bj[:, u:u + 1], in0=bj[:, u:u + 1], in1=pent[:, j:j + 1], op=ALU.add),
                     reads=[Bpent, Bbias], writes=[Bbias])
        if stop == 'F1':
            dbg = dr['dbg']
            P.dma('sp', lambda e: e.dma_start(out=dbg[:, 0:64], in_=cc), reads=[Bcc], sembuf=C.Bout)
            P.dma('sp', lambda e: e.dma_start(out=dbg[:, 64:80], in_=cref), reads=[Bcref], sembuf=C.Bout)
            P.dma('sp', lambda e: e.dma_start(out=dbg[:, 128:640], in_=bias), reads=[Bbias], sembuf=C.Bout)
            P.dma('sp', lambda e: e.dma_start(out=dbg[:, 640:704], in_=ffv), reads=[Bffv], sembuf=C.Bout)
            P.dma('sp', lambda e: e.dma_start(out=dbg[:, 704:768], in_=cbc), reads=[Bcbc], sembuf=C.Bout)
            return Bot
        fitems = [(i, u, g) for i in range(2) for u in range(NSLOT) for g in range(u + 1)]

        def emit_Sg(n):
            i, u, g = fitems[n]
            p_s, b_s = C.ps[n % 4], C.Bps[n % 4]
            qs = QT[i][:, u * 128:(u + 1) * 128]
            for jj in range(4):
                j = g * 4 + jj
                P.op("pe", lambda e, p_s=p_s, jj=jj, j=j, i=i, qs=qs: e.matmul(p_s[:, jj * 128:(jj + 1) * 128], lhsT=KT[i][:, j * 128:(j + 1) * 128], rhs=qs,
                                                                           start=True, stop=True), reads=[BKT[i][j // 4], BQT[i]], writes=[b_s])
        for n in range(min(2, len(fitems))):
            emit_Sg(n)
        for n, (i, u, g) in enumerate(fitems):
            hidx = pr * 2 + i
            J = 4 * u + 4
            ngrp = u + 1
            oi = i * NSLOT + u
            p_o, b_o = C.ps[4 + (oi % 2)], C.Bps[4 + (oi % 2)]
            p_r, b_r = C.ps[6 + (oi % 2)], C.Bps[6 + (oi % 2)]
            p_s, b_s = C.ps[n % 4], C.Bps[n % 4]
            pt, bpt = PT[n % 4], BPT[n % 4]
            for jj in range(4):
                j = g * 4 + jj
                bcol = bias[:, (i * NTILE + j) * NSLOT + u:(i * NTILE + j) * NSLOT + u + 1]
                P.op("act", lambda e, p_s=p_s, pt=pt, jj=jj, bcol=bcol: e.activation(out=pt[:, jj * 128:(jj + 1) * 128], in_=p_s[:, jj * 128:(jj + 1) * 128],
                                                                                 func=AF.Exp, scale=128.0 ** -0.5, bias=bcol),
                     reads=[b_s, Bbias], writes=[bpt])
            if g == ngrp - 1:
                P.op("pool", lambda e, pt=pt, u=u: e.tensor_tensor(out=pt, in0=pt, in1=dmask[:, u * 512:(u + 1) * 512], op=ALU.mult),
                     reads=[bpt, Bdmask], writes=[bpt])
            if n + 2 < len(fitems):
                emit_Sg(n + 2)
            for jj in range(4):
                j = g * 4 + jj
                P.op("pe", lambda e, pt=pt, jj=jj, j=j, i=i, p_o=p_o, J=J: e.matmul(p_o[:, 0:128], lhsT=VV[:, j * 256 + i * 128: j * 256 + (i + 1) * 128],
                                                                               rhs=pt[:, jj * 128:(jj + 1) * 128], start=(j == 0), stop=(j == J - 1)),
                     reads=[bpt, BVV[j]], writes=[b_o])
            for jj in range(4):
                j = g * 4 + jj
                P.op("pe", lambda e, pt=pt, jj=jj, j=j, p_r=p_r, J=J: e.matmul(p_r[:, 0:128], lhsT=onesbf, rhs=pt[:, jj * 128:(jj + 1) * 128],
                                                                          start=(j == 0), stop=(j == J - 1)), reads=[bpt, Bonesbf], writes=[b_r])
            if g == ngrp - 1:
                P.op("dve", lambda e, p_r=p_r: e.reciprocal(out=rin, in_=p_r[:, 0:128]), reads=[b_r], writes=[Brin])
                ch = 8 + hidx
                P.op("dve", lambda e, p_o=p_o, ch=ch, u=u: e.tensor_tensor(out=otslot(ch, u), in0=p_o[:, 0:128], in1=rin, op=ALU.mult),
                     reads=[b_o, Brin], writes=[Bot[ch][u // 4]])
    P.barrier()
    return Bot


def triL_full(K):
    return K["triF32"][0]


def own_tiles(cq):
    return [4 * u + (cq if u % 2 == 0 else 3 - cq) for u in range(NSLOT)]


def host_mix_tables(cq):
    import ml_dtypes
    bf = ml_dtypes.bfloat16
    f = np.float32
    own = own_tiles(cq)
    t = np.arange(128)
    same = (t[:, None] // 64) == (t[None, :] // 64)
    T = {}
    T["triL32"] = ((t[:, None] <= t[None, :]) & same).astype(f)
    T["triU32"] = ((t[:, None] > t[None, :]) & same).astype(f)
    T["triF32"] = (t[:, None] <= t[None, :]).astype(f)
    T["cind32"] = (t[:, None] // 64 == np.arange(2)[None, :]).astype(f)
    s64 = np.zeros((128, 128), f); s64[64, :] = 1.0
    T["sel64"] = s64
    T["glamask"] = ((t[:, None] <= t[None, :]) & same).astype(bf)
    selt = np.zeros((128, 32), f); pent = np.zeros((128, 32), f)
    dmask = np.zeros((128, 32, 128), f)
    negm = np.zeros((128, 32, 128), f)
    for u in range(NSLOT):
        for d_ in range(4):
            j = 4 * u + d_
            if j == own[u]:
                selt[:, j] = 1.0
                dmask[:, j, :] = (t[:, None] <= t[None, :])
                negm[:, j, :] = np.where(t[None, :] <= t[:, None], 0.0, -BIG)
            elif j < own[u]:
                dmask[:, j, :] = 1.0
            else:
                pent[:, j] = -BIG
                negm[:, j, :] = -BIG
    T["selt"] = selt
    T["pent"] = pent
    T["dmask"] = dmask.reshape(128, 32 * 128).astype(bf)
    T["negm"] = negm.reshape(128, 32 * 128).astype(bf)
    T["onesbf"] = np.ones((128, 128), bf)
    T["identbf"] = np.eye(128).astype(bf)
    return T


def fm_layout(W):
    n = W.shape[1]
    return np.ascontiguousarray(W.reshape(NCH, 128, n).transpose(1, 0, 2).reshape(128, NCH * n), dtype=np.float32)


def host_M0_inputs(a_w_in, a_gla_gate_w2, a_gla_gate_b, a_gla_norm_g, a_fox_gate_b):
    W = a_w_in[0]
    oq, ok_, ov, ogr, oglr, ofq, ofk, ofv, off_ = 0, 512, 1024, 2048, 3072, 3088, 4112, 5136, 6160
    d = {}
    d["g_wq"] = np.stack([fm_layout(W[:, oq + h * 128: oq + (h + 1) * 128]) for h in range(4)])
    d["g_wk"] = np.stack([fm_layout(W[:, ok_ + h * 128: ok_ + (h + 1) * 128]) for h in range(4)])
    d["g_wkv"] = np.stack([fm_layout(np.concatenate([W[:, ok_ + h * 128: ok_ + (h + 1) * 128], W[:, ov + h * 256: ov + (h + 1) * 256]], 1)) for h in range(4)])
    d["g_wgr"] = np.stack([np.stack([fm_layout(W[:, ogr + h * 256 + i * 128: ogr + h * 256 + (i + 1) * 128]) for i in range(2)]) for h in range(4)])
    d["g_wglr"] = fm_layout(W[:, oglr:oglr + 16])
    d["g_w2"] = np.stack([np.concatenate([a_gla_gate_w2[0][:, h * 128:(h + 1) * 128], a_gla_gate_b[0][None, h * 128:(h + 1) * 128]], 0) for h in range(4)]).astype(np.float32)
    d["g_ng"] = np.ascontiguousarray(a_gla_norm_g[0].reshape(2, 128).T, dtype=np.float32)
    d["f_gb"] = np.ascontiguousarray(np.broadcast_to(a_fox_gate_b[0][None, :], (128, 8)), dtype=np.float32)
    d["f_wq"] = np.stack([fm_layout(W[:, ofq + h * 128: ofq + (h + 1) * 128]) for h in range(8)])
    d["f_wk"] = np.stack([fm_layout(W[:, ofk + h * 128: ofk + (h + 1) * 128]) for h in range(8)])
    d["f_wvf"] = np.stack([fm_layout(np.concatenate([W[:, ofv + 2 * p * 128: ofv + (2 * p + 2) * 128], W[:, off_ + 2 * p: off_ + 2 * p + 2]], 1)) for p in range(4)])
    return d


M0_DRAM = [("g_wq", [4, 128, 2048]), ("g_wk", [4, 128, 2048]), ("g_wkv", [4, 128, 6144]), ("g_wgr", [4, 2, 128, 2048]), ("g_wglr", [128, 256]),
           ("g_w2", [4, 17, 128]), ("g_ng", [128, 2]), ("f_gb", [128, 8]), ("f_wq", [8, 128, 2048]), ("f_wk", [8, 128, 2048]),
           ("f_wvf", [4, 128, NCH * 258]),
           ("triL32", [128, 128]), ("triU32", [128, 128]), ("triF32", [128, 128]), ("cind32", [128, 2]), ("sel64", [128, 128]),
           ("selt", [128, 32]), ("pent", [128, 32])]
M0_DRAM_BF = [("glamask", [128, 128]), ("dmask", [128, 4096]), ("onesbf", [128, 128])]


def declare_mix_dram(nc, dr, f32list, bflist):
    for nm, shp in f32list:
        dr[nm] = nc.dram_tensor(nm, shp, F32, kind="ExternalInput").ap()
    for nm, shp in bflist:
        dr[nm] = nc.dram_tensor(nm, shp, BF16, kind="ExternalInput").ap()
    dr["xTfull"] = nc.dram_tensor("xTfull", [D, S], F32, kind="ExternalInput").ap()
    dr["xTb"] = nc.dram_tensor("xTb", [D, S], BF16, kind="ExternalOutput").ap()
    dr["xTob"] = nc.dram_tensor("xTob", [D, NT], BF16, kind="ExternalOutput").ap()


NEG2 = -3.0e38


def rope_evac(C, py, bpy, pyp, bpyp, cosb, sinb, Bcs, out_ap, Bout, t1, t2, Bt, view=None):
    P = C.P
    P.op("dve", lambda e: e.tensor_tensor(out=t1, in0=py, in1=cosb, op=ALU.mult), reads=[bpy, Bcs], writes=[Bt[0]])
    P.op("dve", lambda e: e.tensor_tensor(out=t2, in0=pyp, in1=sinb, op=ALU.mult), reads=[bpyp, Bcs], writes=[Bt[1]])
    a, b = (t1, t2) if view is None else (view(t1), view(t2))
    P.op("pool", lambda e: e.tensor_tensor(out=out_ap, in0=a, in1=b, op=ALU.add), reads=[Bt[0], Bt[1]], writes=[Bout])


def phase_M1(C, dr, pre=None):
    P = C.P
    stg = Stager(C, "m1")
    Bot = [[Buf("ot%d_%d" % (k, h)) for h in range(2)] for k in range(NCH)]
    K = setup_mix_consts(C, dr, [("negm", 4096, "bf16"), ("onesbf", 128, "bf16"), ("identbf", 128, "bf16")])
    negm, Bnegm = K["negm"]
    onesbf, Bonesbf = K["onesbf"]
    identbf, Bidentbf = K["identbf"]
    SM = OFF_MISC + 12 * KB
    iw = C.f32(SM, 128); Biw = Buf("iw")
    m8 = C.f32(SM + 512, 8); Bm8 = Buf("m8")
    wiw = C.bf(SM + 1024, 256); Bwiw = Buf("wiw")
    Bx, Bxo = make_xbf_scratch(C, dr, stg) if pre is None else pre
    P.barrier()
    stop = getattr(C, 'stop', None)
    maskT = [C.bf(sum(range(1, u + 1)) * KB, (u + 1) * 512) for u in range(NSLOT)]
    BmaskT = [Buf("maskT%d" % u) for u in range(NSLOT)]
    XBLK = 128 * KB
    Bxb = Buf("xblk")
    iqT = C.bf(36 * KB, 8 * NT); BiqT = Buf("iqT")
    ikA = C.bf(52 * KB, S); ikB = C.bf(60 * KB, S); Bik = [Buf("ik%d" % b) for b in range(NBLK)]
    acc = C.f32(68 * KB, S); Bacc = Buf("acc")
    maskq = C.bf(84 * KB, S); Bmq = Buf("maskq")
    t1 = C.f32(92 * KB, 512); t2 = C.f32(94 * KB, 512); Bt = [Buf("t1"), Buf("t2")]
    wsl = [C.bf(144 * KB + i * 4 * KB, 2048) for i in range(2)]; Bwsl = [Buf("wsl0"), Buf("wsl1")]
    tab = [C.f32(152 * KB + i * 2 * KB, 512) for i in range(4)]; Btab = Buf("tab")
    p0, b0 = C.ps[0], C.Bps[0]
    p1, b1 = C.ps[1], C.Bps[1]
    for i in range(2):
        stg.load_w(dr["d_wik"][i], lambda c0, n, i=i: wsl[i][:, c0:c0 + n], Bwsl[i], 2048)
    for blk in range(NBLK):
        xk = load_xblk(C, dr["xTb"], Bx, blk * 512, 512, XBLK, Bxb)
        for i in range(4):
            P.dma("sp", lambda e, i=i, blk=blk, tb=tab[i]: e.dma_start(out=tb, in_=dr["rtik"][i][:, blk * 512:(blk + 1) * 512]), writes=[Btab])
        proj_fm(C, p0[:, :], b0, wsl[0], Bwsl[0], lambda k, xk=xk: xk(k), Bxb)
        proj_fm(C, p1[:, :], b1, wsl[1], Bwsl[1], lambda k, xk=xk: xk(k), Bxb)
        rope_evac(C, p0[:, :], b0, p1[:, :], b1, tab[0], tab[1], Btab, ikA[:, blk * 512:(blk + 1) * 512], Bik[blk], t1, t2, Bt)
        rope_evac(C, p0[:, :], b0, p1[:, :], b1, tab[2], tab[3], Btab, ikB[:, blk * 512:(blk + 1) * 512], Bik[blk], t1, t2, Bt)
    stg.load_cast(dr["d_wiw"], wiw, Bwiw, 256)
    p2, b2 = C.ps[2], C.Bps[2]
    for half in range(2):
        xk = load_xblk(C, dr["xTob"], Bxo, half * 512, 512, XBLK, Bxb)
        for i in range(2):
            P.dma("sp", lambda e, i=i, half=half, tb=tab[i]: e.dma_start(out=tb, in_=dr["rt64o"][i][:, half * 512:(half + 1) * 512]), writes=[Btab])
        for tt in range(4):
            u = half * 4 + tt
            proj_tm(C, p2[:, u * 16:(u + 1) * 16], b2, wiw, Bwiw, lambda k, xk=xk, tt=tt: xk(k, tt * 128, tt * 128 + 128), Bxb, 16)
        for pc in range(8):
            for i in range(2):
                stg.load_w(dr["d_wiq"][pc, i], lambda c0, n, i=i: wsl[i][:, c0:c0 + n], Bwsl[i], 2048)
            proj_fm(C, p0[:, :], b0, wsl[0], Bwsl[0], lambda k, xk=xk: xk(k), Bxb)
            proj_fm(C, p1[:, :], b1, wsl[1], Bwsl[1], lambda k, xk=xk: xk(k), Bxb)
            rope_evac(C, p0[:, :], b0, p1[:, :], b1, tab[0], tab[1], Btab, iqT[:, pc * NT + half * 512: pc * NT + (half + 1) * 512], BiqT, t1, t2, Bt)
    P.op("dve", lambda e: e.tensor_copy(out=iw, in_=p2[:, 0:128]), reads=[b2], writes=[Biw])
    if stop == 'I0':
        return Bot
    rb = [tab[0], tab[1]]
    Brb = [Buf("rb0"), Buf("rb1")]
    vld = C.bf(156 * KB, 512); Bvld = Buf("vld")
    pT = C.ps[4][:].bitcast(BF16)
    bpT = C.Bps[4]
    cnt = 0
    for u in range(NSLOT):
        L = 512 * (u + 1)
        for kb in range(u + 1):
            for h in range(16):
                pc = h // 2
                ik_ = ikA if h % 2 == 0 else ikB
                ps_, bps_ = C.ps[2 + cnt % 2], C.Bps[2 + cnt % 2]
                r_, br_ = rb[cnt % 2], Brb[cnt % 2]
                cnt += 1
                P.op("pe", lambda e, ps_=ps_, pc=pc, u=u, ik_=ik_, kb=kb: e.matmul(ps_[:, :], lhsT=iqT[:, pc * NT + u * 128: pc * NT + (u + 1) * 128],
                                                                            rhs=ik_[:, kb * 512:(kb + 1) * 512], start=True, stop=True),
                     reads=[BiqT, Bik[kb]], writes=[bps_])
                P.op("act", lambda e, ps_=ps_, r_=r_: e.activation(out=r_, in_=ps_[:, :], func=AF.Relu, scale=1.0 / 32), reads=[bps_], writes=[br_])
                a_ = acc[:, kb * 512:(kb + 1) * 512]
                wcol = iw[:, u * 16 + h: u * 16 + h + 1]
                if h == 0:
                    P.op("dve", lambda e, a_=a_, r_=r_, wcol=wcol: e.tensor_scalar(out=a_, in0=r_, scalar1=wcol, scalar2=None, op0=ALU.mult),
                         reads=[br_, Biw], writes=[Bacc])
                else:
                    P.op("dve", lambda e, a_=a_, r_=r_, wcol=wcol: e.scalar_tensor_tensor(out=a_, in0=r_, scalar=wcol, in1=a_, op0=ALU.mult, op1=ALU.add),
                         reads=[br_, Biw, Bacc], writes=[Bacc])
        aw = acc[:, u * 512:(u + 1) * 512]
        nm = negm[:, u * 512:(u + 1) * 512]
        P.op("dve", lambda e, aw=aw, nm=nm: e.tensor_tensor(out=aw, in0=aw, in1=nm, op=ALU.add), reads=[Bacc, Bnegm], writes=[Bacc])
        if stop == 'I1' and u == 1:
            P.dma('sp', lambda e: e.dma_start(out=dr['dbg'][:, 0:1024], in_=acc[:, 0:1024]), reads=[Bacc], sembuf=C.Bout)
            return Bot
        al = acc[:, 0:L]
        for r in range(32):
            P.op("dve", lambda e, al=al: e.max(out=m8, in_=al), reads=[Bacc], writes=[Bm8])
            P.op("dve", lambda e, al=al: e.match_replace(out=al, in_to_replace=m8, in_values=al, imm_value=NEG2), reads=[Bacc, Bm8], writes=[Bacc])
        mq = maskq[:, 0:L]
        P.op("dve", lambda e, al=al, mq=mq: e.tensor_scalar(out=mq, in0=al, scalar1=-2.0e38, scalar2=None, op0=ALU.is_le), reads=[Bacc], writes=[Bmq])
        P.op("pool", lambda e, nm=nm: e.tensor_scalar(out=vld, in0=nm, scalar1=-1.0, scalar2=None, op0=ALU.is_ge), reads=[Bnegm], writes=[Bvld])
        mw = maskq[:, u * 512:(u + 1) * 512]
        P.op("pool", lambda e, mw=mw: e.tensor_tensor(out=mw, in0=mw, in1=vld, op=ALU.mult), reads=[Bmq, Bvld], writes=[Bmq])
        for j4 in range(u + 1):
            for jj in range(4):
                j = j4 * 4 + jj
                P.op("pe", lambda e, jj=jj, j=j: e.transpose(pT[:, jj * 128:(jj + 1) * 128], maskq[:, j * 128:(j + 1) * 128], identbf),
                     reads=[Bmq, Bidentbf], writes=[bpT])
            P.op("act", lambda e, u=u, j4=j4: e.activation(out=maskT[u][:, j4 * 512:(j4 + 1) * 512], in_=pT[:, 0:512], func=AF.Copy),
                 reads=[bpT], writes=[BmaskT[u]])
    if stop == 'I2':
        for u in range(NSLOT):
            P.dma('sp', lambda e, u=u: e.dma_start(out=dr['dbgm'][:, sum(range(1, u + 1)) * 512: sum(range(1, u + 2)) * 512], in_=maskT[u]),
                  reads=[BmaskT[u]], sembuf=C.Bout)
        return Bot
    XB2 = [128 * KB, 144 * KB]
    Bxb2 = [Buf("xblkA0"), Buf("xblkA1")]
    for kvh in range(4):
        P.barrier()
        KT = C.bf(36 * KB, S); BKT = [Buf("KT%d" % b) for b in range(NBLK)]
        VV = C.bf(44 * KB, S); BVV = [Buf("VV%d" % t) for t in range(NTILE)]
        qT4 = C.bf(52 * KB, NSLOT * 512); BqT = [Buf("qT%d" % h) for h in range(2)]
        qT4v = qT4.rearrange("p (u g t) -> p u g t", u=NSLOT, g=4)
        PT = [C.bf(60 * KB + i * KB, 512) for i in range(4)]; BPT = [Buf("PT%d" % i) for i in range(4)]
        t1 = C.f32(64 * KB, 512); t2 = C.f32(66 * KB, 512); Bt = [Buf("t1"), Buf("t2")]
        rin = t1; Brin = Bt[0]
        tabs = [[C.f32(68 * KB + s_ * 4 * KB + i * 2 * KB, 512) for i in range(2)] for s_ in range(2)]
        Btabs = [Buf("tabA"), Buf("tabB")]
        wset = [[C.bf(76 * KB + s_ * 8 * KB + i * 4 * KB, 2048) for i in range(2)] for s_ in range(2)]
        Bwset = [[Buf("ws%d_%d" % (s_, i)) for i in range(2)] for s_ in range(2)]
        wv = C.bf(92 * KB, 2048); Bwv = Buf("wv")
        wk, Bwk = wset[1], Bwset[1]
        for i in range(2):
            stg.load_w(dr["d_wk"][kvh, i], lambda c0, n, i=i, wk=wk: wk[i][:, c0:c0 + n], Bwk[i], 2048)
        stg.load_w(dr["d_wv"][kvh], lambda c0, n, wv=wv: wv[:, c0:c0 + n], Bwv, 2048)
        nt = 0
        for blk in range(NBLK):
            xk = load_xblk(C, dr["xTb"], Bx, blk * 512, 512, XB2[blk % 2], Bxb2[blk % 2])
            tab, Btab = tabs[nt % 2], Btabs[nt % 2]
            nt += 1
            for i in range(2):
                P.dma("sp", lambda e, i=i, blk=blk, tb=tab[i]: e.dma_start(out=tb, in_=dr["rt128"][i][:, blk * 512:(blk + 1) * 512]), writes=[Btab])
            proj_fm(C, p0[:, :], b0, wk[0], Bwk[0], lambda k, xk=xk: xk(k), Bxb2[blk % 2])
            proj_fm(C, p1[:, :], b1, wk[1], Bwk[1], lambda k, xk=xk: xk(k), Bxb2[blk % 2])
            rope_evac(C, p0[:, :], b0, p1[:, :], b1, tab[0], tab[1], Btab, KT[:, blk * 512:(blk + 1) * 512], BKT[blk], t1, t2, Bt)
            for tt in range(4):
                t = blk * 4 + tt
                pv, bv = (p2, b2) if tt % 2 == 0 else (C.ps[3], C.Bps[3])
                proj_tm(C, pv[:, 0:128], bv, wv, Bwv, lambda k, xk=xk, tt=tt: xk(k, tt * 128, tt * 128 + 128), Bxb2[blk % 2], 128)
                P.op("act", lambda e, t=t, VV=VV, pv=pv: e.activation(out=VV[:, t * 128:(t + 1) * 128], in_=pv[:, 0:128], func=AF.Copy), reads=[bv], writes=[BVV[t]])
        ng = 0
        for half in range(2):
            xk = load_xblk(C, dr["xTob"], Bxo, half * 512, 512, XB2[half], Bxb2[half])
            tab, Btab = tabs[nt % 2], Btabs[nt % 2]
            nt += 1
            for i in range(2):
                P.dma("sp", lambda e, i=i, half=half, tb=tab[i]: e.dma_start(out=tb, in_=dr["rt128o"][i][:, half * 512:(half + 1) * 512]), writes=[Btab])
            for g in range(4):
                hq = kvh * 4 + g
                wq, Bwq = wset[ng % 2], Bwset[ng % 2]
                ng += 1
                for i in range(2):
                    stg.load_w(dr["d_wq"][hq, i], lambda c0, n, i=i, wq=wq: wq[i][:, c0:c0 + n], Bwq[i], 2048)
                proj_fm(C, p0[:, :], b0, wq[0], Bwq[0], lambda k, xk=xk: xk(k), Bxb2[half])
                proj_fm(C, p1[:, :], b1, wq[1], Bwq[1], lambda k, xk=xk: xk(k), Bxb2[half])
                v4 = lambda ap: ap.rearrange("p (u t) -> p u t", u=4)
                rope_evac(C, p0[:, :], b0, p1[:, :], b1, tab[0], tab[1], Btab, qT4v[:, half * 4:(half + 1) * 4, g, :], BqT[half], t1, t2, Bt, view=v4)
        items = [(u, j) for u in range(NSLOT) for j in range(4 * (u + 1))]

        def emit_S(idx):
            u, j = items[idx]
            p_s, b_s = C.ps[idx % 4], C.Bps[idx % 4]
            qs = qT4[:, u * 512:(u + 1) * 512]
            P.op("pe", lambda e, p_s=p_s, j=j, qs=qs, KT=KT: e.matmul(p_s[:, :], lhsT=KT[:, j * 128:(j + 1) * 128], rhs=qs, start=True, stop=True),
                 reads=[BKT[j // 4], BqT[u // 4]], writes=[b_s])
        LOOK = 2
        for idx in range(min(LOOK, len(items))):
            emit_S(idx)
        for idx, (u, j) in enumerate(items):
            J = 4 * (u + 1)
            p_o, b_o = C.ps[4 + u % 2], C.Bps[4 + u % 2]
            p_r, b_r = C.ps[6 + u % 2], C.Bps[6 + u % 2]
            p_s, b_s = C.ps[idx % 4], C.Bps[idx % 4]
            pt, bpt = PT[idx % 4], BPT[idx % 4]
            P.op("act", lambda e, p_s=p_s, pt=pt: e.activation(out=pt, in_=p_s[:, :], func=AF.Exp, scale=128.0 ** -0.5), reads=[b_s], writes=[bpt])
            mk = maskT[u][:, j * 128:(j + 1) * 128]
            eng = "pool" if idx % 3 == 2 else "dve"
            P.op(eng, lambda e, pt=pt, mk=mk: e.tensor_tensor(out=pt.rearrange("p (g t) -> p g t", g=4), in0=pt.rearrange("p (g t) -> p g t", g=4),
                                                         in1=mk.unsqueeze(1).to_broadcast([128, 4, 128]), op=ALU.mult),
                 reads=[bpt, BmaskT[u]], writes=[bpt])
            if idx + LOOK < len(items):
                emit_S(idx + LOOK)
            P.op("pe", lambda e, p_o=p_o, j=j, pt=pt, J=J, VV=VV: e.matmul(p_o[:, :], lhsT=VV[:, j * 128:(j + 1) * 128], rhs=pt, start=(j == 0), stop=(j == J - 1)),
                 reads=[BVV[j], bpt], writes=[b_o])
            P.op("pe", lambda e, p_r=p_r, j=j, pt=pt, J=J: e.matmul(p_r[:, :], lhsT=onesbf, rhs=pt, start=(j == 0), stop=(j == J - 1)),
                 reads=[Bonesbf, bpt], writes=[b_r])
            if j == J - 1:
                P.op("dve", lambda e, p_r=p_r, rin=rin: e.reciprocal(out=rin, in_=p_r[:, :]), reads=[b_r], writes=[Brin])
                otv = C.bf(OFF_OT, NCH * NT).rearrange("p (c t) -> p c t", c=NCH)[:, kvh * 4:(kvh + 1) * 4, u * 128:(u + 1) * 128]
                P.op("dve", lambda e, p_o=p_o, otv=otv, rin=rin: e.tensor_tensor(out=otv, in0=p_o[:, :].rearrange("p (g t) -> p g t", g=4),
                                                                               in1=rin.rearrange("p (g t) -> p g t", g=4), op=ALU.mult),
                     reads=[b_o, Brin], writes=[Bot[kvh * 4 + g][u // 4] for g in range(4)])
    P.barrier()
    return Bot


def rope_tables(pos, dh, npieces_heads=1):
    half = dh // 2
    inv = (10000.0 ** (-(np.arange(half, dtype=np.float32)) / np.float32(half))).astype(np.float32)
    ang = pos.astype(np.float32)[None, :] * inv[:, None]
    c = np.cos(ang).astype(np.float32)
    s = np.sin(ang).astype(np.float32)
    d = np.arange(128) % dh
    cosT = c[d % half]
    sinT = np.where((d < half)[:, None], -s[d % half], s[d % half])
    return cosT.astype(np.float32), sinT.astype(np.float32)


def perm_cols(n, dh):
    idx = np.arange(n)
    d = idx % dh
    half = dh // 2
    return np.where(d < half, idx + half, idx - half)


def host_M1_inputs(c_w_in, cq):
    W = c_w_in[0]
    oq, ok_, ov, oiq, oik, oiw = 0, 2048, 2560, 3072, 4096, 4160
    d = {}
    p128 = perm_cols(128, 128)
    p64 = perm_cols(128, 64)
    Wq = W[:, oq:oq + 2048]
    d["d_wq"] = np.stack([np.stack([fm_layout(Wq[:, h * 128:(h + 1) * 128]), fm_layout(Wq[:, h * 128:(h + 1) * 128][:, p128])]) for h in range(16)])
    Wk = W[:, ok_:ok_ + 512]
    d["d_wk"] = np.stack([np.stack([fm_layout(Wk[:, h * 128:(h + 1) * 128]), fm_layout(Wk[:, h * 128:(h + 1) * 128][:, p128])]) for h in range(4)])
    d["d_wv"] = np.stack([fm_layout(W[:, ov + h * 128: ov + (h + 1) * 128]) for h in range(4)])
    Wiq = W[:, oiq:oiq + 1024]
    d["d_wiq"] = np.stack([np.stack([fm_layout(Wiq[:, pc * 128:(pc + 1) * 128]), fm_layout(Wiq[:, pc * 128:(pc + 1) * 128][:, p64])]) for pc in range(8)])
    Wik2 = np.concatenate([W[:, oik:oik + 64], W[:, oik:oik + 64]], 1)
    d["d_wik"] = np.stack([fm_layout(Wik2), fm_layout(Wik2[:, p64])])
    d["d_wiw"] = fm_layout(W[:, oiw:oiw + 16])
    pos = np.arange(S)
    own = own_tiles(cq)
    opos = np.concatenate([np.arange(i * 128, (i + 1) * 128) for i in own])
    c128, s128 = rope_tables(pos, 128)
    c64, s64 = rope_tables(pos, 64)
    d["rt128"] = np.stack([c128, s128])
    d["rt128o"] = np.stack([c128[:, opos], s128[:, opos]])
    d["rt64o"] = np.stack([c64[:, opos], s64[:, opos]])
    mA = (np.arange(128) < 64)[:, None].astype(np.float32)
    d["rtik"] = np.stack([c64 * mA, s64 * mA, c64 * (1 - mA), s64 * (1 - mA)])
    return {k: np.ascontiguousarray(v, dtype=np.float32) for k, v in d.items()}


M1_DRAM = [("d_wq", [16, 2, 128, 2048]), ("d_wk", [4, 2, 128, 2048]), ("d_wv", [4, 128, 2048]), ("d_wiq", [8, 2, 128, 2048]),
           ("d_wik", [2, 128, 2048]), ("d_wiw", [128, 256]), ("rt128", [2, 128, S]), ("rt128o", [2, 128, NT]), ("rt64o", [2, 128, NT]),
           ("rtik", [4, 128, S])]
M1_DRAM_BF = [("negm", [128, 4096]), ("onesbf", [128, 128]), ("identbf", [128, 128])]


_PROG_CACHE = {}
R_NAMES = ("woutp", "moew", "wr", "rb", "lnp")


def prep_layer1(C, dr):
    P = C.P
    P.new_epoch()
    stg = Stager(C, "p1")
    oh = C.f32(OFF_MISC, 4)
    Boh = Buf("oh")
    P.dma("sp", lambda e: e.dma_start(out=oh, in_=dr["onehot"]), writes=[Boh])
    tmp = [C.bf(OFF_XB + i * 2 * KB, NT) for i in range(4)]
    Bt = [Buf("p1t%d" % i) for i in range(4)]
    Bx1, Bxo1 = Buf("xTb1"), Buf("xTob1")
    Bz = [Buf("p1z%d" % k) for k in range(NCH)]
    z = lambda k: C.f32(OFF_ACC + k * NT * 4, NT)
    n = 0
    for vq in range(4):
        for k in range(NCH):
            si = stg.n % NSTAGE
            stg.n += 1
            st, bst = stg.stage[si][:, 0:NT], stg.B[si]
            P.dma("sp", lambda e, st=st, vq=vq, k=k: e.dma_start(out=st, in_=dr["x1sh"][vq][k * 128:(k + 1) * 128, :]), writes=[bst])
            t, bt = tmp[n % 4], Bt[n % 4]
            n += 1
            P.op("act", lambda e, t=t, st=st: e.activation(out=t, in_=st, func=AF.Copy), reads=[bst], writes=[bt])
            dstv = dr["xTb1"][k * 128:(k + 1) * 128, :].rearrange("p (u2 par d t) -> p u2 par d t", u2=4, par=2, d=4)
            tv = t.rearrange("p (u2 par t) -> p u2 par t", u2=4, par=2)
            for par in range(2):
                d_ = vq if par == 0 else 3 - vq
                P.dma("pool", lambda e, dstv=dstv, tv=tv, par=par, d_=d_: e.dma_start(out=dstv[:, :, par, d_, :], in_=tv[:, :, par, :]),
                      reads=[bt], writes=[Bx1], sembuf=bt)
            if vq == 0:
                P.op("dve", lambda e, k=k, st=st: e.tensor_scalar(out=z(k), in0=st, scalar1=oh[:, 0:1], scalar2=None, op0=ALU.mult),
                     reads=[bst, Boh], writes=[Bz[k]])
            else:
                P.op("dve", lambda e, k=k, st=st, vq=vq: e.scalar_tensor_tensor(out=z(k), in0=st, scalar=oh[:, vq:vq + 1], in1=z(k), op0=ALU.mult, op1=ALU.add),
                     reads=[bst, Boh, Bz[k]], writes=[Bz[k]])
    for k in range(NCH):
        P.dma("sp", lambda e, k=k: e.dma_start(out=dr["x1own"][k * 128:(k + 1) * 128, :], in_=z(k)), reads=[Bz[k]], sembuf=C.Bout)
        t, bt = tmp[n % 4], Bt[n % 4]
        n += 1
        P.op("act", lambda e, t=t, k=k: e.activation(out=t, in_=z(k), func=AF.Copy), reads=[Bz[k]], writes=[bt])
        P.dma("pool", lambda e, t=t, k=k: e.dma_start(out=dr["xTob"][k * 128:(k + 1) * 128, :], in_=t), reads=[bt], writes=[Bxo1], sembuf=bt)
    P.barrier()
    return Bx1, Bxo1


M0_TABLES = ("selt", "pent", "dmask")


def build_fused_program():
    if "fused" in _PROG_CACHE:
        return _PROG_CACHE["fused"]
    nc = bass.Bass("TRN2", target_bir_lowering=False)
    dr = {}
    f32in = lambda nm, shp: dr.__setitem__(nm, nc.dram_tensor(nm, shp, F32, kind="ExternalInput").ap())
    bfin = lambda nm, shp: dr.__setitem__(nm, nc.dram_tensor(nm, shp, BF16, kind="ExternalInput").ap())
    f32in("xTfull", [D, S])
    f32in("xTsh", [4, D, NT])
    f32in("ident", [128, 128])
    f32in("onehot", [128, 4])
    for L in range(2):
        f32in("woutp%d" % L, [NCH, 128, 2048])
        f32in("moew%d" % L, [NEXP, 12, 128, 2048])
        f32in("wr%d" % L, [128, NCH * 36])
        f32in("rb%d" % L, [1, 36])
        f32in("lnp%d" % L, [128, 4 * NCH])
    for nm, shp in M0_DRAM:
        if nm in M0_TABLES:
            f32in(nm + "_v", [4] + shp)
        else:
            f32in(nm, shp)
    for nm, shp in M0_DRAM_BF:
        if nm in M0_TABLES:
            bfin(nm + "_v", [4] + shp)
        else:
            bfin(nm, shp)
    for nm, shp in M1_DRAM:
        f32in(nm, shp)
    for nm, shp in M1_DRAM_BF:
        if nm not in dr:
            bfin(nm, shp)
    scr = lambda nm, shp, dt_: dr.__setitem__(nm, nc.dram_tensor(nm, shp, dt_, kind="ExternalOutput").ap())
    scr("xTb", [D, S], BF16)
    scr("xTob", [D, NT], BF16)
    scr("x1sh", [4, D, NT], F32)
    scr("xTb1", [D, S], BF16)
    scr("x1own", [D, NT], F32)
    scr("snap", [4 * NTILE, 128, 256], F32)
    scr("fk_s", [4, 128, 2 * S], BF16)
    scr("fv_s", [4, 128, NTILE * 256], BF16)
    scr("fc_s", [4, 128, 128], F32)
    scr("outT", [D, NT], F32)
    dr["cwd"] = nc.dram_tensor("cwd", [NEXP, NT], F32, kind="Internal").ap()
    with contextlib.ExitStack() as st:
        C = Ctx(nc, st)
        setup_consts(C, dr)
        P = C.P
        Bx = None
        for vq in range(4):
            P.new_epoch()
            drv = dict(dr)
            drv["xT"] = dr["xTsh"][vq]
            for nm in M0_TABLES:
                drv[nm] = dr[nm + "_v"][vq]
            for nm in R_NAMES:
                drv[nm] = dr[nm + "0"]
            drv["outT"] = dr["x1sh"][vq]
            Bot = phase_M0(C, drv, Bx_prev=Bx, vq=vq)
            Bx = C.last_Bx
            P.new_epoch()
            load_lnp(C, drv)
            Bz = load_z(C, drv)
            phase_R(C, drv, Bz, Bot)
        Bx1, Bxo1 = prep_layer1(C, dr)
        P.new_epoch()
        drv = dict(dr)
        drv["xTb"] = dr["xTb1"]
        drv["xT"] = dr["x1own"]
        for nm in R_NAMES:
            drv[nm] = dr[nm + "1"]
        Bot = phase_M1(C, drv, pre=(Bx1, Bxo1))
        P.new_epoch()
        load_lnp(C, drv)
        Bz = load_z(C, drv)
        phase_R(C, drv, Bz, Bot)
        P.emit()
    _PROG_CACHE["fused"] = nc
    return nc


def core_tokens(cq):
    return np.concatenate([np.arange(i * 128, (i + 1) * 128) for i in own_tiles(cq)])


def kernel(x, a_w_in, a_gla_gate_w2, a_gla_gate_b, a_gla_norm_g, a_fox_gate_b, a_w_out,
           c_w_in, c_w_out, ln_mix_g, ln_mix_b, ln_ffn_g, ln_ffn_b,
           moe_group_w, moe_group_b, moe_expert_w, moe_expert_b, moe_w_gate, moe_w_up, moe_w_down):
    A = lambda v: np.asarray(v)
    x = np.asarray(x, dtype=np.float32)
    nc = build_fused_program()
    shared = {}
    for L in range(2):
        r = host_R_inputs(L, A(a_w_out[0] if L == 0 else c_w_out[0]), A(ln_mix_g), A(ln_mix_b), A(ln_ffn_g), A(ln_ffn_b), A(moe_group_w),
                          A(moe_group_b), A(moe_expert_w), A(moe_expert_b), A(moe_w_gate), A(moe_w_up), A(moe_w_down))
        for nm in R_NAMES:
            shared[nm + str(L)] = r[nm]
        shared["ident"] = r["ident"]
    shared.update(host_M0_inputs(A(a_w_in), A(a_gla_gate_w2), A(a_gla_gate_b), A(a_gla_norm_g), A(a_fox_gate_b)))
    Tv = [host_mix_tables(vq) for vq in range(4)]
    for nm, _ in M0_DRAM[11:] + M0_DRAM_BF:
        if nm in M0_TABLES:
            shared[nm + "_v"] = np.stack([Tv[vq][nm] for vq in range(4)])
        else:
            shared[nm] = Tv[0][nm]
    xTb = [np.ascontiguousarray(x[b].T) for b in range(2)]
    xTsh = [np.stack([np.ascontiguousarray(xTb[b][:, core_tokens(vq)]) for vq in range(4)]) for b in range(2)]
    in_maps = []
    for core in range(8):
        b, cq = core // 4, core % 4
        m = dict(shared)
        m.update(host_M1_inputs(A(c_w_in), cq))
        for nm, _ in M1_DRAM_BF:
            if nm not in m:
                m[nm] = Tv[cq][nm]
        oh = np.zeros((128, 4), np.float32)
        oh[:, cq] = 1.0
        m["onehot"] = oh
        m["xTfull"] = xTb[b]
        m["xTsh"] = xTsh[b]
        in_maps.append(m)
    res = run_bass_kernel_spmd(nc, in_maps, core_ids=list(range(8)))
    out = np.empty_like(x)
    for core in range(8):
        b, cq = core // 4, core % 4
        out[b][core_tokens(cq)] = np.asarray(res.results[core]["outT"]).T
    return out
```

```python
import contextlib
import numpy as np
import concourse.bass as bass
import concourse.mybir as mybir
from concourse.bass_utils import run_bass_kernel_spmd

F32 = mybir.dt.float32
BF16 = mybir.dt.bfloat16
AF = mybir.ActivationFunctionType
ALU = mybir.AluOpType
AX = mybir.AxisListType

D = 2048
NCH = 16
NT = 1024
S = 4096
ALPHA = 4.0 ** 0.25
EPS = 1e-5
NEXP = 32
FF = 512
BIG = 1.0e30


class Buf:
    __slots__ = ("name", "lw", "rd", "dsem", "persist", "depoch")

    def __init__(self, name="", persist=False):
        self.name = name
        self.lw = None
        self.rd = []
        self.dsem = None
        self.persist = persist
        self.depoch = -1


class Prog:
    ENG = ("pe", "act", "dve", "pool", "sp")

    def __init__(self, nc, safe_same_engine=True):
        self.nc = nc
        self.safe = safe_same_engine
        self.ins = []
        self.perq = {e: [] for e in self.ENG}
        self.ndsem = 0
        self.pending = {e: set() for e in self.ENG}
        self.last_dma = {}
        self.epoch = 0
        self.NPERSIST = 6
        self.npersist = 0
        self.next_edsem = self.NPERSIST

    def _add(self, eng, fn, reads, writes, kind, dsem=None):
        iid = len(self.ins)
        deps = set(self.pending[eng])
        self.pending[eng] = set()
        for b in reads:
            if b.lw is not None:
                deps.add(b.lw)
        for b in writes:
            if b.lw is not None:
                deps.add(b.lw)
            last = {}
            for r in b.rd:
                rr = self.ins[r]
                if rr["kind"] == "dma":
                    deps.add(r)
                else:
                    last[rr["eng"]] = max(last.get(rr["eng"], -1), r)
            deps.update(last.values())
        self.ins.append(dict(eng=eng, fn=fn, deps=deps, kind=kind, dsem=dsem))
        self.perq[eng].append(iid)
        for b in reads:
            b.rd.append(iid)
        for b in writes:
            b.lw = iid
            b.rd = []
        if kind == "dma":
            self.last_dma[dsem] = iid
        return iid

    def op(self, eng, fn, reads=(), writes=()):
        return self._add(eng, fn, list(reads), list(writes), "op")

    def dma(self, q, fn, reads=(), writes=(), sembuf=None):
        if sembuf is None:
            sembuf = (list(writes) + list(reads))[0]
        if sembuf.persist:
            if sembuf.dsem is None:
                assert self.npersist < self.NPERSIST
                sembuf.dsem = self.npersist
                self.npersist += 1
        elif sembuf.dsem is None or sembuf.depoch != self.epoch:
            sembuf.dsem = self.next_edsem
            sembuf.depoch = self.epoch
            self.next_edsem += 1
        self.ndsem = max(self.ndsem, sembuf.dsem + 1)
        return self._add(q, fn, list(reads), list(writes), "dma", dsem=sembuf.dsem)

    def new_epoch(self):
        self.barrier()
        self.epoch += 1
        self.next_edsem = self.NPERSIST

    def barrier(self):
        dset = set()
        for e in self.ENG:
            if self.perq[e]:
                dset.add(self.perq[e][-1])
        dset.update(self.last_dma.values())
        for e in self.ENG:
            self.pending[e] |= dset

    def emit(self, final_wait_eng="sp"):
        nc = self.nc
        ins = self.ins
        marked = set()
        for it in ins:
            for d in it["deps"]:
                dd = ins[d]
                if dd["kind"] == "dma":
                    continue
                if dd["eng"] == it["eng"]:
                    if not self.safe:
                        continue
                    if dd["eng"] == "pe" and it["kind"] == "op":
                        continue
                marked.add(d)
        rank = {}
        for e in self.ENG:
            c = 0
            for iid in self.perq[e]:
                if ins[iid]["kind"] == "op" and iid in marked:
                    c += 1
                    rank[iid] = c
        dval = {}
        dcount = [0] * self.ndsem
        for i, it in enumerate(ins):
            if it["kind"] == "dma":
                dcount[it["dsem"]] += 16
                dval[i] = dcount[it["dsem"]]
        with contextlib.ExitStack() as st:
            esem = {e: st.enter_context(nc.semaphore("s_" + e)) for e in self.ENG}
            dsems = [st.enter_context(nc.semaphore("d%d" % k)) for k in range(self.ndsem)]
            block = st.enter_context(nc.Block())

            def make(e):
                def body(engobj):
                    seen = {}
                    for iid in self.perq[e]:
                        it = ins[iid]
                        need = {}
                        for d in it["deps"]:
                            dd = ins[d]
                            if dd["kind"] == "dma":
                                key = ("d", dd["dsem"])
                                val = dval[d]
                            else:
                                if d not in rank:
                                    continue
                                key = ("e", dd["eng"])
                                val = rank[d]
                            if need.get(key, 0) < val:
                                need[key] = val
                        for key, val in need.items():
                            if seen.get(key, 0) >= val:
                                continue
                            seen[key] = val
                            sem = dsems[key[1]] if key[0] == "d" else esem[key[1]]
                            engobj.wait_ge(sem, val)
                        r = it["fn"](engobj)
                        if it["kind"] == "dma":
                            r.then_inc(dsems[it["dsem"]], 16)
                        elif iid in rank:
                            r.then_inc(esem[e], 1)
                    if e == final_wait_eng:
                        for k in range(self.ndsem):
                            if dcount[k] and seen.get(("d", k), 0) < dcount[k]:
                                engobj.wait_ge(dsems[k], dcount[k])
                return body

            block.tensor(make("pe"))
            block.scalar(make("act"))
            block.vector(make("dve"))
            block.gpsimd(make("pool"))
            block.sync(make("sp"))


class Ctx:
    def __init__(self, nc, st):
        self.nc = nc
        import os as _os
        self.P = Prog(nc, safe_same_engine=not _os.environ.get("UNSAFE"))
        self.arena_words = 206 * 1024 // 4
        self.arena = st.enter_context(nc.sbuf_tensor("arena", [128, self.arena_words], F32))
        self.ps = [st.enter_context(nc.psum_tensor("ps%d" % i, [128, 512], F32)) for i in range(8)]
        self.Bps = [Buf("ps%d" % i) for i in range(8)]

    def f32(self, off, n, parts=128):
        assert off % 4 == 0 and off // 4 + n <= self.arena_words, (off, n)
        return self.arena[0:parts, off // 4: off // 4 + n]

    def bf(self, off, n, parts=128):
        assert off % 4 == 0 and n % 2 == 0 and off // 4 + n // 2 <= self.arena_words, (off, n)
        return self.arena[0:parts, off // 4: off // 4 + n // 2].bitcast(BF16)


KB = 1024
OFF_ACC = 0
OFF_XB = 64 * KB
OFF_OT = 96 * KB
OFF_RING = 128 * KB
OFF_STAGE = 160 * KB
OFF_MISC = 184 * KB
NRING = 8
NSTAGE = 3


class WStream:
    def __init__(self, C):
        self.C = C
        self.stage = [C.f32(OFF_STAGE + i * 8 * KB, 2048) for i in range(NSTAGE)]
        self.Bst = [Buf("st%d" % i) for i in range(NSTAGE)]
        self.ring = [C.bf(OFF_RING + i * 4 * KB, 2048) for i in range(NRING)]
        self.Brg = [Buf("rg%d" % i) for i in range(NRING)]
        self.n = 0

    def push(self, src_ap):
        P = self.C.P
        i = self.n
        self.n += 1
        s = i % NSTAGE
        r = i % NRING
        st, bst, rg, brg = self.stage[s], self.Bst[s], self.ring[r], self.Brg[r]
        q = "sp"
        P.dma(q, lambda e: e.dma_start(out=st, in_=src_ap), writes=[bst])
        ce = "act"
        if ce == "pool":
            P.op("pool", lambda e: e.tensor_copy(out=rg, in_=st), reads=[bst], writes=[brg])
        else:
            P.op("act", lambda e: e.activation(out=rg, in_=st, func=AF.Copy), reads=[bst], writes=[brg])
        return rg, brg


def layer_norm_fm(C, zoff, Bz, gcol, bcol, outs, tmp_off):
    P = C.P
    nc = C.nc
    ones = C.ones32
    Bones = C.Bones
    z = lambda m, h: C.f32(zoff + (m * NT + h * 512) * 4, 512)
    sq = [C.f32(tmp_off + i * 2 * KB, 512) for i in range(2)]
    Bsq = [Buf("sq0"), Buf("sq1")]
    mean = C.f32(tmp_off + 4 * KB, 512)
    rstd = C.f32(tmp_off + 6 * KB, 512)
    var = C.f32(tmp_off + 8 * KB, 512)
    Bmean, Brstd, Bvar = Buf("mean"), Buf("rstd"), Buf("var")
    for h in range(2):
        pa, pb = C.ps[6], C.ps[7]
        Bpa, Bpb = C.Bps[6], C.Bps[7]
        for m in range(NCH):
            zz = z(m, h)
            P.op("pe", lambda e, zz=zz, m=m: e.matmul(pa[:], lhsT=ones, rhs=zz, start=(m == 0), stop=(m == NCH - 1)),
                 reads=[Bones, Bz[m][h]], writes=[Bpa])
        for m in range(NCH):
            zz = z(m, h)
            s_, bs_ = sq[m % 2], Bsq[m % 2]
            P.op("act", lambda e, zz=zz, s_=s_: e.activation(out=s_, in_=zz, func=AF.Square), reads=[Bz[m][h]], writes=[bs_])
            P.op("pe", lambda e, s_=s_, m=m: e.matmul(pb[:], lhsT=ones, rhs=s_, start=(m == 0), stop=(m == NCH - 1)),
                 reads=[Bones, bs_], writes=[Bpb])
        P.op("dve", lambda e: e.tensor_scalar(out=mean, in0=pa[:], scalar1=1.0 / D, scalar2=None, op0=ALU.mult),
             reads=[Bpa], writes=[Bmean])
        P.op("dve", lambda e: e.tensor_tensor(out=var, in0=mean, in1=mean, op=ALU.mult), reads=[Bmean], writes=[Bvar])
        P.op("dve", lambda e: e.scalar_tensor_tensor(out=var, in0=pb[:], scalar=1.0 / D, in1=var, op0=ALU.mult, op1=ALU.subtract),
             reads=[Bpb, Bvar], writes=[Bvar])
        P.op("dve", lambda e: e.tensor_scalar(out=var, in0=var, scalar1=EPS, scalar2=None, op0=ALU.add), reads=[Bvar], writes=[Bvar])
        P.op("act", lambda e: e.activation(out=var, in_=var, func=AF.Sqrt), reads=[Bvar], writes=[Bvar])
        P.op("dve", lambda e: e.reciprocal(out=rstd, in_=var), reads=[Bvar], writes=[Brstd])
        for m in range(NCH):
            zz = z(m, h)
            P.op("pool", lambda e, zz=zz: e.tensor_tensor(out=zz, in0=zz, in1=mean, op=ALU.subtract),
                 reads=[Bz[m][h], Bmean], writes=[Bz[m][h]])
            P.op("dve", lambda e, zz=zz: e.tensor_tensor(out=zz, in0=zz, in1=rstd, op=ALU.mult),
                 reads=[Bz[m][h], Brstd], writes=[Bz[m][h]])
            for o in outs:
                if o[0] == "bf16":
                    _, off, Bo, sc, bi = o
                    dst = C.bf(off + (m * NT + h * 512) * 2, 512)
                    P.op("act", lambda e, zz=zz, dst=dst, m=m, sc=sc, bi=bi: e.activation(out=dst, in_=zz, func=AF.Identity, scale=sc(m), bias=bi(m)),
                         reads=[Bz[m][h]], writes=[Bo[m][h]])
            for o in outs:
                if o[0] == "f32":
                    _, sc, bi = o
                    P.op("act", lambda e, zz=zz, m=m, sc=sc, bi=bi: e.activation(out=zz, in_=zz, func=AF.Identity, scale=sc(m), bias=bi(m)),
                         reads=[Bz[m][h]], writes=[Bz[m][h]])


def phase_R(C, dr, Bz, Bot):
    P = C.P
    nc = C.nc
    ws = WStream(C)
    z = lambda m, h: C.f32(OFF_ACC + (m * NT + h * 512) * 4, 512)
    ot = lambda k, h: C.bf(OFF_OT + (k * NT + h * 512) * 2, 512)
    xb = lambda k, h: C.bf(OFF_XB + (k * NT + h * 512) * 2, 512)
    Bxb = [[Buf("xb%d_%d" % (m, h)) for h in range(2)] for m in range(NCH)]
    lnp = C.lnp
    col = lambda w: (lambda m: lnp[:, w * NCH + m: w * NCH + m + 1])

    for m in range(NCH):
        wr, bwr = ws.push(dr["woutp"][m])
        for h in range(2):
            pt, bpt = C.ps[(m * 2 + h) % 4], C.Bps[(m * 2 + h) % 4]
            for k in range(NCH):
                P.op("pe", lambda e, pt=pt, wr=wr, k=k, h=h: e.matmul(pt[:], lhsT=wr[:, k * 128:(k + 1) * 128], rhs=ot(k, h),
                                                                  start=(k == 0), stop=(k == NCH - 1)),
                     reads=[bwr, Bot[k][h]], writes=[bpt])
            P.op("dve", lambda e, pt=pt, m=m, h=h: e.scalar_tensor_tensor(out=z(m, h), in0=z(m, h), scalar=ALPHA, in1=pt[:],
                                                                        op0=ALU.mult, op1=ALU.add),
                 reads=[bpt, Bz[m][h]], writes=[Bz[m][h]])
    if getattr(C, 'stop', None) == 'R1':
        return store_z(C, dr, Bz)
    layer_norm_fm(C, OFF_ACC, Bz, None, None,
                  [("bf16", OFF_XB, Bxb, col(0), col(1)), ("f32", col(4), col(5))], OFF_MISC + 8 * KB)

    if getattr(C, 'stop', None) == 'R2':
        return store_z(C, dr, Bz)
    MO = OFF_OT
    P.barrier()
    wrt = C.f32(MO, NCH * 36)
    Bwrt = Buf("wrt")
    rbb = C.f32(MO + 4 * KB, 36)
    Brbb = Buf("rbb")
    P.dma("sp", lambda e: e.dma_start(out=wrt, in_=dr["wr"]), writes=[Bwrt])
    P.dma("sp", lambda e: e.dma_start(out=rbb, in_=dr["rb"].broadcast_to([128, 36])), writes=[Brbb])
    lg = C.f32(MO + 5 * KB, 8 * 36)
    Blg = Buf("lg")
    comb = C.f32(MO + 7 * KB, 8 * 32)
    Bcomb = Buf("comb")
    sc = C.f32(MO + 9 * KB, 64)
    Bsc = Buf("sc")
    tmp32 = C.f32(MO + 10 * KB, 64)
    Btmp = Buf("tmp32")
    prt, bprt = C.ps[4], C.Bps[4]
    for tt in range(8):
        h, o = tt // 4, (tt % 4) * 128
        for m in range(NCH):
            P.op("pe", lambda e, tt=tt, m=m, h=h, o=o: e.matmul(prt[:, tt * 36:(tt + 1) * 36], lhsT=z(m, h)[:, o:o + 128],
                                                              rhs=wrt[:, m * 36:(m + 1) * 36], start=(m == 0), stop=(m == NCH - 1)),
                 reads=[Bz[m][h], Bwrt], writes=[bprt])
    for tt in range(8):
        l = lg[:, tt * 36:(tt + 1) * 36]
        P.op("dve", lambda e, tt=tt, l=l: e.scalar_tensor_tensor(out=l, in0=prt[:, tt * 36:(tt + 1) * 36], scalar=1.0 / ALPHA, in1=rbb,
                                                               op0=ALU.mult, op1=ALU.add), reads=[bprt, Brbb], writes=[Blg])
        gl = lg[:, tt * 36: tt * 36 + 4]
        el = lg[:, tt * 36 + 4: tt * 36 + 36]
        s = lambda i: sc[:, i:i + 1]
        cm = comb[:, tt * 32:(tt + 1) * 32]
        t32 = tmp32[:, 0:32]
        t4 = tmp32[:, 32:36]
        t4b = tmp32[:, 36:40]
        V = lambda fn, r=(Blg, Bsc, Btmp, Bcomb), w=(Blg, Bsc, Btmp, Bcomb): P.op("dve", fn, reads=list(r), writes=list(w))
        A = lambda fn: P.op("act", fn, reads=[Blg, Bsc, Btmp, Bcomb], writes=[Blg, Bsc, Btmp, Bcomb])
        V(lambda e, gl=gl: e.reduce_max(out=s(0), in_=gl, axis=AX.X))
        V(lambda e, gl=gl: e.tensor_scalar(out=t4, in0=gl, scalar1=s(0), scalar2=None, op0=ALU.subtract))
        A(lambda e: e.activation(out=t4b, in_=t4, func=AF.Exp))
        V(lambda e: e.reduce_sum(out=s(1), in_=t4b, axis=AX.X))
        V(lambda e: e.reciprocal(out=s(1), in_=s(1)))
        V(lambda e: e.tensor_scalar(out=t4, in0=t4, scalar1=0.0, scalar2=-BIG, op0=ALU.is_lt, op1=ALU.mult))
        for g in range(4):
            V(lambda e, g=g, el=el: e.tensor_scalar(out=el[:, g * 8:(g + 1) * 8], in0=el[:, g * 8:(g + 1) * 8],
                                                   scalar1=t4[:, g:g + 1], scalar2=None, op0=ALU.add))
        V(lambda e, el=el: e.reduce_max(out=s(2), in_=el, axis=AX.X))
        V(lambda e, el=el: e.tensor_scalar(out=t32, in0=el, scalar1=s(2), scalar2=None, op0=ALU.is_equal))
        V(lambda e, el=el: e.scalar_tensor_tensor(out=el, in0=t32, scalar=-BIG, in1=el, op0=ALU.mult, op1=ALU.add))
        V(lambda e, el=el: e.reduce_max(out=s(3), in_=el, axis=AX.X))
        V(lambda e: e.tensor_tensor(out=s(4), in0=s(3), in1=s(2), op=ALU.subtract))
        A(lambda e: e.activation(out=s(4), in_=s(4), func=AF.Exp))
        V(lambda e: e.tensor_scalar(out=s(4), in0=s(4), scalar1=1.0, scalar2=None, op0=ALU.add))
        V(lambda e: e.reciprocal(out=s(5), in_=s(4)))
        V(lambda e: e.tensor_scalar(out=s(6), in0=s(5), scalar1=-1.0, scalar2=1.0, op0=ALU.mult, op1=ALU.add))
        V(lambda e: e.tensor_tensor(out=s(5), in0=s(5), in1=s(1), op=ALU.mult))
        V(lambda e: e.tensor_tensor(out=s(6), in0=s(6), in1=s(1), op=ALU.mult))
        V(lambda e, cm=cm: e.tensor_scalar(out=cm, in0=t32, scalar1=s(5), scalar2=None, op0=ALU.mult))
        V(lambda e, el=el: e.tensor_scalar(out=t32, in0=el, scalar1=s(3), scalar2=None, op0=ALU.is_equal))
        V(lambda e, cm=cm: e.scalar_tensor_tensor(out=cm, in0=t32, scalar=s(6), in1=cm, op0=ALU.mult, op1=ALU.add))
    combT = C.f32(MO + 12 * KB, 1024)
    BcombT = Buf("combT")
    pct, bpct = C.ps[5], C.Bps[5]
    for tt in range(8):
        hh, o = tt // 4, (tt % 4) * 128
        P.op("pe", lambda e, tt=tt, o=o: e.transpose(pct[0:32, o:o + 128], comb[:, tt * 32:(tt + 1) * 32], C.ident32),
             reads=[Bcomb, C.Bident], writes=[bpct])
        if tt % 4 == 3:
            P.op("dve", lambda e, hh=hh: e.tensor_copy(out=combT[0:32, hh * 512:(hh + 1) * 512], in_=pct[0:32, :]),
                 reads=[bpct], writes=[BcombT])
    Bcwd = Buf("cwd")
    P.dma("sp", lambda e: e.dma_start(out=dr["cwd"], in_=combT[0:32, :]), reads=[BcombT], writes=[Bcwd])

    if getattr(C, 'stop', None) == 'R3':
        P.dma('sp', lambda e: e.dma_start(out=dr['outT'][0:32, :], in_=combT[0:32, :]), reads=[BcombT], sembuf=C.Bout)
        P.dma('sp', lambda e: e.dma_start(out=dr['outT'][128:256, 0:288], in_=lg), reads=[Blg], sembuf=C.Bout)
        P.dma('sp', lambda e: e.dma_start(out=dr['outT'][256:384, 0:64], in_=sc), reads=[Bsc], sembuf=C.Bout)
        P.dma('sp', lambda e: e.dma_start(out=dr['outT'][512:640, 0:36], in_=rbb), reads=[Brbb], sembuf=C.Bout)
        P.dma('sp', lambda e: e.dma_start(out=dr['outT'][640:768, 0:576], in_=wrt), reads=[Bwrt], sembuf=C.Bout)
        P.op('dve', lambda e: e.tensor_copy(out=tmp32[:, 0:36], in_=prt[:, 0:36]), reads=[bprt], writes=[Btmp])
        P.dma('sp', lambda e: e.dma_start(out=dr['outT'][768:896, 0:36], in_=tmp32[:, 0:36]), reads=[Btmp], sembuf=C.Bout)
        P.dma('sp', lambda e: e.dma_start(out=dr['outT'][384:512, 0:256], in_=comb), reads=[Bcomb], sembuf=C.Bout)
        return
    cwb = [C.f32(MO + 16 * KB + i * 4 * KB, 1024) for i in range(2)]
    Bcwb = [Buf("cwb0"), Buf("cwb1")]
    hT = [[C.bf(MO + (24 if i == 0 else 0) * KB + fc * 2 * KB, 1024) for fc in range(4)] for i in range(2)]
    BhT = [[Buf("h%d_%d" % (i, fc)) for fc in range(4)] for i in range(2)]
    sg = [C.f32(OFF_MISC + i * 2 * KB, 512) for i in range(2)]
    Bsg = [Buf("sg0"), Buf("sg1")]
    tu = [C.f32(OFF_MISC + 4 * KB + i * 2 * KB, 512) for i in range(2)]
    Btu = [Buf("tu0"), Buf("tu1")]
    dead = [Bwrt, Brbb, Blg, Bcomb, Bsc, Btmp, BcombT]
    st8 = dict(cnt=0, dcnt=0, first1=True)
    exl = list(C.expert_list if getattr(C, 'expert_list', None) is not None else range(NEXP))

    def load_cw(ei):
        ex = exl[ei]
        cw, bcw = cwb[ei % 2], Bcwb[ei % 2]
        P.dma("pool", lambda e, cw=cw, ex=ex: e.dma_start(out=cw, in_=dr["cwd"][ex:ex + 1, :].broadcast_to([128, 1024])),
              reads=[Bcwd], writes=[bcw])

    def gu_seg(ei, fc, wts):
        ex = exl[ei]
        cw, bcw = cwb[ei % 2], Bcwb[ei % 2]
        hset, Bhset = hT[ei % 2], BhT[ei % 2]
        (wg, bwg), (wu, bwu) = wts
        for h in range(2):
            cnt = st8["cnt"]
            pg, bpg = C.ps[(cnt % 2) * 2], C.Bps[(cnt % 2) * 2]
            pu, bpu = C.ps[(cnt % 2) * 2 + 1], C.Bps[(cnt % 2) * 2 + 1]
            sgi, bsgi = sg[cnt % 2], Bsg[cnt % 2]
            tui, btui = tu[cnt % 2], Btu[cnt % 2]
            st8["cnt"] = cnt + 1
            for k in range(NCH):
                P.op("pe", lambda e, pg=pg, wg=wg, k=k, h=h: e.matmul(pg[:], lhsT=wg[:, k * 128:(k + 1) * 128], rhs=xb(k, h),
                                                                  start=(k == 0), stop=(k == NCH - 1)),
                     reads=[bwg, Bxb[k][h]], writes=[bpg])
            for k in range(NCH):
                P.op("pe", lambda e, pu=pu, wu=wu, k=k, h=h: e.matmul(pu[:], lhsT=wu[:, k * 128:(k + 1) * 128], rhs=xb(k, h),
                                                                  start=(k == 0), stop=(k == NCH - 1)),
                     reads=[bwu, Bxb[k][h]], writes=[bpu])
            P.op("act", lambda e, pg=pg, sgi=sgi: e.activation(out=sgi, in_=pg[:], func=AF.Silu), reads=[bpg], writes=[bsgi])
            P.op("dve", lambda e, pu=pu, tui=tui, cw=cw, h=h: e.tensor_tensor(out=tui, in0=pu[:], in1=cw[:, h * 512:(h + 1) * 512], op=ALU.mult),
                 reads=[bpu, bcw], writes=[btui])
            hh = hset[fc][:, h * 512:(h + 1) * 512]
            extra = []
            if ei % 2 == 1 and st8["first1"]:
                extra = dead
            P.op("dve", lambda e, hh=hh, sgi=sgi, tui=tui: e.tensor_tensor(out=hh, in0=sgi, in1=tui, op=ALU.mult),
                 reads=[bsgi, btui], writes=[Bhset[fc]] + extra)
        if ei % 2 == 1 and fc == 3:
            st8["first1"] = False

    def dn(ei, wd):
        ex = exl[ei]
        hset, Bhset = hT[ei % 2], BhT[ei % 2]
        for m in range(NCH):
            for h in range(2):
                dcnt = st8["dcnt"]
                pd, bpd = C.ps[4 + dcnt % 2], C.Bps[4 + dcnt % 2]
                st8["dcnt"] = dcnt + 1
                for fc in range(4):
                    P.op("pe", lambda e, pd=pd, fc=fc, m=m, h=h, w=wd[fc][0], hset=hset: e.matmul(pd[:], lhsT=w[:, m * 128:(m + 1) * 128],
                                                                                           rhs=hset[fc][:, h * 512:(h + 1) * 512],
                                                                                           start=(fc == 0), stop=(fc == 3)),
                         reads=[wd[fc][1], Bhset[fc]], writes=[bpd])
                P.op("dve", lambda e, pd=pd, m=m, h=h: e.tensor_tensor(out=z(m, h), in0=z(m, h), in1=pd[:], op=ALU.add),
                     reads=[bpd, Bz[m][h]], writes=[Bz[m][h]])

    segs = []
    if exl:
        segs += [("gu", 0, fc) for fc in range(4)]
    for ei in range(len(exl)):
        nxt = ei + 1 < len(exl)
        if nxt:
            segs.append(("gu", ei + 1, 0))
        segs.append(("dn", ei, None))
        if nxt:
            segs += [("gu", ei + 1, fc) for fc in range(1, 4)]
    pushed = {}

    def ensure(si):
        if si >= len(segs) or si in pushed:
            return
        kind, ei, fc = segs[si]
        ex = exl[ei]
        if kind == "gu":
            if fc == 0:
                load_cw(ei)
            pushed[si] = [ws.push(dr["moew"][ex, 2 * fc]), ws.push(dr["moew"][ex, 2 * fc + 1])]
        else:
            pushed[si] = [ws.push(dr["moew"][ex, 8 + f_]) for f_ in range(4)]

    for si, (kind, ei, fc) in enumerate(segs):
        ensure(si)
        ensure(si + 1)
        if kind == "gu":
            gu_seg(ei, fc, pushed[si])
        else:
            dn(ei, pushed[si])
        del pushed[si]
    if getattr(C, 'stop', None) == 'R4':
        return store_z(C, dr, Bz)
    layer_norm_fm(C, OFF_ACC, Bz, None, None, [("f32", col(2), col(3))], OFF_MISC + 8 * KB)
    store_z(C, dr, Bz)


def store_z(C, dr, Bz):
    for m in range(NCH):
        C.P.dma("sp", lambda e, m=m: e.dma_start(out=dr["outT"][m * 128:(m + 1) * 128, :], in_=C.f32(OFF_ACC + m * NT * 4, NT)),
                reads=[Bz[m][0], Bz[m][1]], sembuf=C.Bout)


def setup_consts(C, dr):
    P = C.P
    base = 202 * KB
    C.ones32 = C.f32(base, 128)
    C.ident32 = C.f32(base + 512, 128)
    C.lnp = C.f32(base + 1024, 6 * NCH)
    C.Bones, C.Bident, C.Blnp, C.Bout = Buf("ones"), Buf("ident", persist=True), Buf("lnp", persist=True), Buf("out", persist=True)
    P.op("dve", lambda e: e.memset(C.ones32, 1.0), writes=[C.Bones])
    P.dma("sp", lambda e: e.dma_start(out=C.ident32, in_=dr["ident"]), writes=[C.Bident])
    if "lnp" in dr:
        load_lnp(C, dr)


def load_lnp(C, dr):
    P = C.P
    P.dma("sp", lambda e: e.dma_start(out=C.lnp[:, 0:4 * NCH], in_=dr["lnp"]), writes=[C.Blnp])
    P.op("dve", lambda e: e.tensor_scalar(out=C.lnp[:, 4 * NCH:6 * NCH], in0=C.lnp[:, 0:2 * NCH], scalar1=ALPHA, scalar2=None, op0=ALU.mult),
         reads=[C.Blnp], writes=[C.Blnp])


def declare_R_dram(nc):
    dr = {}
    dr["xT"] = nc.dram_tensor("xT", [D, NT], F32, kind="ExternalInput").ap()
    dr["woutp"] = nc.dram_tensor("woutp", [NCH, 128, 2048], F32, kind="ExternalInput").ap()
    dr["moew"] = nc.dram_tensor("moew", [NEXP, 12, 128, 2048], F32, kind="ExternalInput").ap()
    dr["wr"] = nc.dram_tensor("wr", [128, NCH * 36], F32, kind="ExternalInput").ap()
    dr["rb"] = nc.dram_tensor("rb", [1, 36], F32, kind="ExternalInput").ap()
    dr["lnp"] = nc.dram_tensor("lnp", [128, 4 * NCH], F32, kind="ExternalInput").ap()
    dr["ident"] = nc.dram_tensor("ident", [128, 128], F32, kind="ExternalInput").ap()
    dr["cwd"] = nc.dram_tensor("cwd", [NEXP, NT], F32, kind="Internal").ap()
    dr["outT"] = nc.dram_tensor("outT", [D, NT], F32, kind="ExternalOutput").ap()
    return dr


def load_z(C, dr):
    Bz = [[Buf("z%d_%d" % (m, h)) for h in range(2)] for m in range(NCH)]
    for m in range(NCH):
        C.P.dma("sp", lambda e, m=m: e.dma_start(out=C.f32(OFF_ACC + m * NT * 4, NT), in_=dr["xT"][m * 128:(m + 1) * 128, :]),
                writes=[Bz[m][0], Bz[m][1]], sembuf=Bz[m][0])
    return Bz


def host_R_inputs(layer, w_out, ln_mix_g, ln_mix_b, ln_ffn_g, ln_ffn_b, moe_group_w, moe_group_b, moe_expert_w,
                  moe_expert_b, moe_w_gate, moe_w_up, moe_w_down):
    f = np.float32
    woutp = np.ascontiguousarray(w_out.reshape(NCH, 128, NCH, 128).transpose(2, 1, 0, 3).reshape(NCH, 128, 2048), dtype=f)
    wg = moe_w_gate[layer].reshape(NEXP, NCH, 128, 4, 128).transpose(0, 3, 2, 1, 4).reshape(NEXP, 4, 128, 2048)
    wu = moe_w_up[layer].reshape(NEXP, NCH, 128, 4, 128).transpose(0, 3, 2, 1, 4).reshape(NEXP, 4, 128, 2048)
    wd = moe_w_down[layer].reshape(NEXP, 4, 128, 2048)
    moew = np.empty((NEXP, 12, 128, 2048), dtype=f)
    moew[:, 0:8:2] = wg
    moew[:, 1:8:2] = wu
    moew[:, 8:12] = wd
    wr_full = np.concatenate([moe_group_w[layer], moe_expert_w[layer].transpose(1, 0, 2).reshape(D, 32)], axis=1)
    wr = np.ascontiguousarray(wr_full.reshape(NCH, 128, 36).transpose(1, 0, 2).reshape(128, NCH * 36), dtype=f)
    rb = np.concatenate([moe_group_b[layer], moe_expert_b[layer].reshape(32)])[None, :].astype(f)
    lnp = np.stack([ln_mix_g[layer], ln_mix_b[layer], ln_ffn_g[layer], ln_ffn_b[layer]], 0)
    lnp = np.ascontiguousarray(lnp.reshape(4, NCH, 128).transpose(2, 0, 1).reshape(128, 4 * NCH), dtype=f)
    return dict(woutp=woutp, moew=moew, wr=wr, rb=rb, lnp=lnp, ident=np.eye(128, dtype=f))


NBLK = 8
NTILE = 32
NSLOT = 8


class Stager:
    def __init__(self, C, tag):
        self.C = C
        self.stage = [C.f32(OFF_STAGE + i * 8 * KB, 2048) for i in range(NSTAGE)]
        self.B = [Buf("%s_st%d" % (tag, i)) for i in range(NSTAGE)]
        self.n = 0

    def load_cast(self, src, dst, Bdst, n, parts=128, q="sp"):
        P = self.C.P
        assert n <= 2048
        i = self.n
        self.n += 1
        s = i % NSTAGE
        st, bst = self.stage[s][0:parts, 0:n], self.B[s]
        P.dma(q, lambda e: e.dma_start(out=st, in_=src), writes=[bst])
        if i % 2 == 0:
            P.op("dve", lambda e: e.tensor_copy(out=dst, in_=st), reads=[bst], writes=[Bdst])
        else:
            P.op("act", lambda e: e.activation(out=dst, in_=st, func=AF.Copy), reads=[bst], writes=[Bdst])

    def load_w(self, src, dst_fn, Bdst, ntot, parts=128):
        for c0 in range(0, ntot, 2048):
            n = min(2048, ntot - c0)
            self.load_cast(src[:, c0:c0 + n], dst_fn(c0, n), Bdst, n, parts)


def make_xbf_scratch(C, dr, stg, Bx_prev=None):
    P = C.P
    tmp = [C.bf(i * 4 * KB, 2048) for i in range(4)]
    Bt = [Buf("xc%d" % i) for i in range(4)]
    Bx = Buf("xTb") if Bx_prev is None else Bx_prev
    Bxo = Buf("xTob")
    n = 0
    jobs = ((dr["xTfull"], dr["xTb"], S, Bx), (dr["xT"], dr["xTob"], NT, Bxo))
    if Bx_prev is not None:
        jobs = jobs[1:]
    for (src, dst, ncol, B) in jobs:
        for k in range(NCH):
            for c0 in range(0, ncol, 2048):
                w = min(2048, ncol - c0)
                t, bt = tmp[n % 4][:, 0:w], Bt[n % 4]
                n += 1
                stg.load_cast(src[k * 128:(k + 1) * 128, c0:c0 + w], t, bt, w)
                P.dma("pool", lambda e, t=t, dst=dst, k=k, c0=c0, w=w: e.dma_start(out=dst[k * 128:(k + 1) * 128, c0:c0 + w], in_=t),
                      reads=[bt], writes=[B], sembuf=bt)
    return Bx, Bxo


def load_xblk(C, src, Bsrc, c0, ncol, dst_off, Bdst, q="sp"):
    dst = C.bf(dst_off, NCH * ncol).rearrange("p (k n) -> p k n", k=NCH)
    s = src[:, c0:c0 + ncol].rearrange("(k p) n -> p k n", p=128)
    for g in range(4):
        C.P.dma(q, lambda e, g=g: e.dma_start(out=dst[:, g * 4:(g + 1) * 4, :], in_=s[:, g * 4:(g + 1) * 4, :]), reads=[Bsrc], writes=[Bdst])
    return lambda k, a=0, b=None: C.bf(dst_off + (k * ncol + a) * 2, (ncol if b is None else b) - a)


def proj_fm(C, ps_ap, Bps, w_ap, Bw, xk, Bx, M=128):
    for k in range(NCH):
        xa = xk(k)
        C.P.op("pe", lambda e, k=k, xa=xa: e.matmul(ps_ap, lhsT=w_ap[:, k * M:(k + 1) * M], rhs=xa, start=(k == 0), stop=(k == NCH - 1)),
               reads=[Bw, Bx], writes=[Bps])


def proj_tm(C, ps_ap, Bps, w_ap, Bw, xk, Bx, ncols):
    for k in range(NCH):
        xa = xk(k)
        C.P.op("pe", lambda e, k=k, xa=xa: e.matmul(ps_ap, lhsT=xa, rhs=w_ap[:, k * ncols:(k + 1) * ncols], start=(k == 0), stop=(k == NCH - 1)),
               reads=[Bw, Bx], writes=[Bps])


def setup_mix_consts(C, dr, names):
    P = C.P
    out = {}
    off = OFF_MISC
    for (nm, n, dt_) in names:
        if dt_ == "f32":
            ap = C.f32(off, n)
            off += n * 4
        else:
            ap = C.bf(off, n)
            off += n * 2
        off = (off + 3) // 4 * 4
        b = Buf(nm)
        P.dma("sp", lambda e, ap=ap, nm=nm: e.dma_start(out=ap, in_=dr[nm]), writes=[b])
        out[nm] = (ap, b)
    assert off <= 202 * KB, off
    return out


def phase_M0(C, dr, Bx_prev=None, vq=None):
    P = C.P
    cached = vq is not None and vq > 0
    store = vq == 0
    nc = C.nc
    stg = Stager(C, "m0")
    Bot = [[Buf("ot%d_%d" % (k, h)) for h in range(2)] for k in range(NCH)]
    otslot = lambda ch, u: C.bf(OFF_OT + (ch * NT + u * 128) * 2, 128)
    K = setup_mix_consts(C, dr, [("triL32", 128, "f32"), ("triU32", 128, "f32"), ("cind32", 2, "f32"), ("sel64", 128, "f32"),
                                 ("glamask", 128, "bf16"), ("selt", 32, "f32"), ("pent", 32, "f32"), ("dmask", 32 * 128, "bf16"),
                                 ("onesbf", 128, "bf16"), ("g_ng", 2, "f32"), ("f_gb", 8, "f32"), ("triF32", 128, "f32")])
    Bx, Bxo = make_xbf_scratch(C, dr, stg, Bx_prev)
    C.last_Bx = Bx
    P.barrier()
    ones32, Bones = C.ones32, C.Bones
    stop = getattr(C, 'stop', None)
    if stop == 'A':
        return Bot
    A0 = 0
    for hd in (range(4) if not getattr(C, 'skip_gla', False) else []):
        P.barrier()
        wq = C.bf(A0, 2048); wk = C.bf(A0 + 4 * KB, 2048); wkv = C.bf(A0 + 8 * KB, 6144)
        wgr = [C.bf(A0 + 20 * KB + i * 4 * KB, 2048) for i in range(2)]
        wglr = C.bf(A0 + 28 * KB, 256)
        w2 = C.bf(A0 + 29 * KB, 128, parts=17)
        Bw = Buf("gw")
        stg.load_w(dr["g_wq"][hd], lambda c0, n: wq[:, c0:c0 + n], Bw, 2048)
        stg.load_w(dr["g_wk"][hd], lambda c0, n: wk[:, c0:c0 + n], Bw, 2048)
        stg.load_w(dr["g_wkv"][hd], lambda c0, n: wkv[:, c0:c0 + n], Bw, 6144)
        for i in range(2):
            stg.load_w(dr["g_wgr"][hd, i], lambda c0, n, i=i: wgr[i][:, c0:c0 + n], Bw, 2048)
        stg.load_w(dr["g_wglr"], lambda c0, n: wglr[:, c0:c0 + n], Bw, 256)
        stg.load_cast(dr["g_w2"][hd], w2, Bw, 128, parts=17)
        if stop == 'G0a':
            return Bot
        XO = A0 + 32 * KB
        Bxb = [Buf("xblk0"), Buf("xblk1")]
        T0 = A0 + 64 * KB
        glrT = C.bf(T0, 512, parts=17); BglrT = Buf("glrT")
        kv = C.bf(T0 + 1 * KB, 384); Bkv = Buf("kv")
        e1 = C.f32(T0 + 2 * KB, 128); Be1 = Buf("e1")
        lap = C.f32(T0 + 3 * KB, 128); Blap = Buf("lap")
        ek = C.f32(T0 + 4 * KB, 128); Bek = Buf("ek")
        kend = C.bf(T0 + 5 * KB, 256); Bkend = Buf("kend")
        dec = C.f32(T0 + 6 * KB, 2); Bdec = Buf("dec")
        Sst = C.f32(T0 + 7 * KB, 256); BS = Buf("S")
        Ssel = [C.f32(T0 + 8 * KB + u * KB, 256) for u in range(NSLOT)]; BSsel = [Buf("Ssel%d" % u) for u in range(NSLOT)]
        selt, Bselt = K["selt"]
        P.op("pool", lambda e: e.memset(glrT[0:17, :], 1.0), writes=[BglrT])
        P.op("dve", lambda e: e.memset(Sst, 0.0), writes=[BS])
        for u in range(NSLOT):
            P.op("dve", lambda e, u=u: e.memset(Ssel[u], 0.0), writes=[BSsel[u]])
        p_kv, b_kv = C.ps[0], C.Bps[0]
        p_gl, b_gl = C.ps[1], C.Bps[1]
        p_m, b_m = C.ps[2], C.Bps[2]
        p_cs, b_cs = C.ps[3], C.Bps[3]

        def gate_and_la(xk, Bxk, t0, w, with_kv=True):
            xs = lambda k: xk(k, t0, t0 + 128)
            proj_tm(C, p_kv[:, 0:384], b_kv, wkv, Bw, xs, Bxk, 384)
            P.op("act", lambda e: e.activation(out=kv, in_=p_kv[:, 0:384], func=AF.Copy), reads=[b_kv], writes=[Bkv])
            P.op("pe", lambda e: e.matmul(p_m[:, 0:128], lhsT=glrT[0:17, t0:t0 + 128], rhs=w2[0:17, :], start=True, stop=True),
                 reads=[BglrT, Bw], writes=[b_m])
            P.op("act", lambda e: e.activation(out=e1, in_=p_m[:, 0:128], func=AF.Exp, scale=-1.0), reads=[b_m], writes=[Be1])
            P.op("act", lambda e: e.activation(out=lap, in_=e1, func=AF.Ln, bias=1.0), reads=[Be1], writes=[Blap])

        def glr_block(xk, Bxk, ntok):
            proj_fm(C, p_gl[0:16, 0:ntok], b_gl, wglr, Bw, lambda k: xk(k), Bxk, M=16)
            P.op("dve", lambda e: e.tensor_copy(out=glrT[0:16, 0:ntok], in_=p_gl[0:16, 0:ntok]), reads=[b_gl], writes=[BglrT])

        def state_steps(upd_sel_tile=None):
            triU, BtriU = K["triU32"]
            cind, Bcind = K["cind32"]
            P.op("pe", lambda e: e.matmul(p_m[:, 128:256], lhsT=triU, rhs=lap, start=True, stop=True), reads=[BtriU, Blap], writes=[b_m])
            P.op("act", lambda e: e.activation(out=ek, in_=p_m[:, 128:256], func=AF.Exp, scale=-1.0 / 16), reads=[b_m], writes=[Bek])
            if stop == 'G0d1':
                return
            P.op("pe", lambda e: e.matmul(p_m[:, 256:258], lhsT=lap, rhs=cind, start=True, stop=True), reads=[Bcind, Blap], writes=[b_m])
            P.op("act", lambda e: e.activation(out=dec, in_=p_m[:, 256:258], func=AF.Exp, scale=-1.0 / 16), reads=[b_m], writes=[Bdec])
            if stop == 'G0d2':
                return
            for c in range(2):
                P.op("dve", lambda e, c=c: e.scalar_tensor_tensor(out=kend[:, c * 128:(c + 1) * 128], in0=kv[:, 0:128], scalar=cind[:, c:c + 1], in1=ek,
                                                                 op0=ALU.mult, op1=ALU.mult), reads=[Bkv, Bek, Bcind], writes=[Bkend])
            for c in range(2):
                P.op("pe", lambda e, c=c: e.matmul(p_cs[:, c * 256:(c + 1) * 256], lhsT=kend[:, c * 128:(c + 1) * 128], rhs=kv[:, 128:384],
                                                   start=True, stop=True), reads=[Bkend, Bkv], writes=[b_cs])

        snapb = [C.f32(T0 + 26 * KB + i * KB, 256) for i in range(4)]
        Bsn = [Buf("snapb%d" % i) for i in range(4)]
        Bsnapd = Buf("snapd")
        if cached:
            ot_ = own_tiles(vq)
            for u in range(NSLOT):
                P.dma("sp", lambda e, u=u, tl=ot_[u], hd=hd, Ssel=Ssel: e.dma_start(out=Ssel[u], in_=dr["snap"][hd * NTILE + tl]), writes=[BSsel[u]])
        for blk in (range(NBLK) if not cached else []):
            xk = load_xblk(C, dr["xTb"], Bx, blk * 512, 512, XO + (blk % 2) * 16 * KB, Bxb[blk % 2])
            if stop == 'G0b0':
                return Bot
            glr_block(xk, Bxb[blk % 2], 512)
            if stop == 'G0b':
                return Bot
            for tt in range(4):
                tile_i = blk * 4 + tt
                gate_and_la(xk, Bxb[blk % 2], tt * 128, None)
                if stop == 'G0c':
                    return Bot
                state_steps()
                if stop and stop.startswith('G0d'):
                    return Bot
                u = tile_i // 4
                P.op("dve", lambda e, u=u, tile_i=tile_i: e.scalar_tensor_tensor(out=Ssel[u], in0=Sst, scalar=selt[:, tile_i:tile_i + 1], in1=Ssel[u],
                                                                               op0=ALU.mult, op1=ALU.add), reads=[BS, Bselt, BSsel[u]], writes=[BSsel[u]])
                if store:
                    sn, bsn = snapb[tile_i % 4], Bsn[tile_i % 4]
                    P.op("act", lambda e, sn=sn: e.activation(out=sn, in_=Sst, func=AF.Copy), reads=[BS], writes=[bsn])
                    P.dma("pool", lambda e, sn=sn, tile_i=tile_i, hd=hd: e.dma_start(out=dr["snap"][hd * NTILE + tile_i], in_=sn), reads=[bsn], writes=[Bsnapd], sembuf=bsn)
                for c in range(2):
                    P.op("dve", lambda e, c=c: e.scalar_tensor_tensor(out=Sst, in0=Sst, scalar=dec[:, c:c + 1], in1=p_cs[:, c * 256:(c + 1) * 256],
                                                                     op0=ALU.mult, op1=ALU.add), reads=[BS, Bdec, b_cs], writes=[BS])
        if stop == 'G1':
            return Bot
        O0 = T0 + 16 * KB
        eb = C.f32(O0, 128); enb = C.f32(O0 + 512, 128); Beb = Buf("eb")
        qd = C.bf(O0 + 1 * KB, 128); kin = C.bf(O0 + 1 * KB + 256, 128); Bqk = Buf("qk")
        sT = C.bf(O0 + 1 * KB + 512, 128); BsT = Buf("sT")
        Sa = C.bf(O0 + 2 * KB, 256); Sb32 = C.f32(O0 + 3 * KB, 256); Sb = C.bf(O0 + 4 * KB, 256); BSab = Buf("Sab")
        sq = [C.f32(O0 + 5 * KB + i * 512, 128) for i in range(2)]; Bsq = Buf("gsq")
        rs = C.f32(O0 + 6 * KB, 128); Brs = Buf("grs")
        sgr = [C.f32(O0 + 7 * KB + i * 512, 128) for i in range(2)]; Bsgr = Buf("sgr")
        tt_ = C.f32(O0 + 8 * KB, 128); Btt = Buf("gtt")
        triL, BtriL = K["triL32"]
        gmask, Bgmask = K["glamask"]
        ng, Bng = K["g_ng"]
        p_q, b_q = C.ps[4], C.Bps[4]
        p_k, b_k = C.ps[5], C.Bps[5]
        p_o, b_o = C.ps[6], C.Bps[6]
        p_g, b_g = C.ps[7], C.Bps[7]
        for half in range(2):
            xk = load_xblk(C, dr["xTob"], Bxo, half * 512, 512, XO + half * 16 * KB, Bxb[half])
            glr_block(xk, Bxb[half], 512)
            for tt in range(4):
                u = half * 4 + tt
                t0 = tt * 128
                gate_and_la(xk, Bxb[half], t0, None)
                state_steps()
                xs = lambda k, xk=xk, t0=t0: xk(k, t0, t0 + 128)
                P.op("pe", lambda e: e.matmul(p_m[:, 258:386], lhsT=lap, rhs=triL, start=True, stop=True), reads=[Blap, BtriL], writes=[b_m])
                P.op("act", lambda e: e.activation(out=eb, in_=p_m[:, 258:386], func=AF.Exp, scale=-1.0 / 16), reads=[b_m], writes=[Beb])
                P.op("act", lambda e: e.activation(out=enb, in_=p_m[:, 258:386], func=AF.Exp, scale=1.0 / 16), reads=[b_m], writes=[Beb])
                proj_fm(C, p_q[:, 0:128], b_q, wq, Bw, xs, Bxb[half])
                proj_fm(C, p_k[:, 0:128], b_k, wk, Bw, xs, Bxb[half])
                P.op("dve", lambda e: e.scalar_tensor_tensor(out=qd, in0=p_q[:, 0:128], scalar=128.0 ** -0.5, in1=eb, op0=ALU.mult, op1=ALU.mult),
                     reads=[b_q, Beb], writes=[Bqk])
                P.op("dve", lambda e: e.tensor_tensor(out=kin, in0=p_k[:, 0:128], in1=enb, op=ALU.mult), reads=[b_k, Beb], writes=[Bqk])
                P.op("act", lambda e, u=u: e.activation(out=Sa, in_=Ssel[u], func=AF.Copy), reads=[BSsel[u]], writes=[BSab])
                P.op("dve", lambda e, u=u: e.scalar_tensor_tensor(out=Sb32, in0=Ssel[u], scalar=dec[:, 0:1], in1=p_cs[:, 0:256], op0=ALU.mult, op1=ALU.add),
                     reads=[BSsel[u], Bdec, b_cs], writes=[BSab])
                P.op("act", lambda e: e.activation(out=Sb, in_=Sb32, func=AF.Copy), reads=[BSab], writes=[BSab])
                P.op("pe", lambda e: e.matmul(p_o[:, 0:128], lhsT=kin, rhs=qd, start=True, stop=True), reads=[Bqk], writes=[b_o])
                P.op("dve", lambda e: e.tensor_tensor(out=sT, in0=p_o[:, 0:128], in1=gmask, op=ALU.mult), reads=[b_o, Bgmask], writes=[BsT])
                for vc in range(2):
                    po = p_o[:, 128 + vc * 128: 256 + vc * 128]
                    P.op("pe", lambda e, vc=vc, po=po: e.matmul(po, lhsT=kv[:, 128 + vc * 128: 256 + vc * 128], rhs=sT, start=True, stop=False),
                         reads=[Bkv, BsT], writes=[b_o])
                    P.op("pe", lambda e, vc=vc, po=po: e.matmul(po[:, 0:64], lhsT=Sa[:, vc * 128:(vc + 1) * 128], rhs=qd[:, 0:64], start=False, stop=False),
                         reads=[BSab, Bqk], writes=[b_o])
                    P.op("pe", lambda e, vc=vc, po=po: e.matmul(po[:, 64:128], lhsT=Sb[:, vc * 128:(vc + 1) * 128], rhs=qd[:, 64:128], start=False, stop=True),
                         reads=[BSab, Bqk], writes=[b_o])
                    P.op("act", lambda e, vc=vc, po=po: e.activation(out=sq[vc], in_=po, func=AF.Square), reads=[b_o], writes=[Bsq])
                for vc in range(2):
                    P.op("pe", lambda e, vc=vc: e.matmul(p_k[:, 128:256], lhsT=ones32, rhs=sq[vc], start=(vc == 0), stop=(vc == 1)),
                         reads=[Bones, Bsq], writes=[b_k])
                P.op("dve", lambda e: e.tensor_scalar(out=rs, in0=p_k[:, 128:256], scalar1=1.0 / 256, scalar2=EPS, op0=ALU.mult, op1=ALU.add),
                     reads=[b_k], writes=[Brs])
                P.op("act", lambda e: e.activation(out=rs, in_=rs, func=AF.Sqrt), reads=[Brs], writes=[Brs])
                P.op("dve", lambda e: e.reciprocal(out=rs, in_=rs), reads=[Brs], writes=[Brs])
                for vc in range(2):
                    proj_fm(C, p_g[:, vc * 128:(vc + 1) * 128], b_g, wgr[vc], Bw, xs, Bxb[half])
                    P.op("act", lambda e, vc=vc: e.activation(out=sgr[vc], in_=p_g[:, vc * 128:(vc + 1) * 128], func=AF.Silu), reads=[b_g], writes=[Bsgr])
                    po = p_o[:, 128 + vc * 128: 256 + vc * 128]
                    P.op("dve", lambda e, po=po: e.tensor_tensor(out=tt_, in0=po, in1=rs, op=ALU.mult), reads=[b_o, Brs], writes=[Btt])
                    ch = hd * 2 + vc
                    P.op("dve", lambda e, vc=vc, ch=ch, u=u: e.scalar_tensor_tensor(out=otslot(ch, u), in0=tt_, scalar=ng[:, vc:vc + 1], in1=sgr[vc],
                                                                                  op0=ALU.mult, op1=ALU.mult),
                         reads=[Btt, Bng, Bsgr], writes=[Bot[ch][u // 4]])
    if stop == 'G':
        return Bot
    for pr in range(4):
        P.barrier()
        wq = [C.bf(A0 + i * 4 * KB, 2048) for i in range(2)]
        wk = [C.bf(A0 + 8 * KB + i * 4 * KB, 2048) for i in range(2)]
        wvf = C.bf(A0 + 16 * KB, NCH * 258)
        Bw = Buf("fw")
        for i in range(2):
            stg.load_w(dr["f_wq"][pr * 2 + i], lambda c0, n, i=i: wq[i][:, c0:c0 + n], Bw, 2048)
            stg.load_w(dr["f_wk"][pr * 2 + i], lambda c0, n, i=i: wk[i][:, c0:c0 + n], Bw, 2048)
        stg.load_w(dr["f_wvf"][pr], lambda c0, n: wvf[:, c0:c0 + n], Bw, NCH * 258)
        XO = A0 + 26 * KB
        Bxb = [Buf("fxblk0"), Buf("fxblk1")]
        KT = [C.bf(A0 + 58 * KB + i * 8 * KB, S) for i in range(2)]
        BKT = [[Buf("KT%d_%d" % (i, b)) for b in range(NBLK)] for i in range(2)]
        VV = C.bf(A0 + 74 * KB, NTILE * 256)
        BVV = [Buf("VV%d" % t) for t in range(NTILE)]
        FB = 128 * KB
        QT = [C.bf(FB + i * 2 * KB, NT) for i in range(2)]
        BQT = [Buf("QT0"), Buf("QT1")]
        ffv = C.f32(FB + 4 * KB, 64); Bffv = Buf("ffv")
        cc = C.f32(FB + 4 * KB + 256, 64); Bcc = Buf("cc")
        cbc = C.f32(FB + 4 * KB + 512, 64); Bcbc = Buf("cbc")
        tot = C.f32(FB + 4 * KB + 768, 64); Btot = Buf("tot")
        cref = C.f32(FB + 5 * KB, 16); Bcref = Buf("cref")
        bias = C.f32(FB + 6 * KB, 2 * NTILE * NSLOT); Bbias = Buf("bias")
        PT = [C.bf(FB + 8 * KB + i * KB, 512) for i in range(4)]; BPT = [Buf("PT%d" % i) for i in range(4)]
        rin = C.f32(FB + 12 * KB, 128); Brin = Buf("rin")
        gb, Bgb = K["f_gb"]
        selt, Bselt = K["selt"]
        pent, Bpent = K["pent"]
        dmask, Bdmask = K["dmask"]
        onesbf, Bonesbf = K["onesbf"]
        triL, BtriL = K["triL32"]
        sel64, Bsel64 = K["sel64"]
        p_a, b_a = C.ps[0], C.Bps[0]
        p_b, b_b = C.ps[1], C.Bps[1]
        Bfsc = Buf("fscr")
        if cached:
            for i in range(2):
                P.dma("sp", lambda e, i=i, pr=pr, KT=KT: e.dma_start(out=KT[i], in_=dr["fk_s"][pr][:, i * S:(i + 1) * S]), writes=BKT[i], sembuf=BKT[i][0])
            P.dma("sp", lambda e, pr=pr, VV=VV: e.dma_start(out=VV, in_=dr["fv_s"][pr]), writes=BVV, sembuf=BVV[0])
            P.dma("sp", lambda e, pr=pr, cc=cc: e.dma_start(out=cc, in_=dr["fc_s"][pr][:, 0:64]), writes=[Bcc])
            P.dma("sp", lambda e, pr=pr, cbc=cbc: e.dma_start(out=cbc, in_=dr["fc_s"][pr][:, 64:128]), writes=[Bcbc])
        for blk in (range(NBLK) if not cached else []):
            xk = load_xblk(C, dr["xTb"], Bx, blk * 512, 512, XO + (blk % 2) * 16 * KB, Bxb[blk % 2])
            for i in range(2):
                proj_fm(C, p_a[:, :], b_a, wk[i], Bw, lambda k: xk(k), Bxb[blk % 2])
                P.op("act", lambda e, i=i, blk=blk: e.activation(out=KT[i][:, blk * 512:(blk + 1) * 512], in_=p_a[:, :], func=AF.Copy),
                     reads=[b_a], writes=[BKT[i][blk]])
            for tt in range(4):
                t = blk * 4 + tt
                proj_tm(C, p_b[:, 0:258], b_b, wvf, Bw, lambda k, tt=tt: xk(k, tt * 128, tt * 128 + 128), Bxb[blk % 2], 258)
                P.op("dve", lambda e, t=t: e.tensor_copy(out=VV[:, t * 256:(t + 1) * 256], in_=p_b[:, 0:256]), reads=[b_b], writes=[BVV[t]])
                for i in range(2):
                    P.op("dve", lambda e, t=t, i=i: e.tensor_copy(out=ffv[:, i * 32 + t: i * 32 + t + 1], in_=p_b[:, 256 + i:257 + i]),
                         reads=[b_b], writes=[Bffv])
        for half in range(2):
            xk = load_xblk(C, dr["xTob"], Bxo, half * 512, 512, XO + half * 16 * KB, Bxb[half])
            for i in range(2):
                proj_fm(C, p_a[:, :], b_a, wq[i], Bw, lambda k: xk(k), Bxb[half])
                P.op("act", lambda e, i=i, half=half: e.activation(out=QT[i][:, half * 512:(half + 1) * 512], in_=p_a[:, :], func=AF.Copy),
                     reads=[b_a], writes=[BQT[i]])
        if stop == 'F0':
            return Bot
        for i in (range(2) if not cached else []):
            hidx = pr * 2 + i
            fs = ffv[:, i * 32:(i + 1) * 32]
            P.op("dve", lambda e, fs=fs, hidx=hidx: e.tensor_scalar(out=fs, in0=fs, scalar1=gb[:, hidx:hidx + 1], scalar2=None, op0=ALU.add),
                 reads=[Bffv, Bgb], writes=[Bffv])
            P.op("act", lambda e, fs=fs: e.activation(out=fs, in_=fs, func=AF.Exp, scale=-1.0), reads=[Bffv], writes=[Bffv])
            P.op("act", lambda e, fs=fs: e.activation(out=fs, in_=fs, func=AF.Ln, bias=1.0), reads=[Bffv], writes=[Bffv])
            P.op("pe", lambda e, fs=fs: e.matmul(p_a[:, 0:32], lhsT=triL_full(K), rhs=fs, start=True, stop=True), reads=[Bffv, K["triF32"][1]], writes=[b_a])
            P.op("pe", lambda e, fs=fs: e.matmul(p_a[:, 32:64], lhsT=ones32, rhs=fs, start=True, stop=True), reads=[Bffv, Bones], writes=[b_a])
            ci = cc[:, i * 32:(i + 1) * 32]
            ti = tot[:, i * 32:(i + 1) * 32]
            P.op("dve", lambda e, ti=ti: e.tensor_copy(out=ti, in_=p_a[:, 32:64]), reads=[b_a], writes=[Btot])
            P.op("dve", lambda e, ci=ci: e.tensor_copy(out=ci, in_=p_a[:, 0:32]), reads=[b_a], writes=[Bcc])
            for j in range(1, NTILE):
                if j >= 2:
                    P.op("dve", lambda e, ti=ti, j=j: e.tensor_tensor(out=ti[:, j - 1:j], in0=ti[:, j - 1:j], in1=ti[:, j - 2:j - 1], op=ALU.add),
                         reads=[Btot], writes=[Btot])
                P.op("dve", lambda e, ci=ci, ti=ti, j=j: e.tensor_tensor(out=ci[:, j:j + 1], in0=ci[:, j:j + 1], in1=ti[:, j - 1:j], op=ALU.add),
                     reads=[Btot, Bcc], writes=[Bcc])
            P.op("pe", lambda e, ci=ci: e.matmul(p_a[:, 64:96], lhsT=sel64, rhs=ci, start=True, stop=True), reads=[Bcc, Bsel64], writes=[b_a])
            cb = cbc[:, i * 32:(i + 1) * 32]
            P.op("dve", lambda e, cb=cb: e.tensor_copy(out=cb, in_=p_a[:, 64:96]), reads=[b_a], writes=[Bcbc])
        if store:
            for i in range(2):
                P.dma("pool", lambda e, i=i, pr=pr, KT=KT: e.dma_start(out=dr["fk_s"][pr][:, i * S:(i + 1) * S], in_=KT[i]), reads=BKT[i], writes=[Bfsc], sembuf=BKT[i][0])
            P.dma("pool", lambda e, pr=pr, VV=VV: e.dma_start(out=dr["fv_s"][pr], in_=VV), reads=BVV, writes=[Bfsc], sembuf=BVV[0])
            P.dma("pool", lambda e, pr=pr, cc=cc: e.dma_start(out=dr["fc_s"][pr][:, 0:64], in_=cc), reads=[Bcc], writes=[Bfsc], sembuf=Bcc)
            P.dma("pool", lambda e, pr=pr, cbc=cbc: e.dma_start(out=dr["fc_s"][pr][:, 64:128], in_=cbc), reads=[Bcbc], writes=[Bfsc], sembuf=Bcbc)
        for i in range(2):
            ci = cc[:, i * 32:(i + 1) * 32]
            cb = cbc[:, i * 32:(i + 1) * 32]
            for u in range(NSLOT):
                cr = cref[:, i * 8 + u: i * 8 + u + 1]
                for d_ in range(4):
                    j = 4 * u + d_
                    if d_ == 0:
                        P.op("dve", lambda e, cr=cr, cb=cb, j=j: e.tensor_scalar(out=cr, in0=cb[:, j:j + 1], scalar1=selt[:, j:j + 1], scalar2=None, op0=ALU.mult),
                             reads=[Bcbc, Bselt], writes=[Bcref])
                    else:
                        P.op("dve", lambda e, cr=cr, cb=cb, j=j: e.scalar_tensor_tensor(out=cr, in0=cb[:, j:j + 1], scalar=selt[:, j:j + 1], in1=cr,
                                                                                     op0=ALU.mult, op1=ALU.add), reads=[Bcbc, Bselt, Bcref], writes=[Bcref])
            for j in range(NTILE):
                bj = bias[:, (i * NTILE + j) * NSLOT:(i * NTILE + j + 1) * NSLOT]
                P.op("dve", lambda e, bj=bj, ci=ci, j=j, i=i: e.tensor_scalar(out=bj, in0=cref[:, i * 8:(i + 1) * 8], scalar1=-1.0, scalar2=ci[:, j:j + 1],
                                                                        op0=ALU.mult, op1=ALU.add), reads=[Bcref, Bcc], writes=[Bbias])
                u = j // 4
                P.op("dve", lambda e, bj=bj, j=j, u=u: e.tensor_tensor(out=bj[:, u:u + 1], in0=bj[:, u:u + 1], in1=pent[:, j:j + 1], op=ALU.add),
                     reads=[Bpent, Bbias], writes=[Bbias])
        if stop == 'F1':
            dbg = dr['dbg']
            P.dma('sp', lambda e: e.dma_start(out=dbg[:, 0:64], in_=cc), reads=[Bcc], sembuf=C.Bout)
            P.dma('sp', lambda e: e.dma_start(out=dbg[:, 64:80], in_=cref), reads=[Bcref], sembuf=C.Bout)
            P.dma('sp', lambda e: e.dma_start(out=dbg[:, 128:640], in_=bias), reads=[Bbias], sembuf=C.Bout)
            P.dma('sp', lambda e: e.dma_start(out=dbg[:, 640:704], in_=ffv), reads=[Bffv], sembuf=C.Bout)
            P.dma('sp', lambda e: e.dma_start(out=dbg[:, 704:768], in_=cbc), reads=[Bcbc], sembuf=C.Bout)
            return Bot
        fitems = [(i, u, g) for i in range(2) for u in range(NSLOT) for g in range(u + 1)]

        def emit_Sg(n):
            i, u, g = fitems[n]
            p_s, b_s = C.ps[n % 4], C.Bps[n % 4]
            qs = QT[i][:, u * 128:(u + 1) * 128]
            for jj in range(4):
                j = g * 4 + jj
                P.op("pe", lambda e, p_s=p_s, jj=jj, j=j, i=i, qs=qs: e.matmul(p_s[:, jj * 128:(jj + 1) * 128], lhsT=KT[i][:, j * 128:(j + 1) * 128], rhs=qs,
                                                                           start=True, stop=True), reads=[BKT[i][j // 4], BQT[i]], writes=[b_s])
        for n in range(min(2, len(fitems))):
            emit_Sg(n)
        for n, (i, u, g) in enumerate(fitems):
            hidx = pr * 2 + i
            J = 4 * u + 4
            ngrp = u + 1
            oi = i * NSLOT + u
            p_o, b_o = C.ps[4 + (oi % 2)], C.Bps[4 + (oi % 2)]
            p_r, b_r = C.ps[6 + (oi % 2)], C.Bps[6 + (oi % 2)]
            p_s, b_s = C.ps[n % 4], C.Bps[n % 4]
            pt, bpt = PT[n % 4], BPT[n % 4]
            for jj in range(4):
                j = g * 4 + jj
                bcol = bias[:, (i * NTILE + j) * NSLOT + u:(i * NTILE + j) * NSLOT + u + 1]
                P.op("act", lambda e, p_s=p_s, pt=pt, jj=jj, bcol=bcol: e.activation(out=pt[:, jj * 128:(jj + 1) * 128], in_=p_s[:, jj * 128:(jj + 1) * 128],
                                                                                 func=AF.Exp, scale=128.0 ** -0.5, bias=bcol),
                     reads=[b_s, Bbias], writes=[bpt])
            if g == ngrp - 1:
                P.op("pool", lambda e, pt=pt, u=u: e.tensor_tensor(out=pt, in0=pt, in1=dmask[:, u * 512:(u + 1) * 512], op=ALU.mult),
                     reads=[bpt, Bdmask], writes=[bpt])
            if n + 2 < len(fitems):
                emit_Sg(n + 2)
            for jj in range(4):
                j = g * 4 + jj
                P.op("pe", lambda e, pt=pt, jj=jj, j=j, i=i, p_o=p_o, J=J: e.matmul(p_o[:, 0:128], lhsT=VV[:, j * 256 + i * 128: j * 256 + (i + 1) * 128],
                                                                               rhs=pt[:, jj * 128:(jj + 1) * 128], start=(j == 0), stop=(j == J - 1)),
                     reads=[bpt, BVV[j]], writes=[b_o])
            for jj in range(4):
                j = g * 4 + jj
                P.op("pe", lambda e, pt=pt, jj=jj, j=j, p_r=p_r, J=J: e.matmul(p_r[:, 0:128], lhsT=onesbf, rhs=pt[:, jj * 128:(jj + 1) * 128],
                                                                          start=(j == 0), stop=(j == J - 1)), reads=[bpt, Bonesbf], writes=[b_r])
            if g == ngrp - 1:
                P.op("dve", lambda e, p_r=p_r: e.reciprocal(out=rin, in_=p_r[:, 0:128]), reads=[b_r], writes=[Brin])
                ch = 8 + hidx
                P.op("dve", lambda e, p_o=p_o, ch=ch, u=u: e.tensor_tensor(out=otslot(ch, u), in0=p_o[:, 0:128], in1=rin, op=ALU.mult),
                     reads=[b_o, Brin], writes=[Bot[ch][u // 4]])
    P.barrier()
    return Bot


def triL_full(K):
    return K["triF32"][0]


def own_tiles(cq):
    return [4 * u + (cq if u % 2 == 0 else 3 - cq) for u in range(NSLOT)]


def host_mix_tables(cq):
    import ml_dtypes
    bf = ml_dtypes.bfloat16
    f = np.float32
    own = own_tiles(cq)
    t = np.arange(128)
    same = (t[:, None] // 64) == (t[None, :] // 64)
    T = {}
    T["triL32"] = ((t[:, None] <= t[None, :]) & same).astype(f)
    T["triU32"] = ((t[:, None] > t[None, :]) & same).astype(f)
    T["triF32"] = (t[:, None] <= t[None, :]).astype(f)
    T["cind32"] = (t[:, None] // 64 == np.arange(2)[None, :]).astype(f)
    s64 = np.zeros((128, 128), f); s64[64, :] = 1.0
    T["sel64"] = s64
    T["glamask"] = ((t[:, None] <= t[None, :]) & same).astype(bf)
    selt = np.zeros((128, 32), f); pent = np.zeros((128, 32), f)
    dmask = np.zeros((128, 32, 128), f)
    negm = np.zeros((128, 32, 128), f)
    for u in range(NSLOT):
        for d_ in range(4):
            j = 4 * u + d_
            if j == own[u]:
                selt[:, j] = 1.0
                dmask[:, j, :] = (t[:, None] <= t[None, :])
                negm[:, j, :] = np.where(t[None, :] <= t[:, None], 0.0, -BIG)
            elif j < own[u]:
                dmask[:, j, :] = 1.0
            else:
                pent[:, j] = -BIG
                negm[:, j, :] = -BIG
    T["selt"] = selt
    T["pent"] = pent
    T["dmask"] = dmask.reshape(128, 32 * 128).astype(bf)
    T["negm"] = negm.reshape(128, 32 * 128).astype(bf)
    T["onesbf"] = np.ones((128, 128), bf)
    T["identbf"] = np.eye(128).astype(bf)
    return T


def fm_layout(W):
    n = W.shape[1]
    return np.ascontiguousarray(W.reshape(NCH, 128, n).transpose(1, 0, 2).reshape(128, NCH * n), dtype=np.float32)


def host_M0_inputs(a_w_in, a_gla_gate_w2, a_gla_gate_b, a_gla_norm_g, a_fox_gate_b):
    W = a_w_in[0]
    oq, ok_, ov, ogr, oglr, ofq, ofk, ofv, off_ = 0, 512, 1024, 2048, 3072, 3088, 4112, 5136, 6160
    d = {}
    d["g_wq"] = np.stack([fm_layout(W[:, oq + h * 128: oq + (h + 1) * 128]) for h in range(4)])
    d["g_wk"] = np.stack([fm_layout(W[:, ok_ + h * 128: ok_ + (h + 1) * 128]) for h in range(4)])
    d["g_wkv"] = np.stack([fm_layout(np.concatenate([W[:, ok_ + h * 128: ok_ + (h + 1) * 128], W[:, ov + h * 256: ov + (h + 1) * 256]], 1)) for h in range(4)])
    d["g_wgr"] = np.stack([np.stack([fm_layout(W[:, ogr + h * 256 + i * 128: ogr + h * 256 + (i + 1) * 128]) for i in range(2)]) for h in range(4)])
    d["g_wglr"] = fm_layout(W[:, oglr:oglr + 16])
    d["g_w2"] = np.stack([np.concatenate([a_gla_gate_w2[0][:, h * 128:(h + 1) * 128], a_gla_gate_b[0][None, h * 128:(h + 1) * 128]], 0) for h in range(4)]).astype(np.float32)
    d["g_ng"] = np.ascontiguousarray(a_gla_norm_g[0].reshape(2, 128).T, dtype=np.float32)
    d["f_gb"] = np.ascontiguousarray(np.broadcast_to(a_fox_gate_b[0][None, :], (128, 8)), dtype=np.float32)
    d["f_wq"] = np.stack([fm_layout(W[:, ofq + h * 128: ofq + (h + 1) * 128]) for h in range(8)])
    d["f_wk"] = np.stack([fm_layout(W[:, ofk + h * 128: ofk + (h + 1) * 128]) for h in range(8)])
    d["f_wvf"] = np.stack([fm_layout(np.concatenate([W[:, ofv + 2 * p * 128: ofv + (2 * p + 2) * 128], W[:, off_ + 2 * p: off_ + 2 * p + 2]], 1)) for p in range(4)])
    return d


M0_DRAM = [("g_wq", [4, 128, 2048]), ("g_wk", [4, 128, 2048]), ("g_wkv", [4, 128, 6144]), ("g_wgr", [4, 2, 128, 2048]), ("g_wglr", [128, 256]),
           ("g_w2", [4, 17, 128]), ("g_ng", [128, 2]), ("f_gb", [128, 8]), ("f_wq", [8, 128, 2048]), ("f_wk", [8, 128, 2048]),
           ("f_wvf", [4, 128, NCH * 258]),
           ("triL32", [128, 128]), ("triU32", [128, 128]), ("triF32", [128, 128]), ("cind32", [128, 2]), ("sel64", [128, 128]),
           ("selt", [128, 32]), ("pent", [128, 32])]
M0_DRAM_BF = [("glamask", [128, 128]), ("dmask", [128, 4096]), ("onesbf", [128, 128])]


def declare_mix_dram(nc, dr, f32list, bflist):
    for nm, shp in f32list:
        dr[nm] = nc.dram_tensor(nm, shp, F32, kind="ExternalInput").ap()
    for nm, shp in bflist:
        dr[nm] = nc.dram_tensor(nm, shp, BF16, kind="ExternalInput").ap()
    dr["xTfull"] = nc.dram_tensor("xTfull", [D, S], F32, kind="ExternalInput").ap()
    dr["xTb"] = nc.dram_tensor("xTb", [D, S], BF16, kind="ExternalOutput").ap()
    dr["xTob"] = nc.dram_tensor("xTob", [D, NT], BF16, kind="ExternalOutput").ap()


NEG2 = -3.0e38


def rope_evac(C, py, bpy, pyp, bpyp, cosb, sinb, Bcs, out_ap, Bout, t1, t2, Bt, view=None):
    P = C.P
    P.op("dve", lambda e: e.tensor_tensor(out=t1, in0=py, in1=cosb, op=ALU.mult), reads=[bpy, Bcs], writes=[Bt[0]])
    P.op("dve", lambda e: e.tensor_tensor(out=t2, in0=pyp, in1=sinb, op=ALU.mult), reads=[bpyp, Bcs], writes=[Bt[1]])
    a, b = (t1, t2) if view is None else (view(t1), view(t2))
    P.op("pool", lambda e: e.tensor_tensor(out=out_ap, in0=a, in1=b, op=ALU.add), reads=[Bt[0], Bt[1]], writes=[Bout])


def phase_M1(C, dr, pre=None):
    P = C.P
    stg = Stager(C, "m1")
    Bot = [[Buf("ot%d_%d" % (k, h)) for h in range(2)] for k in range(NCH)]
    K = setup_mix_consts(C, dr, [("negm", 4096, "bf16"), ("onesbf", 128, "bf16"), ("identbf", 128, "bf16")])
    negm, Bnegm = K["negm"]
    onesbf, Bonesbf = K["onesbf"]
    identbf, Bidentbf = K["identbf"]
    SM = OFF_MISC + 12 * KB
    iw = C.f32(SM, 128); Biw = Buf("iw")
    m8 = C.f32(SM + 512, 8); Bm8 = Buf("m8")
    wiw = C.bf(SM + 1024, 256); Bwiw = Buf("wiw")
    Bx, Bxo = make_xbf_scratch(C, dr, stg) if pre is None else pre
    P.barrier()
    stop = getattr(C, 'stop', None)
    maskT = [C.bf(sum(range(1, u + 1)) * KB, (u + 1) * 512) for u in range(NSLOT)]
    BmaskT = [Buf("maskT%d" % u) for u in range(NSLOT)]
    XBLK = 128 * KB
    Bxb = Buf("xblk")
    iqT = C.bf(36 * KB, 8 * NT); BiqT = Buf("iqT")
    ikA = C.bf(52 * KB, S); ikB = C.bf(60 * KB, S); Bik = [Buf("ik%d" % b) for b in range(NBLK)]
    acc = C.f32(68 * KB, S); Bacc = Buf("acc")
    maskq = C.bf(84 * KB, S); Bmq = Buf("maskq")
    t1 = C.f32(92 * KB, 512); t2 = C.f32(94 * KB, 512); Bt = [Buf("t1"), Buf("t2")]
    wsl = [C.bf(144 * KB + i * 4 * KB, 2048) for i in range(2)]; Bwsl = [Buf("wsl0"), Buf("wsl1")]
    tab = [C.f32(152 * KB + i * 2 * KB, 512) for i in range(4)]; Btab = Buf("tab")
    p0, b0 = C.ps[0], C.Bps[0]
    p1, b1 = C.ps[1], C.Bps[1]
    for i in range(2):
        stg.load_w(dr["d_wik"][i], lambda c0, n, i=i: wsl[i][:, c0:c0 + n], Bwsl[i], 2048)
    for blk in range(NBLK):
        xk = load_xblk(C, dr["xTb"], Bx, blk * 512, 512, XBLK, Bxb)
        for i in range(4):
            P.dma("sp", lambda e, i=i, blk=blk, tb=tab[i]: e.dma_start(out=tb, in_=dr["rtik"][i][:, blk * 512:(blk + 1) * 512]), writes=[Btab])
        proj_fm(C, p0[:, :], b0, wsl[0], Bwsl[0], lambda k, xk=xk: xk(k), Bxb)
        proj_fm(C, p1[:, :], b1, wsl[1], Bwsl[1], lambda k, xk=xk: xk(k), Bxb)
        rope_evac(C, p0[:, :], b0, p1[:, :], b1, tab[0], tab[1], Btab, ikA[:, blk * 512:(blk + 1) * 512], Bik[blk], t1, t2, Bt)
        rope_evac(C, p0[:, :], b0, p1[:, :], b1, tab[2], tab[3], Btab, ikB[:, blk * 512:(blk + 1) * 512], Bik[blk], t1, t2, Bt)
    stg.load_cast(dr["d_wiw"], wiw, Bwiw, 256)
    p2, b2 = C.ps[2], C.Bps[2]
    for half in range(2):
        xk = load_xblk(C, dr["xTob"], Bxo, half * 512, 512, XBLK, Bxb)
        for i in range(2):
            P.dma("sp", lambda e, i=i, half=half, tb=tab[i]: e.dma_start(out=tb, in_=dr["rt64o"][i][:, half * 512:(half + 1) * 512]), writes=[Btab])
        for tt in range(4):
            u = half * 4 + tt
            proj_tm(C, p2[:, u * 16:(u + 1) * 16], b2, wiw, Bwiw, lambda k, xk=xk, tt=tt: xk(k, tt * 128, tt * 128 + 128), Bxb, 16)
        for pc in range(8):
            for i in range(2):
                stg.load_w(dr["d_wiq"][pc, i], lambda c0, n, i=i: wsl[i][:, c0:c0 + n], Bwsl[i], 2048)
            proj_fm(C, p0[:, :], b0, wsl[0], Bwsl[0], lambda k, xk=xk: xk(k), Bxb)
            proj_fm(C, p1[:, :], b1, wsl[1], Bwsl[1], lambda k, xk=xk: xk(k), Bxb)
            rope_evac(C, p0[:, :], b0, p1[:, :], b1, tab[0], tab[1], Btab, iqT[:, pc * NT + half * 512: pc * NT + (half + 1) * 512], BiqT, t1, t2, Bt)
    P.op("dve", lambda e: e.tensor_copy(out=iw, in_=p2[:, 0:128]), reads=[b2], writes=[Biw])
    if stop == 'I0':
        return Bot
    rb = [tab[0], tab[1]]
    Brb = [Buf("rb0"), Buf("rb1")]
    vld = C.bf(156 * KB, 512); Bvld = Buf("vld")
    pT = C.ps[4][:].bitcast(BF16)
    bpT = C.Bps[4]
    cnt = 0
    for u in range(NSLOT):
        L = 512 * (u + 1)
        for kb in range(u + 1):
            for h in range(16):
                pc = h // 2
                ik_ = ikA if h % 2 == 0 else ikB
                ps_, bps_ = C.ps[2 + cnt % 2], C.Bps[2 + cnt % 2]
                r_, br_ = rb[cnt % 2], Brb[cnt % 2]
                cnt += 1
                P.op("pe", lambda e, ps_=ps_, pc=pc, u=u, ik_=ik_, kb=kb: e.matmul(ps_[:, :], lhsT=iqT[:, pc * NT + u * 128: pc * NT + (u + 1) * 128],
                                                                            rhs=ik_[:, kb * 512:(kb + 1) * 512], start=True, stop=True),
                     reads=[BiqT, Bik[kb]], writes=[bps_])
                P.op("act", lambda e, ps_=ps_, r_=r_: e.activation(out=r_, in_=ps_[:, :], func=AF.Relu, scale=1.0 / 32), reads=[bps_], writes=[br_])
                a_ = acc[:, kb * 512:(kb + 1) * 512]
                wcol = iw[:, u * 16 + h: u * 16 + h + 1]
                if h == 0:
                    P.op("dve", lambda e, a_=a_, r_=r_, wcol=wcol: e.tensor_scalar(out=a_, in0=r_, scalar1=wcol, scalar2=None, op0=ALU.mult),
                         reads=[br_, Biw], writes=[Bacc])
                else:
                    P.op("dve", lambda e, a_=a_, r_=r_, wcol=wcol: e.scalar_tensor_tensor(out=a_, in0=r_, scalar=wcol, in1=a_, op0=ALU.mult, op1=ALU.add),
                         reads=[br_, Biw, Bacc], writes=[Bacc])
        aw = acc[:, u * 512:(u + 1) * 512]
        nm = negm[:, u * 512:(u + 1) * 512]
        P.op("dve", lambda e, aw=aw, nm=nm: e.tensor_tensor(out=aw, in0=aw, in1=nm, op=ALU.add), reads=[Bacc, Bnegm], writes=[Bacc])
        if stop == 'I1' and u == 1:
            P.dma('sp', lambda e: e.dma_start(out=dr['dbg'][:, 0:1024], in_=acc[:, 0:1024]), reads=[Bacc], sembuf=C.Bout)
            return Bot
        al = acc[:, 0:L]
        for r in range(32):
            P.op("dve", lambda e, al=al: e.max(out=m8, in_=al), reads=[Bacc], writes=[Bm8])
            P.op("dve", lambda e, al=al: e.match_replace(out=al, in_to_replace=m8, in_values=al, imm_value=NEG2), reads=[Bacc, Bm8], writes=[Bacc])
        mq = maskq[:, 0:L]
        P.op("dve", lambda e, al=al, mq=mq: e.tensor_scalar(out=mq, in0=al, scalar1=-2.0e38, scalar2=None, op0=ALU.is_le), reads=[Bacc], writes=[Bmq])
        P.op("pool", lambda e, nm=nm: e.tensor_scalar(out=vld, in0=nm, scalar1=-1.0, scalar2=None, op0=ALU.is_ge), reads=[Bnegm], writes=[Bvld])
        mw = maskq[:, u * 512:(u + 1) * 512]
        P.op("pool", lambda e, mw=mw: e.tensor_tensor(out=mw, in0=mw, in1=vld, op=ALU.mult), reads=[Bmq, Bvld], writes=[Bmq])
        for j4 in range(u + 1):
            for jj in range(4):
                j = j4 * 4 + jj
                P.op("pe", lambda e, jj=jj, j=j: e.transpose(pT[:, jj * 128:(jj + 1) * 128], maskq[:, j * 128:(j + 1) * 128], identbf),
                     reads=[Bmq, Bidentbf], writes=[bpT])
            P.op("act", lambda e, u=u, j4=j4: e.activation(out=maskT[u][:, j4 * 512:(j4 + 1) * 512], in_=pT[:, 0:512], func=AF.Copy),
                 reads=[bpT], writes=[BmaskT[u]])
    if stop == 'I2':
        for u in range(NSLOT):
            P.dma('sp', lambda e, u=u: e.dma_start(out=dr['dbgm'][:, sum(range(1, u + 1)) * 512: sum(range(1, u + 2)) * 512], in_=maskT[u]),
                  reads=[BmaskT[u]], sembuf=C.Bout)
        return Bot
    XB2 = [128 * KB, 144 * KB]
    Bxb2 = [Buf("xblkA0"), Buf("xblkA1")]
    for kvh in range(4):
        P.barrier()
        KT = C.bf(36 * KB, S); BKT = [Buf("KT%d" % b) for b in range(NBLK)]
        VV = C.bf(44 * KB, S); BVV = [Buf("VV%d" % t) for t in range(NTILE)]
        qT4 = C.bf(52 * KB, NSLOT * 512); BqT = [Buf("qT%d" % h) for h in range(2)]
        qT4v = qT4.rearrange("p (u g t) -> p u g t", u=NSLOT, g=4)
        PT = [C.bf(60 * KB + i * KB, 512) for i in range(4)]; BPT = [Buf("PT%d" % i) for i in range(4)]
        t1 = C.f32(64 * KB, 512); t2 = C.f32(66 * KB, 512); Bt = [Buf("t1"), Buf("t2")]
        rin = t1; Brin = Bt[0]
        tabs = [[C.f32(68 * KB + s_ * 4 * KB + i * 2 * KB, 512) for i in range(2)] for s_ in range(2)]
        Btabs = [Buf("tabA"), Buf("tabB")]
        wset = [[C.bf(76 * KB + s_ * 8 * KB + i * 4 * KB, 2048) for i in range(2)] for s_ in range(2)]
        Bwset = [[Buf("ws%d_%d" % (s_, i)) for i in range(2)] for s_ in range(2)]
        wv = C.bf(92 * KB, 2048); Bwv = Buf("wv")
        wk, Bwk = wset[1], Bwset[1]
        for i in range(2):
            stg.load_w(dr["d_wk"][kvh, i], lambda c0, n, i=i, wk=wk: wk[i][:, c0:c0 + n], Bwk[i], 2048)
        stg.load_w(dr["d_wv"][kvh], lambda c0, n, wv=wv: wv[:, c0:c0 + n], Bwv, 2048)
        nt = 0
        for blk in range(NBLK):
            xk = load_xblk(C, dr["xTb"], Bx, blk * 512, 512, XB2[blk % 2], Bxb2[blk % 2])
            tab, Btab = tabs[nt % 2], Btabs[nt % 2]
            nt += 1
            for i in range(2):
                P.dma("sp", lambda e, i=i, blk=blk, tb=tab[i]: e.dma_start(out=tb, in_=dr["rt128"][i][:, blk * 512:(blk + 1) * 512]), writes=[Btab])
            proj_fm(C, p0[:, :], b0, wk[0], Bwk[0], lambda k, xk=xk: xk(k), Bxb2[blk % 2])
            proj_fm(C, p1[:, :], b1, wk[1], Bwk[1], lambda k, xk=xk: xk(k), Bxb2[blk % 2])
            rope_evac(C, p0[:, :], b0, p1[:, :], b1, tab[0], tab[1], Btab, KT[:, blk * 512:(blk + 1) * 512], BKT[blk], t1, t2, Bt)
            for tt in range(4):
                t = blk * 4 + tt
                pv, bv = (p2, b2) if tt % 2 == 0 else (C.ps[3], C.Bps[3])
                proj_tm(C, pv[:, 0:128], bv, wv, Bwv, lambda k, xk=xk, tt=tt: xk(k, tt * 128, tt * 128 + 128), Bxb2[blk % 2], 128)
                P.op("act", lambda e, t=t, VV=VV, pv=pv: e.activation(out=VV[:, t * 128:(t + 1) * 128], in_=pv[:, 0:128], func=AF.Copy), reads=[bv], writes=[BVV[t]])
        ng = 0
        for half in range(2):
            xk = load_xblk(C, dr["xTob"], Bxo, half * 512, 512, XB2[half], Bxb2[half])
            tab, Btab = tabs[nt % 2], Btabs[nt % 2]
            nt += 1
            for i in range(2):
                P.dma("sp", lambda e, i=i, half=half, tb=tab[i]: e.dma_start(out=tb, in_=dr["rt128o"][i][:, half * 512:(half + 1) * 512]), writes=[Btab])
            for g in range(4):
                hq = kvh * 4 + g
                wq, Bwq = wset[ng % 2], Bwset[ng % 2]
                ng += 1
                for i in range(2):
                    stg.load_w(dr["d_wq"][hq, i], lambda c0, n, i=i, wq=wq: wq[i][:, c0:c0 + n], Bwq[i], 2048)
                proj_fm(C, p0[:, :], b0, wq[0], Bwq[0], lambda k, xk=xk: xk(k), Bxb2[half])
                proj_fm(C, p1[:, :], b1, wq[1], Bwq[1], lambda k, xk=xk: xk(k), Bxb2[half])
                v4 = lambda ap: ap.rearrange("p (u t) -> p u t", u=4)
                rope_evac(C, p0[:, :], b0, p1[:, :], b1, tab[0], tab[1], Btab, qT4v[:, half * 4:(half + 1) * 4, g, :], BqT[half], t1, t2, Bt, view=v4)
        items = [(u, j) for u in range(NSLOT) for j in range(4 * (u + 1))]

        def emit_S(idx):
            u, j = items[idx]
            p_s, b_s = C.ps[idx % 4], C.Bps[idx % 4]
            qs = qT4[:, u * 512:(u + 1) * 512]
            P.op("pe", lambda e, p_s=p_s, j=j, qs=qs, KT=KT: e.matmul(p_s[:, :], lhsT=KT[:, j * 128:(j + 1) * 128], rhs=qs, start=True, stop=True),
                 reads=[BKT[j // 4], BqT[u // 4]], writes=[b_s])
        LOOK = 2
        for idx in range(min(LOOK, len(items))):
            emit_S(idx)
        for idx, (u, j) in enumerate(items):
            J = 4 * (u + 1)
            p_o, b_o = C.ps[4 + u % 2], C.Bps[4 + u % 2]
            p_r, b_r = C.ps[6 + u % 2], C.Bps[6 + u % 2]
            p_s, b_s = C.ps[idx % 4], C.Bps[idx % 4]
            pt, bpt = PT[idx % 4], BPT[idx % 4]
            P.op("act", lambda e, p_s=p_s, pt=pt: e.activation(out=pt, in_=p_s[:, :], func=AF.Exp, scale=128.0 ** -0.5), reads=[b_s], writes=[bpt])
            mk = maskT[u][:, j * 128:(j + 1) * 128]
            eng = "pool" if idx % 3 == 2 else "dve"
            P.op(eng, lambda e, pt=pt, mk=mk: e.tensor_tensor(out=pt.rearrange("p (g t) -> p g t", g=4), in0=pt.rearrange("p (g t) -> p g t", g=4),
                                                         in1=mk.unsqueeze(1).to_broadcast([128, 4, 128]), op=ALU.mult),
                 reads=[bpt, BmaskT[u]], writes=[bpt])
            if idx + LOOK < len(items):
                emit_S(idx + LOOK)
            P.op("pe", lambda e, p_o=p_o, j=j, pt=pt, J=J, VV=VV: e.matmul(p_o[:, :], lhsT=VV[:, j * 128:(j + 1) * 128], rhs=pt, start=(j == 0), stop=(j == J - 1)),
                 reads=[BVV[j], bpt], writes=[b_o])
            P.op("pe", lambda e, p_r=p_r, j=j, pt=pt, J=J: e.matmul(p_r[:, :], lhsT=onesbf, rhs=pt, start=(j == 0), stop=(j == J - 1)),
                 reads=[Bonesbf, bpt], writes=[b_r])
            if j == J - 1:
                P.op("dve", lambda e, p_r=p_r, rin=rin: e.reciprocal(out=rin, in_=p_r[:, :]), reads=[b_r], writes=[Brin])
                otv = C.bf(OFF_OT, NCH * NT).rearrange("p (c t) -> p c t", c=NCH)[:, kvh * 4:(kvh + 1) * 4, u * 128:(u + 1) * 128]
                P.op("dve", lambda e, p_o=p_o, otv=otv, rin=rin: e.tensor_tensor(out=otv, in0=p_o[:, :].rearrange("p (g t) -> p g t", g=4),
                                                                               in1=rin.rearrange("p (g t) -> p g t", g=4), op=ALU.mult),
                     reads=[b_o, Brin], writes=[Bot[kvh * 4 + g][u // 4] for g in range(4)])
    P.barrier()
    return Bot


def rope_tables(pos, dh, npieces_heads=1):
    half = dh // 2
    inv = (10000.0 ** (-(np.arange(half, dtype=np.float32)) / np.float32(half))).astype(np.float32)
    ang = pos.astype(np.float32)[None, :] * inv[:, None]
    c = np.cos(ang).astype(np.float32)
    s = np.sin(ang).astype(np.float32)
    d = np.arange(128) % dh
    cosT = c[d % half]
    sinT = np.where((d < half)[:, None], -s[d % half], s[d % half])
    return cosT.astype(np.float32), sinT.astype(np.float32)


def perm_cols(n, dh):
    idx = np.arange(n)
    d = idx % dh
    half = dh // 2
    return np.where(d < half, idx + half, idx - half)


def host_M1_inputs(c_w_in, cq):
    W = c_w_in[0]
    oq, ok_, ov, oiq, oik, oiw = 0, 2048, 2560, 3072, 4096, 4160
    d = {}
    p128 = perm_cols(128, 128)
    p64 = perm_cols(128, 64)
    Wq = W[:, oq:oq + 2048]
    d["d_wq"] = np.stack([np.stack([fm_layout(Wq[:, h * 128:(h + 1) * 128]), fm_layout(Wq[:, h * 128:(h + 1) * 128][:, p128])]) for h in range(16)])
    Wk = W[:, ok_:ok_ + 512]
    d["d_wk"] = np.stack([np.stack([fm_layout(Wk[:, h * 128:(h + 1) * 128]), fm_layout(Wk[:, h * 128:(h + 1) * 128][:, p128])]) for h in range(4)])
    d["d_wv"] = np.stack([fm_layout(W[:, ov + h * 128: ov + (h + 1) * 128]) for h in range(4)])
    Wiq = W[:, oiq:oiq + 1024]
    d["d_wiq"] = np.stack([np.stack([fm_layout(Wiq[:, pc * 128:(pc + 1) * 128]), fm_layout(Wiq[:, pc * 128:(pc + 1) * 128][:, p64])]) for pc in range(8)])
    Wik2 = np.concatenate([W[:, oik:oik + 64], W[:, oik:oik + 64]], 1)
    d["d_wik"] = np.stack([fm_layout(Wik2), fm_layout(Wik2[:, p64])])
    d["d_wiw"] = fm_layout(W[:, oiw:oiw + 16])
    pos = np.arange(S)
    own = own_tiles(cq)
    opos = np.concatenate([np.arange(i * 128, (i + 1) * 128) for i in own])
    c128, s128 = rope_tables(pos, 128)
    c64, s64 = rope_tables(pos, 64)
    d["rt128"] = np.stack([c128, s128])
    d["rt128o"] = np.stack([c128[:, opos], s128[:, opos]])
    d["rt64o"] = np.stack([c64[:, opos], s64[:, opos]])
    mA = (np.arange(128) < 64)[:, None].astype(np.float32)
    d["rtik"] = np.stack([c64 * mA, s64 * mA, c64 * (1 - mA), s64 * (1 - mA)])
    return {k: np.ascontiguousarray(v, dtype=np.float32) for k, v in d.items()}


M1_DRAM = [("d_wq", [16, 2, 128, 2048]), ("d_wk", [4, 2, 128, 2048]), ("d_wv", [4, 128, 2048]), ("d_wiq", [8, 2, 128, 2048]),
           ("d_wik", [2, 128, 2048]), ("d_wiw", [128, 256]), ("rt128", [2, 128, S]), ("rt128o", [2, 128, NT]), ("rt64o", [2, 128, NT]),
           ("rtik", [4, 128, S])]
M1_DRAM_BF = [("negm", [128, 4096]), ("onesbf", [128, 128]), ("identbf", [128, 128])]


_PROG_CACHE = {}
R_NAMES = ("woutp", "moew", "wr", "rb", "lnp")


def prep_layer1(C, dr):
    P = C.P
    P.new_epoch()
    stg = Stager(C, "p1")
    oh = C.f32(OFF_MISC, 4)
    Boh = Buf("oh")
    P.dma("sp", lambda e: e.dma_start(out=oh, in_=dr["onehot"]), writes=[Boh])
    tmp = [C.bf(OFF_XB + i * 2 * KB, NT) for i in range(4)]
    Bt = [Buf("p1t%d" % i) for i in range(4)]
    Bx1, Bxo1 = Buf("xTb1"), Buf("xTob1")
    Bz = [Buf("p1z%d" % k) for k in range(NCH)]
    z = lambda k: C.f32(OFF_ACC + k * NT * 4, NT)
    n = 0
    for vq in range(4):
        for k in range(NCH):
            si = stg.n % NSTAGE
            stg.n += 1
            st, bst = stg.stage[si][:, 0:NT], stg.B[si]
            P.dma("sp", lambda e, st=st, vq=vq, k=k: e.dma_start(out=st, in_=dr["x1sh"][vq][k * 128:(k + 1) * 128, :]), writes=[bst])
            t, bt = tmp[n % 4], Bt[n % 4]
            n += 1
            P.op("act", lambda e, t=t, st=st: e.activation(out=t, in_=st, func=AF.Copy), reads=[bst], writes=[bt])
            dstv = dr["xTb1"][k * 128:(k + 1) * 128, :].rearrange("p (u2 par d t) -> p u2 par d t", u2=4, par=2, d=4)
            tv = t.rearrange("p (u2 par t) -> p u2 par t", u2=4, par=2)
            for par in range(2):
                d_ = vq if par == 0 else 3 - vq
                P.dma("pool", lambda e, dstv=dstv, tv=tv, par=par, d_=d_: e.dma_start(out=dstv[:, :, par, d_, :], in_=tv[:, :, par, :]),
                      reads=[bt], writes=[Bx1], sembuf=bt)
            if vq == 0:
                P.op("dve", lambda e, k=k, st=st: e.tensor_scalar(out=z(k), in0=st, scalar1=oh[:, 0:1], scalar2=None, op0=ALU.mult),
                     reads=[bst, Boh], writes=[Bz[k]])
            else:
                P.op("dve", lambda e, k=k, st=st, vq=vq: e.scalar_tensor_tensor(out=z(k), in0=st, scalar=oh[:, vq:vq + 1], in1=z(k), op0=ALU.mult, op1=ALU.add),
                     reads=[bst, Boh, Bz[k]], writes=[Bz[k]])
    for k in range(NCH):
        P.dma("sp", lambda e, k=k: e.dma_start(out=dr["x1own"][k * 128:(k + 1) * 128, :], in_=z(k)), reads=[Bz[k]], sembuf=C.Bout)
        t, bt = tmp[n % 4], Bt[n % 4]
        n += 1
        P.op("act", lambda e, t=t, k=k: e.activation(out=t, in_=z(k), func=AF.Copy), reads=[Bz[k]], writes=[bt])
        P.dma("pool", lambda e, t=t, k=k: e.dma_start(out=dr["xTob"][k * 128:(k + 1) * 128, :], in_=t), reads=[bt], writes=[Bxo1], sembuf=bt)
    P.barrier()
    return Bx1, Bxo1


M0_TABLES = ("selt", "pent", "dmask")


def build_fused_program():
    if "fused" in _PROG_CACHE:
        return _PROG_CACHE["fused"]
    nc = bass.Bass("TRN2", target_bir_lowering=False)
    dr = {}
    f32in = lambda nm, shp: dr.__setitem__(nm, nc.dram_tensor(nm, shp, F32, kind="ExternalInput").ap())
    bfin = lambda nm, shp: dr.__setitem__(nm, nc.dram_tensor(nm, shp, BF16, kind="ExternalInput").ap())
    f32in("xTfull", [D, S])
    f32in("xTsh", [4, D, NT])
    f32in("ident", [128, 128])
    f32in("onehot", [128, 4])
    for L in range(2):
        f32in("woutp%d" % L, [NCH, 128, 2048])
        f32in("moew%d" % L, [NEXP, 12, 128, 2048])
        f32in("wr%d" % L, [128, NCH * 36])
        f32in("rb%d" % L, [1, 36])
        f32in("lnp%d" % L, [128, 4 * NCH])
    for nm, shp in M0_DRAM:
        if nm in M0_TABLES:
            f32in(nm + "_v", [4] + shp)
        else:
            f32in(nm, shp)
    for nm, shp in M0_DRAM_BF:
        if nm in M0_TABLES:
            bfin(nm + "_v", [4] + shp)
        else:
            bfin(nm, shp)
    for nm, shp in M1_DRAM:
        f32in(nm, shp)
    for nm, shp in M1_DRAM_BF:
        if nm not in dr:
            bfin(nm, shp)
    scr = lambda nm, shp, dt_: dr.__setitem__(nm, nc.dram_tensor(nm, shp, dt_, kind="ExternalOutput").ap())
    scr("xTb", [D, S], BF16)
    scr("xTob", [D, NT], BF16)
    scr("x1sh", [4, D, NT], F32)
    scr("xTb1", [D, S], BF16)
    scr("x1own", [D, NT], F32)
    scr("snap", [4 * NTILE, 128, 256], F32)
    scr("fk_s", [4, 128, 2 * S], BF16)
    scr("fv_s", [4, 128, NTILE * 256], BF16)
    scr("fc_s", [4, 128, 128], F32)
    scr("outT", [D, NT], F32)
    dr["cwd"] = nc.dram_tensor("cwd", [NEXP, NT], F32, kind="Internal").ap()
    with contextlib.ExitStack() as st:
        C = Ctx(nc, st)
        setup_consts(C, dr)
        P = C.P
        Bx = None
        for vq in range(4):
            P.new_epoch()
            drv = dict(dr)
            drv["xT"] = dr["xTsh"][vq]
            for nm in M0_TABLES:
                drv[nm] = dr[nm + "_v"][vq]
            for nm in R_NAMES:
                drv[nm] = dr[nm + "0"]
            drv["outT"] = dr["x1sh"][vq]
            Bot = phase_M0(C, drv, Bx_prev=Bx, vq=vq)
            Bx = C.last_Bx
            P.new_epoch()
            load_lnp(C, drv)
            Bz = load_z(C, drv)
            phase_R(C, drv, Bz, Bot)
        Bx1, Bxo1 = prep_layer1(C, dr)
        P.new_epoch()
        drv = dict(dr)
        drv["xTb"] = dr["xTb1"]
        drv["xT"] = dr["x1own"]
        for nm in R_NAMES:
            drv[nm] = dr[nm + "1"]
        Bot = phase_M1(C, drv, pre=(Bx1, Bxo1))
        P.new_epoch()
        load_lnp(C, drv)
        Bz = load_z(C, drv)
        phase_R(C, drv, Bz, Bot)
        P.emit()
    _PROG_CACHE["fused"] = nc
    return nc


def core_tokens(cq):
    return np.concatenate([np.arange(i * 128, (i + 1) * 128) for i in own_tiles(cq)])


def kernel(x, a_w_in, a_gla_gate_w2, a_gla_gate_b, a_gla_norm_g, a_fox_gate_b, a_w_out,
           c_w_in, c_w_out, ln_mix_g, ln_mix_b, ln_ffn_g, ln_ffn_b,
           moe_group_w, moe_group_b, moe_expert_w, moe_expert_b, moe_w_gate, moe_w_up, moe_w_down):
    A = lambda v: np.asarray(v)
    x = np.asarray(x, dtype=np.float32)
    nc = build_fused_program()
    shared = {}
    for L in range(2):
        r = host_R_inputs(L, A(a_w_out[0] if L == 0 else c_w_out[0]), A(ln_mix_g), A(ln_mix_b), A(ln_ffn_g), A(ln_ffn_b), A(moe_group_w),
                          A(moe_group_b), A(moe_expert_w), A(moe_expert_b), A(moe_w_gate), A(moe_w_up), A(moe_w_down))
        for nm in R_NAMES:
            shared[nm + str(L)] = r[nm]
        shared["ident"] = r["ident"]
    shared.update(host_M0_inputs(A(a_w_in), A(a_gla_gate_w2), A(a_gla_gate_b), A(a_gla_norm_g), A(a_fox_gate_b)))
    Tv = [host_mix_tables(vq) for vq in range(4)]
    for nm, _ in M0_DRAM[11:] + M0_DRAM_BF:
        if nm in M0_TABLES:
            shared[nm + "_v"] = np.stack([Tv[vq][nm] for vq in range(4)])
        else:
            shared[nm] = Tv[0][nm]
    xTb = [np.ascontiguousarray(x[b].T) for b in range(2)]
    xTsh = [np.stack([np.ascontiguousarray(xTb[b][:, core_tokens(vq)]) for vq in range(4)]) for b in range(2)]
    in_maps = []
    for core in range(8):
        b, cq = core // 4, core % 4
        m = dict(shared)
        m.update(host_M1_inputs(A(c_w_in), cq))
        for nm, _ in M1_DRAM_BF:
            if nm not in m:
                m[nm] = Tv[cq][nm]
        oh = np.zeros((128, 4), np.float32)
        oh[:, cq] = 1.0
        m["onehot"] = oh
        m["xTfull"] = xTb[b]
        m["xTsh"] = xTsh[b]
        in_maps.append(m)
    res = run_bass_kernel_spmd(nc, in_maps, core_ids=list(range(8)))
    out = np.empty_like(x)
    for core in range(8):
        b, cq = core // 4, core % 4
        out[b][core_tokens(cq)] = np.asarray(res.results[core]["outT"]).T
    return out
```

```python
import contextlib
import numpy as np
import concourse.bass as bass
import concourse.mybir as mybir
from concourse.bass_utils import run_bass_kernel_spmd

F32 = mybir.dt.float32
BF16 = mybir.dt.bfloat16
AF = mybir.ActivationFunctionType
ALU = mybir.AluOpType
AX = mybir.AxisListType

D = 2048
NCH = 16
NT = 1024
S = 4096
ALPHA = 4.0 ** 0.25
EPS = 1e-5
NEXP = 32
FF = 512
BIG = 1.0e30


class Buf:
    __slots__ = ("name", "lw", "rd", "dsem", "persist", "depoch")

    def __init__(self, name="", persist=False):
        self.name = name
        self.lw = None
        self.rd = []
        self.dsem = None
        self.persist = persist
        self.depoch = -1


class Prog:
    ENG = ("pe", "act", "dve", "pool", "sp")

    def __init__(self, nc, safe_same_engine=True):
        self.nc = nc
        self.safe = safe_same_engine
        self.ins = []
        self.perq = {e: [] for e in self.ENG}
        self.ndsem = 0
        self.pending = {e: set() for e in self.ENG}
        self.last_dma = {}
        self.epoch = 0
        self.NPERSIST = 6
        self.npersist = 0
        self.next_edsem = self.NPERSIST

    def _add(self, eng, fn, reads, writes, kind, dsem=None):
        iid = len(self.ins)
        deps = set(self.pending[eng])
        self.pending[eng] = set()
        for b in reads:
            if b.lw is not None:
                deps.add(b.lw)
        for b in writes:
            if b.lw is not None:
                deps.add(b.lw)
            last = {}
            for r in b.rd:
                rr = self.ins[r]
                if rr["kind"] == "dma":
                    deps.add(r)
                else:
                    last[rr["eng"]] = max(last.get(rr["eng"], -1), r)
            deps.update(last.values())
        self.ins.append(dict(eng=eng, fn=fn, deps=deps, kind=kind, dsem=dsem))
        self.perq[eng].append(iid)
        for b in reads:
            b.rd.append(iid)
        for b in writes:
            b.lw = iid
            b.rd = []
        if kind == "dma":
            self.last_dma[dsem] = iid
        return iid

    def op(self, eng, fn, reads=(), writes=()):
        return self._add(eng, fn, list(reads), list(writes), "op")

    def dma(self, q, fn, reads=(), writes=(), sembuf=None):
        if sembuf is None:
            sembuf = (list(writes) + list(reads))[0]
        if sembuf.persist:
            if sembuf.dsem is None:
                assert self.npersist < self.NPERSIST
                sembuf.dsem = self.npersist
                self.npersist += 1
        elif sembuf.dsem is None or sembuf.depoch != self.epoch:
            sembuf.dsem = self.next_edsem
            sembuf.depoch = self.epoch
            self.next_edsem += 1
        self.ndsem = max(self.ndsem, sembuf.dsem + 1)
        return self._add(q, fn, list(reads), list(writes), "dma", dsem=sembuf.dsem)

    def new_epoch(self):
        self.barrier()
        self.epoch += 1
        self.next_edsem = self.NPERSIST

    def barrier(self):
        dset = set()
        for e in self.ENG:
            if self.perq[e]:
                dset.add(self.perq[e][-1])
        dset.update(self.last_dma.values())
        for e in self.ENG:
            self.pending[e] |= dset

    def emit(self, final_wait_eng="sp"):
        nc = self.nc
        ins = self.ins
        marked = set()
        for it in ins:
            for d in it["deps"]:
                dd = ins[d]
                if dd["kind"] == "dma":
                    continue
                if dd["eng"] == it["eng"]:
                    if not self.safe:
                        continue
                    if dd["eng"] == "pe" and it["kind"] == "op":
                        continue
                marked.add(d)
        rank = {}
        for e in self.ENG:
            c = 0
            for iid in self.perq[e]:
                if ins[iid]["kind"] == "op" and iid in marked:
                    c += 1
                    rank[iid] = c
        dval = {}
        dcount = [0] * self.ndsem
        for i, it in enumerate(ins):
            if it["kind"] == "dma":
                dcount[it["dsem"]] += 16
                dval[i] = dcount[it["dsem"]]
        with contextlib.ExitStack() as st:
            esem = {e: st.enter_context(nc.semaphore("s_" + e)) for e in self.ENG}
            dsems = [st.enter_context(nc.semaphore("d%d" % k)) for k in range(self.ndsem)]
            block = st.enter_context(nc.Block())

            def make(e):
                def body(engobj):
                    seen = {}
                    for iid in self.perq[e]:
                        it = ins[iid]
                        need = {}
                        for d in it["deps"]:
                            dd = ins[d]
                            if dd["kind"] == "dma":
                                key = ("d", dd["dsem"])
                                val = dval[d]
                            else:
                                if d not in rank:
                                    continue
                                key = ("e", dd["eng"])
                                val = rank[d]
                            if need.get(key, 0) < val:
                                need[key] = val
                        for key, val in need.items():
                            if seen.get(key, 0) >= val:
                                continue
                            seen[key] = val
                            sem = dsems[key[1]] if key[0] == "d" else esem[key[1]]
                            engobj.wait_ge(sem, val)
                        r = it["fn"](engobj)
                        if it["kind"] == "dma":
                            r.then_inc(dsems[it["dsem"]], 16)
                        elif iid in rank:
                            r.then_inc(esem[e], 1)
                    if e == final_wait_eng:
                        for k in range(self.ndsem):
                            if dcount[k] and seen.get(("d", k), 0) < dcount[k]:
                                engobj.wait_ge(dsems[k], dcount[k])
                return body

            block.tensor(make("pe"))
            block.scalar(make("act"))
            block.vector(make("dve"))
            block.gpsimd(make("pool"))
            block.sync(make("sp"))


class Ctx:
    def __init__(self, nc, st):
        self.nc = nc
        import os as _os
        self.P = Prog(nc, safe_same_engine=not _os.environ.get("UNSAFE"))
        self.arena_words = 206 * 1024 // 4
        self.arena = st.enter_context(nc.sbuf_tensor("arena", [128, self.arena_words], F32))
        self.ps = [st.enter_context(nc.psum_tensor("ps%d" % i, [128, 512], F32)) for i in range(8)]
        self.Bps = [Buf("ps%d" % i) for i in range(8)]

    def f32(self, off, n, parts=128):
        assert off % 4 == 0 and off // 4 + n <= self.arena_words, (off, n)
        return self.arena[0:parts, off // 4: off // 4 + n]

    def bf(self, off, n, parts=128):
        assert off % 4 == 0 and n % 2 == 0 and off // 4 + n // 2 <= self.arena_words, (off, n)
        return self.arena[0:parts, off // 4: off // 4 + n // 2].bitcast(BF16)


KB = 1024
OFF_ACC = 0
OFF_XB = 64 * KB
OFF_OT = 96 * KB
OFF_RING = 128 * KB
OFF_STAGE = 160 * KB
OFF_MISC = 184 * KB
NRING = 8
NSTAGE = 3


class WStream:
    def __init__(self, C):
        self.C = C
        self.stage = [C.f32(OFF_STAGE + i * 8 * KB, 2048) for i in range(NSTAGE)]
        self.Bst = [Buf("st%d" % i) for i in range(NSTAGE)]
        self.ring = [C.bf(OFF_RING + i * 4 * KB, 2048) for i in range(NRING)]
        self.Brg = [Buf("rg%d" % i) for i in range(NRING)]
        self.n = 0

    def push(self, src_ap):
        P = self.C.P
        i = self.n
        self.n += 1
        s = i % NSTAGE
        r = i % NRING
        st, bst, rg, brg = self.stage[s], self.Bst[s], self.ring[r], self.Brg[r]
        q = "sp"
        P.dma(q, lambda e: e.dma_start(out=st, in_=src_ap), writes=[bst])
        ce = "act"
        if ce == "pool":
            P.op("pool", lambda e: e.tensor_copy(out=rg, in_=st), reads=[bst], writes=[brg])
        else:
            P.op("act", lambda e: e.activation(out=rg, in_=st, func=AF.Copy), reads=[bst], writes=[brg])
        return rg, brg


def layer_norm_fm(C, zoff, Bz, gcol, bcol, outs, tmp_off):
    P = C.P
    nc = C.nc
    ones = C.ones32
    Bones = C.Bones
    z = lambda m, h: C.f32(zoff + (m * NT + h * 512) * 4, 512)
    sq = [C.f32(tmp_off + i * 2 * KB, 512) for i in range(2)]
    Bsq = [Buf("sq0"), Buf("sq1")]
    mean = C.f32(tmp_off + 4 * KB, 512)
    rstd = C.f32(tmp_off + 6 * KB, 512)
    var = C.f32(tmp_off + 8 * KB, 512)
    Bmean, Brstd, Bvar = Buf("mean"), Buf("rstd"), Buf("var")
    for h in range(2):
        pa, pb = C.ps[6], C.ps[7]
        Bpa, Bpb = C.Bps[6], C.Bps[7]
        for m in range(NCH):
            zz = z(m, h)
            P.op("pe", lambda e, zz=zz, m=m: e.matmul(pa[:], lhsT=ones, rhs=zz, start=(m == 0), stop=(m == NCH - 1)),
                 reads=[Bones, Bz[m][h]], writes=[Bpa])
        for m in range(NCH):
            zz = z(m, h)
            s_, bs_ = sq[m % 2], Bsq[m % 2]
            P.op("act", lambda e, zz=zz, s_=s_: e.activation(out=s_, in_=zz, func=AF.Square), reads=[Bz[m][h]], writes=[bs_])
            P.op("pe", lambda e, s_=s_, m=m: e.matmul(pb[:], lhsT=ones, rhs=s_, start=(m == 0), stop=(m == NCH - 1)),
                 reads=[Bones, bs_], writes=[Bpb])
        P.op("dve", lambda e: e.tensor_scalar(out=mean, in0=pa[:], scalar1=1.0 / D, scalar2=None, op0=ALU.mult),
             reads=[Bpa], writes=[Bmean])
        P.op("dve", lambda e: e.tensor_tensor(out=var, in0=mean, in1=mean, op=ALU.mult), reads=[Bmean], writes=[Bvar])
        P.op("dve", lambda e: e.scalar_tensor_tensor(out=var, in0=pb[:], scalar=1.0 / D, in1=var, op0=ALU.mult, op1=ALU.subtract),
             reads=[Bpb, Bvar], writes=[Bvar])
        P.op("dve", lambda e: e.tensor_scalar(out=var, in0=var, scalar1=EPS, scalar2=None, op0=ALU.add), reads=[Bvar], writes=[Bvar])
        P.op("act", lambda e: e.activation(out=var, in_=var, func=AF.Sqrt), reads=[Bvar], writes=[Bvar])
        P.op("dve", lambda e: e.reciprocal(out=rstd, in_=var), reads=[Bvar], writes=[Brstd])
        for m in range(NCH):
            zz = z(m, h)
            P.op("pool", lambda e, zz=zz: e.tensor_tensor(out=zz, in0=zz, in1=mean, op=ALU.subtract),
                 reads=[Bz[m][h], Bmean], writes=[Bz[m][h]])
            P.op("dve", lambda e, zz=zz: e.tensor_tensor(out=zz, in0=zz, in1=rstd, op=ALU.mult),
                 reads=[Bz[m][h], Brstd], writes=[Bz[m][h]])
            for o in outs:
                if o[0] == "bf16":
                    _, off, Bo, sc, bi = o
                    dst = C.bf(off + (m * NT + h * 512) * 2, 512)
                    P.op("act", lambda e, zz=zz, dst=dst, m=m, sc=sc, bi=bi: e.activation(out=dst, in_=zz, func=AF.Identity, scale=sc(m), bias=bi(m)),
                         reads=[Bz[m][h]], writes=[Bo[m][h]])
            for o in outs:
                if o[0] == "f32":
                    _, sc, bi = o
                    P.op("act", lambda e, zz=zz, m=m, sc=sc, bi=bi: e.activation(out=zz, in_=zz, func=AF.Identity, scale=sc(m), bias=bi(m)),
                         reads=[Bz[m][h]], writes=[Bz[m][h]])


def phase_R(C, dr, Bz, Bot):
    P = C.P
    nc = C.nc
    ws = WStream(C)
    z = lambda m, h: C.f32(OFF_ACC + (m * NT + h * 512) * 4, 512)
    ot = lambda k, h: C.bf(OFF_OT + (k * NT + h * 512) * 2, 512)
    xb = lambda k, h: C.bf(OFF_XB + (k * NT + h * 512) * 2, 512)
    Bxb = [[Buf("xb%d_%d" % (m, h)) for h in range(2)] for m in range(NCH)]
    lnp = C.lnp
    col = lambda w: (lambda m: lnp[:, w * NCH + m: w * NCH + m + 1])

    for m in range(NCH):
        wr, bwr = ws.push(dr["woutp"][m])
        for h in range(2):
            pt, bpt = C.ps[(m * 2 + h) % 4], C.Bps[(m * 2 + h) % 4]
            for k in range(NCH):
                P.op("pe", lambda e, pt=pt, wr=wr, k=k, h=h: e.matmul(pt[:], lhsT=wr[:, k * 128:(k + 1) * 128], rhs=ot(k, h),
                                                                  start=(k == 0), stop=(k == NCH - 1)),
                     reads=[bwr, Bot[k][h]], writes=[bpt])
            P.op("dve", lambda e, pt=pt, m=m, h=h: e.scalar_tensor_tensor(out=z(m, h), in0=z(m, h), scalar=ALPHA, in1=pt[:],
                                                                        op0=ALU.mult, op1=ALU.add),
                 reads=[bpt, Bz[m][h]], writes=[Bz[m][h]])
    if getattr(C, 'stop', None) == 'R1':
        return store_z(C, dr, Bz)
    layer_norm_fm(C, OFF_ACC, Bz, None, None,
                  [("bf16", OFF_XB, Bxb, col(0), col(1)), ("f32", col(4), col(5))], OFF_MISC + 8 * KB)

    if getattr(C, 'stop', None) == 'R2':
        return store_z(C, dr, Bz)
    MO = OFF_OT
    P.barrier()
    wrt = C.f32(MO, NCH * 36)
    Bwrt = Buf("wrt")
    rbb = C.f32(MO + 4 * KB, 36)
    Brbb = Buf("rbb")
    P.dma("sp", lambda e: e.dma_start(out=wrt, in_=dr["wr"]), writes=[Bwrt])
    P.dma("sp", lambda e: e.dma_start(out=rbb, in_=dr["rb"].broadcast_to([128, 36])), writes=[Brbb])
    lg = C.f32(MO + 5 * KB, 8 * 36)
    Blg = Buf("lg")
    comb = C.f32(MO + 7 * KB, 8 * 32)
    Bcomb = Buf("comb")
    sc = C.f32(MO + 9 * KB, 64)
    Bsc = Buf("sc")
    tmp32 = C.f32(MO + 10 * KB, 64)
    Btmp = Buf("tmp32")
    prt, bprt = C.ps[4], C.Bps[4]
    for tt in range(8):
        h, o = tt // 4, (tt % 4) * 128
        for m in range(NCH):
            P.op("pe", lambda e, tt=tt, m=m, h=h, o=o: e.matmul(prt[:, tt * 36:(tt + 1) * 36], lhsT=z(m, h)[:, o:o + 128],
                                                              rhs=wrt[:, m * 36:(m + 1) * 36], start=(m == 0), stop=(m == NCH - 1)),
                 reads=[Bz[m][h], Bwrt], writes=[bprt])
    for tt in range(8):
        l = lg[:, tt * 36:(tt + 1) * 36]
        P.op("dve", lambda e, tt=tt, l=l: e.scalar_tensor_tensor(out=l, in0=prt[:, tt * 36:(tt + 1) * 36], scalar=1.0 / ALPHA, in1=rbb,
                                                               op0=ALU.mult, op1=ALU.add), reads=[bprt, Brbb], writes=[Blg])
        gl = lg[:, tt * 36: tt * 36 + 4]
        el = lg[:, tt * 36 + 4: tt * 36 + 36]
        s = lambda i: sc[:, i:i + 1]
        cm = comb[:, tt * 32:(tt + 1) * 32]
        t32 = tmp32[:, 0:32]
        t4 = tmp32[:, 32:36]
        t4b = tmp32[:, 36:40]
        V = lambda fn, r=(Blg, Bsc, Btmp, Bcomb), w=(Blg, Bsc, Btmp, Bcomb): P.op("dve", fn, reads=list(r), writes=list(w))
        A = lambda fn: P.op("act", fn, reads=[Blg, Bsc, Btmp, Bcomb], writes=[Blg, Bsc, Btmp, Bcomb])
        V(lambda e, gl=gl: e.reduce_max(out=s(0), in_=gl, axis=AX.X))
        V(lambda e, gl=gl: e.tensor_scalar(out=t4, in0=gl, scalar1=s(0), scalar2=None, op0=ALU.subtract))
        A(lambda e: e.activation(out=t4b, in_=t4, func=AF.Exp))
        V(lambda e: e.reduce_sum(out=s(1), in_=t4b, axis=AX.X))
        V(lambda e: e.reciprocal(out=s(1), in_=s(1)))
        V(lambda e: e.tensor_scalar(out=t4, in0=t4, scalar1=0.0, scalar2=-BIG, op0=ALU.is_lt, op1=ALU.mult))
        for g in range(4):
            V(lambda e, g=g, el=el: e.tensor_scalar(out=el[:, g * 8:(g + 1) * 8], in0=el[:, g * 8:(g + 1) * 8],
                                                   scalar1=t4[:, g:g + 1], scalar2=None, op0=ALU.add))
        V(lambda e, el=el: e.reduce_max(out=s(2), in_=el, axis=AX.X))
        V(lambda e, el=el: e.tensor_scalar(out=t32, in0=el, scalar1=s(2), scalar2=None, op0=ALU.is_equal))
        V(lambda e, el=el: e.scalar_tensor_tensor(out=el, in0=t32, scalar=-BIG, in1=el, op0=ALU.mult, op1=ALU.add))
        V(lambda e, el=el: e.reduce_max(out=s(3), in_=el, axis=AX.X))
        V(lambda e: e.tensor_tensor(out=s(4), in0=s(3), in1=s(2), op=ALU.subtract))
        A(lambda e: e.activation(out=s(4), in_=s(4), func=AF.Exp))
        V(lambda e: e.tensor_scalar(out=s(4), in0=s(4), scalar1=1.0, scalar2=None, op0=ALU.add))
        V(lambda e: e.reciprocal(out=s(5), in_=s(4)))
        V(lambda e: e.tensor_scalar(out=s(6), in0=s(5), scalar1=-1.0, scalar2=1.0, op0=ALU.mult, op1=ALU.add))
        V(lambda e: e.tensor_tensor(out=s(5), in0=s(5), in1=s(1), op=ALU.mult))
        V(lambda e: e.tensor_tensor(out=s(6), in0=s(6), in1=s(1), op=ALU.mult))
        V(lambda e, cm=cm: e.tensor_scalar(out=cm, in0=t32, scalar1=s(5), scalar2=None, op0=ALU.mult))
        V(lambda e, el=el: e.tensor_scalar(out=t32, in0=el, scalar1=s(3), scalar2=None, op0=ALU.is_equal))
        V(lambda e, cm=cm: e.scalar_tensor_tensor(out=cm, in0=t32, scalar=s(6), in1=cm, op0=ALU.mult, op1=ALU.add))
    combT = C.f32(MO + 12 * KB, 1024)
    BcombT = Buf("combT")
    pct, bpct = C.ps[5], C.Bps[5]
    for tt in range(8):
        hh, o = tt // 4, (tt % 4) * 128
        P.op("pe", lambda e, tt=tt, o=o: e.transpose(pct[0:32, o:o + 128], comb[:, tt * 32:(tt + 1) * 32], C.ident32),
             reads=[Bcomb, C.Bident], writes=[bpct])
        if tt % 4 == 3:
            P.op("dve", lambda e, hh=hh: e.tensor_copy(out=combT[0:32, hh * 512:(hh + 1) * 512], in_=pct[0:32, :]),
                 reads=[bpct], writes=[BcombT])
    Bcwd = Buf("cwd")
    P.dma("sp", lambda e: e.dma_start(out=dr["cwd"], in_=combT[0:32, :]), reads=[BcombT], writes=[Bcwd])

    if getattr(C, 'stop', None) == 'R3':
        P.dma('sp', lambda e: e.dma_start(out=dr['outT'][0:32, :], in_=combT[0:32, :]), reads=[BcombT], sembuf=C.Bout)
        P.dma('sp', lambda e: e.dma_start(out=dr['outT'][128:256, 0:288], in_=lg), reads=[Blg], sembuf=C.Bout)
        P.dma('sp', lambda e: e.dma_start(out=dr['outT'][256:384, 0:64], in_=sc), reads=[Bsc], sembuf=C.Bout)
        P.dma('sp', lambda e: e.dma_start(out=dr['outT'][512:640, 0:36], in_=rbb), reads=[Brbb], sembuf=C.Bout)
        P.dma('sp', lambda e: e.dma_start(out=dr['outT'][640:768, 0:576], in_=wrt), reads=[Bwrt], sembuf=C.Bout)
        P.op('dve', lambda e: e.tensor_copy(out=tmp32[:, 0:36], in_=prt[:, 0:36]), reads=[bprt], writes=[Btmp])
        P.dma('sp', lambda e: e.dma_start(out=dr['outT'][768:896, 0:36], in_=tmp32[:, 0:36]), reads=[Btmp], sembuf=C.Bout)
        P.dma('sp', lambda e: e.dma_start(out=dr['outT'][384:512, 0:256], in_=comb), reads=[Bcomb], sembuf=C.Bout)
        return
    cwb = [C.f32(MO + 16 * KB + i * 4 * KB, 1024) for i in range(2)]
    Bcwb = [Buf("cwb0"), Buf("cwb1")]
    hT = [[C.bf(MO + (24 if i == 0 else 0) * KB + fc * 2 * KB, 1024) for fc in range(4)] for i in range(2)]
    BhT = [[Buf("h%d_%d" % (i, fc)) for fc in range(4)] for i in range(2)]
    sg = [C.f32(OFF_MISC + i * 2 * KB, 512) for i in range(2)]
    Bsg = [Buf("sg0"), Buf("sg1")]
    tu = [C.f32(OFF_MISC + 4 * KB + i * 2 * KB, 512) for i in range(2)]
    Btu = [Buf("tu0"), Buf("tu1")]
    dead = [Bwrt, Brbb, Blg, Bcomb, Bsc, Btmp, BcombT]
    st8 = dict(cnt=0, dcnt=0, first1=True)
    exl = list(C.expert_list if getattr(C, 'expert_list', None) is not None else range(NEXP))

    def load_cw(ei):
        ex = exl[ei]
        cw, bcw = cwb[ei % 2], Bcwb[ei % 2]
        P.dma("pool", lambda e, cw=cw, ex=ex: e.dma_start(out=cw, in_=dr["cwd"][ex:ex + 1, :].broadcast_to([128, 1024])),
              reads=[Bcwd], writes=[bcw])

    def gu_seg(ei, fc, wts):
        ex = exl[ei]
        cw, bcw = cwb[ei % 2], Bcwb[ei % 2]
        hset, Bhset = hT[ei % 2], BhT[ei % 2]
        (wg, bwg), (wu, bwu) = wts
        for h in range(2):
            cnt = st8["cnt"]
            pg, bpg = C.ps[(cnt % 2) * 2], C.Bps[(cnt % 2) * 2]
            pu, bpu = C.ps[(cnt % 2) * 2 + 1], C.Bps[(cnt % 2) * 2 + 1]
            sgi, bsgi = sg[cnt % 2], Bsg[cnt % 2]
            tui, btui = tu[cnt % 2], Btu[cnt % 2]
            st8["cnt"] = cnt + 1
            for k in range(NCH):
                P.op("pe", lambda e, pg=pg, wg=wg, k=k, h=h: e.matmul(pg[:], lhsT=wg[:, k * 128:(k + 1) * 128], rhs=xb(k, h),
                                                                  start=(k == 0), stop=(k == NCH - 1)),
                     reads=[bwg, Bxb[k][h]], writes=[bpg])
            for k in range(NCH):
                P.op("pe", lambda e, pu=pu, wu=wu, k=k, h=h: e.matmul(pu[:], lhsT=wu[:, k * 128:(k + 1) * 128], rhs=xb(k, h),
                                                                  start=(k == 0), stop=(k == NCH - 1)),
                     reads=[bwu, Bxb[k][h]], writes=[bpu])
            P.op("act", lambda e, pg=pg, sgi=sgi: e.activation(out=sgi, in_=pg[:], func=AF.Silu), reads=[bpg], writes=[bsgi])
            P.op("dve", lambda e, pu=pu, tui=tui, cw=cw, h=h: e.tensor_tensor(out=tui, in0=pu[:], in1=cw[:, h * 512:(h + 1) * 512], op=ALU.mult),
                 reads=[bpu, bcw], writes=[btui])
            hh = hset[fc][:, h * 512:(h + 1) * 512]
            extra = []
            if ei % 2 == 1 and st8["first1"]:
                extra = dead
            P.op("dve", lambda e, hh=hh, sgi=sgi, tui=tui: e.tensor_tensor(out=hh, in0=sgi, in1=tui, op=ALU.mult),
                 reads=[bsgi, btui], writes=[Bhset[fc]] + extra)
        if ei % 2 == 1 and fc == 3:
            st8["first1"] = False

    def dn(ei, wd):
        ex = exl[ei]
        hset, Bhset = hT[ei % 2], BhT[ei % 2]
        for m in range(NCH):
            for h in range(2):
                dcnt = st8["dcnt"]
                pd, bpd = C.ps[4 + dcnt % 2], C.Bps[4 + dcnt % 2]
                st8["dcnt"] = dcnt + 1
                for fc in range(4):
                    P.op("pe", lambda e, pd=pd, fc=fc, m=m, h=h, w=wd[fc][0], hset=hset: e.matmul(pd[:], lhsT=w[:, m * 128:(m + 1) * 128],
                                                                                           rhs=hset[fc][:, h * 512:(h + 1) * 512],
                                                                                           start=(fc == 0), stop=(fc == 3)),
                         reads=[wd[fc][1], Bhset[fc]], writes=[bpd])
                P.op("dve", lambda e, pd=pd, m=m, h=h: e.tensor_tensor(out=z(m, h), in0=z(m, h), in1=pd[:], op=ALU.add),
                     reads=[bpd, Bz[m][h]], writes=[Bz[m][h]])

    segs = []
    if exl:
        segs += [("gu", 0, fc) for fc in range(4)]
    for ei in range(len(exl)):
        nxt = ei + 1 < len(exl)
        if nxt:
            segs.append(("gu", ei + 1, 0))
        segs.append(("dn", ei, None))
        if nxt:
            segs += [("gu", ei + 1, fc) for fc in range(1, 4)]
    pushed = {}

    def ensure(si):
        if si >= len(segs) or si in pushed:
            return
        kind, ei, fc = segs[si]
        ex = exl[ei]
        if kind == "gu":
            if fc == 0:
                load_cw(ei)
            pushed[si] = [ws.push(dr["moew"][ex, 2 * fc]), ws.push(dr["moew"][ex, 2 * fc + 1])]
        else:
            pushed[si] = [ws.push(dr["moew"][ex, 8 + f_]) for f_ in range(4)]

    for si, (kind, ei, fc) in enumerate(segs):
        ensure(si)
        ensure(si + 1)
        if kind == "gu":
            gu_seg(ei, fc, pushed[si])
        else:
            dn(ei, pushed[si])
        del pushed[si]
    if getattr(C, 'stop', None) == 'R4':
        return store_z(C, dr, Bz)
    layer_norm_fm(C, OFF_ACC, Bz, None, None, [("f32", col(2), col(3))], OFF_MISC + 8 * KB)
    store_z(C, dr, Bz)


def store_z(C, dr, Bz):
    for m in range(NCH):
        C.P.dma("sp", lambda e, m=m: e.dma_start(out=dr["outT"][m * 128:(m + 1) * 128, :], in_=C.f32(OFF_ACC + m * NT * 4, NT)),
                reads=[Bz[m][0], Bz[m][1]], sembuf=C.Bout)


def setup_consts(C, dr):
    P = C.P
    base = 202 * KB
    C.ones32 = C.f32(base, 128)
    C.ident32 = C.f32(base + 512, 128)
    C.lnp = C.f32(base + 1024, 6 * NCH)
    C.Bones, C.Bident, C.Blnp, C.Bout = Buf("ones"), Buf("ident", persist=True), Buf("lnp", persist=True), Buf("out", persist=True)
    P.op("dve", lambda e: e.memset(C.ones32, 1.0), writes=[C.Bones])
    P.dma("sp", lambda e: e.dma_start(out=C.ident32, in_=dr["ident"]), writes=[C.Bident])
    if "lnp" in dr:
        load_lnp(C, dr)


def load_lnp(C, dr):
    P = C.P
    P.dma("sp", lambda e: e.dma_start(out=C.lnp[:, 0:4 * NCH], in_=dr["lnp"]), writes=[C.Blnp])
    P.op("dve", lambda e: e.tensor_scalar(out=C.lnp[:, 4 * NCH:6 * NCH], in0=C.lnp[:, 0:2 * NCH], scalar1=ALPHA, scalar2=None, op0=ALU.mult),
         reads=[C.Blnp], writes=[C.Blnp])


def declare_R_dram(nc):
    dr = {}
    dr["xT"] = nc.dram_tensor("xT", [D, NT], F32, kind="ExternalInput").ap()
    dr["woutp"] = nc.dram_tensor("woutp", [NCH, 128, 2048], F32, kind="ExternalInput").ap()
    dr["moew"] = nc.dram_tensor("moew", [NEXP, 12, 128, 2048], F32, kind="ExternalInput").ap()
    dr["wr"] = nc.dram_tensor("wr", [128, NCH * 36], F32, kind="ExternalInput").ap()
    dr["rb"] = nc.dram_tensor("rb", [1, 36], F32, kind="ExternalInput").ap()
    dr["lnp"] = nc.dram_tensor("lnp", [128, 4 * NCH], F32, kind="ExternalInput").ap()
    dr["ident"] = nc.dram_tensor("ident", [128, 128], F32, kind="ExternalInput").ap()
    dr["cwd"] = nc.dram_tensor("cwd", [NEXP, NT], F32, kind="Internal").ap()
    dr["outT"] = nc.dram_tensor("outT", [D, NT], F32, kind="ExternalOutput").ap()
    return dr


def load_z(C, dr):
    Bz = [[Buf("z%d_%d" % (m, h)) for h in range(2)] for m in range(NCH)]
    for m in range(NCH):
        C.P.dma("sp", lambda e, m=m: e.dma_start(out=C.f32(OFF_ACC + m * NT * 4, NT), in_=dr["xT"][m * 128:(m + 1) * 128, :]),
                writes=[Bz[m][0], Bz[m][1]], sembuf=Bz[m][0])
    return Bz


def host_R_inputs(layer, w_out, ln_mix_g, ln_mix_b, ln_ffn_g, ln_ffn_b, moe_group_w, moe_group_b, moe_expert_w,
                  moe_expert_b, moe_w_gate, moe_w_up, moe_w_down):
    f = np.float32
    woutp = np.ascontiguousarray(w_out.reshape(NCH, 128, NCH, 128).transpose(2, 1, 0, 3).reshape(NCH, 128, 2048), dtype=f)
    wg = moe_w_gate[layer].reshape(NEXP, NCH, 128, 4, 128).transpose(0, 3, 2, 1, 4).reshape(NEXP, 4, 128, 2048)
    wu = moe_w_up[layer].reshape(NEXP, NCH, 128, 4, 128).transpose(0, 3, 2, 1, 4).reshape(NEXP, 4, 128, 2048)
    wd = moe_w_down[layer].reshape(NEXP, 4, 128, 2048)
    moew = np.empty((NEXP, 12, 128, 2048), dtype=f)
    moew[:, 0:8:2] = wg
    moew[:, 1:8:2] = wu
    moew[:, 8:12] = wd
    wr_full = np.concatenate([moe_group_w[layer], moe_expert_w[layer].transpose(1, 0, 2).reshape(D, 32)], axis=1)
    wr = np.ascontiguousarray(wr_full.reshape(NCH, 128, 36).transpose(1, 0, 2).reshape(128, NCH * 36), dtype=f)
    rb = np.concatenate([moe_group_b[layer], moe_expert_b[layer].reshape(32)])[None, :].astype(f)
    lnp = np.stack([ln_mix_g[layer], ln_mix_b[layer], ln_ffn_g[layer], ln_ffn_b[layer]], 0)
    lnp = np.ascontiguousarray(lnp.reshape(4, NCH, 128).transpose(2, 0, 1).reshape(128, 4 * NCH), dtype=f)
    return dict(woutp=woutp, moew=moew, wr=wr, rb=rb, lnp=lnp, ident=np.eye(128, dtype=f))


NBLK = 8
NTILE = 32
NSLOT = 8


class Stager:
    def __init__(self, C, tag):
        self.C = C
        self.stage = [C.f32(OFF_STAGE + i * 8 * KB, 2048) for i in range(NSTAGE)]
        self.B = [Buf("%s_st%d" % (tag, i)) for i in range(NSTAGE)]
        self.n = 0

    def load_cast(self, src, dst, Bdst, n, parts=128, q="sp"):
        P = self.C.P
        assert n <= 2048
        i = self.n
        self.n += 1
        s = i % NSTAGE
        st, bst = self.stage[s][0:parts, 0:n], self.B[s]
        P.dma(q, lambda e: e.dma_start(out=st, in_=src), writes=[bst])
        if i % 2 == 0:
            P.op("dve", lambda e: e.tensor_copy(out=dst, in_=st), reads=[bst], writes=[Bdst])
        else:
            P.op("act", lambda e: e.activation(out=dst, in_=st, func=AF.Copy), reads=[bst], writes=[Bdst])

    def load_w(self, src, dst_fn, Bdst, ntot, parts=128):
        for c0 in range(0, ntot, 2048):
            n = min(2048, ntot - c0)
            self.load_cast(src[:, c0:c0 + n], dst_fn(c0, n), Bdst, n, parts)


def make_xbf_scratch(C, dr, stg, Bx_prev=None):
    P = C.P
    tmp = [C.bf(i * 4 * KB, 2048) for i in range(4)]
    Bt = [Buf("xc%d" % i) for i in range(4)]
    Bx = Buf("xTb") if Bx_prev is None else Bx_prev
    Bxo = Buf("xTob")
    n = 0
    jobs = ((dr["xTfull"], dr["xTb"], S, Bx), (dr["xT"], dr["xTob"], NT, Bxo))
    if Bx_prev is not None:
        jobs = jobs[1:]
    for (src, dst, ncol, B) in jobs:
        for k in range(NCH):
            for c0 in range(0, ncol, 2048):
                w = min(2048, ncol - c0)
                t, bt = tmp[n % 4][:, 0:w], Bt[n % 4]
                n += 1
                stg.load_cast(src[k * 128:(k + 1) * 128, c0:c0 + w], t, bt, w)
                P.dma("pool", lambda e, t=t, dst=dst, k=k, c0=c0, w=w: e.dma_start(out=dst[k * 128:(k + 1) * 128, c0:c0 + w], in_=t),
                      reads=[bt], writes=[B], sembuf=bt)
    return Bx, Bxo


def load_xblk(C, src, Bsrc, c0, ncol, dst_off, Bdst, q="sp"):
    dst = C.bf(dst_off, NCH * ncol).rearrange("p (k n) -> p k n", k=NCH)
    s = src[:, c0:c0 + ncol].rearrange("(k p) n -> p k n", p=128)
    for g in range(4):
        C.P.dma(q, lambda e, g=g: e.dma_start(out=dst[:, g * 4:(g + 1) * 4, :], in_=s[:, g * 4:(g + 1) * 4, :]), reads=[Bsrc], writes=[Bdst])
    return lambda k, a=0, b=None: C.bf(dst_off + (k * ncol + a) * 2, (ncol if b is None else b) - a)


def proj_fm(C, ps_ap, Bps, w_ap, Bw, xk, Bx, M=128):
    for k in range(NCH):
        xa = xk(k)
        C.P.op("pe", lambda e, k=k, xa=xa: e.matmul(ps_ap, lhsT=w_ap[:, k * M:(k + 1) * M], rhs=xa, start=(k == 0), stop=(k == NCH - 1)),
               reads=[Bw, Bx], writes=[Bps])


def proj_tm(C, ps_ap, Bps, w_ap, Bw, xk, Bx, ncols):
    for k in range(NCH):
        xa = xk(k)
        C.P.op("pe", lambda e, k=k, xa=xa: e.matmul(ps_ap, lhsT=xa, rhs=w_ap[:, k * ncols:(k + 1) * ncols], start=(k == 0), stop=(k == NCH - 1)),
               reads=[Bw, Bx], writes=[Bps])


def setup_mix_consts(C, dr, names):
    P = C.P
    out = {}
    off = OFF_MISC
    for (nm, n, dt_) in names:
        if dt_ == "f32":
            ap = C.f32(off, n)
            off += n * 4
        else:
            ap = C.bf(off, n)
            off += n * 2
        off = (off + 3) // 4 * 4
        b = Buf(nm)
        P.dma("sp", lambda e, ap=ap, nm=nm: e.dma_start(out=ap, in_=dr[nm]), writes=[b])
        out[nm] = (ap, b)
    assert off <= 202 * KB, off
    return out


def phase_M0(C, dr, Bx_prev=None, vq=None):
    P = C.P
    cached = vq is not None and vq > 0
    store = vq == 0
    nc = C.nc
    stg = Stager(C, "m0")
    Bot = [[Buf("ot%d_%d" % (k, h)) for h in range(2)] for k in range(NCH)]
    otslot = lambda ch, u: C.bf(OFF_OT + (ch * NT + u * 128) * 2, 128)
    K = setup_mix_consts(C, dr, [("triL32", 128, "f32"), ("triU32", 128, "f32"), ("cind32", 2, "f32"), ("sel64", 128, "f32"),
                                 ("glamask", 128, "bf16"), ("selt", 32, "f32"), ("pent", 32, "f32"), ("dmask", 32 * 128, "bf16"),
                                 ("onesbf", 128, "bf16"), ("g_ng", 2, "f32"), ("f_gb", 8, "f32"), ("triF32", 128, "f32")])
    Bx, Bxo = make_xbf_scratch(C, dr, stg, Bx_prev)
    C.last_Bx = Bx
    P.barrier()
    ones32, Bones = C.ones32, C.Bones
    stop = getattr(C, 'stop', None)
    if stop == 'A':
        return Bot
    A0 = 0
    for hd in (range(4) if not getattr(C, 'skip_gla', False) else []):
        P.barrier()
        wq = C.bf(A0, 2048); wk = C.bf(A0 + 4 * KB, 2048); wkv = C.bf(A0 + 8 * KB, 6144)
        wgr = [C.bf(A0 + 20 * KB + i * 4 * KB, 2048) for i in range(2)]
        wglr = C.bf(A0 + 28 * KB, 256)
        w2 = C.bf(A0 + 29 * KB, 128, parts=17)
        Bw = Buf("gw")
        stg.load_w(dr["g_wq"][hd], lambda c0, n: wq[:, c0:c0 + n], Bw, 2048)
        stg.load_w(dr["g_wk"][hd], lambda c0, n: wk[:, c0:c0 + n], Bw, 2048)
        stg.load_w(dr["g_wkv"][hd], lambda c0, n: wkv[:, c0:c0 + n], Bw, 6144)
        for i in range(2):
            stg.load_w(dr["g_wgr"][hd, i], lambda c0, n, i=i: wgr[i][:, c0:c0 + n], Bw, 2048)
        stg.load_w(dr["g_wglr"], lambda c0, n: wglr[:, c0:c0 + n], Bw, 256)
        stg.load_cast(dr["g_w2"][hd], w2, Bw, 128, parts=17)
        if stop == 'G0a':
            return Bot
        XO = A0 + 32 * KB
        Bxb = [Buf("xblk0"), Buf("xblk1")]
        T0 = A0 + 64 * KB
        glrT = C.bf(T0, 512, parts=17); BglrT = Buf("glrT")
        kv = C.bf(T0 + 1 * KB, 384); Bkv = Buf("kv")
        e1 = C.f32(T0 + 2 * KB, 128); Be1 = Buf("e1")
        lap = C.f32(T0 + 3 * KB, 128); Blap = Buf("lap")
        ek = C.f32(T0 + 4 * KB, 128); Bek = Buf("ek")
        kend = C.bf(T0 + 5 * KB, 256); Bkend = Buf("kend")
        dec = C.f32(T0 + 6 * KB, 2); Bdec = Buf("dec")
        Sst = C.f32(T0 + 7 * KB, 256); BS = Buf("S")
        Ssel = [C.f32(T0 + 8 * KB + u * KB, 256) for u in range(NSLOT)]; BSsel = [Buf("Ssel%d" % u) for u in range(NSLOT)]
        selt, Bselt = K["selt"]
        P.op("pool", lambda e: e.memset(glrT[0:17, :], 1.0), writes=[BglrT])
        P.op("dve", lambda e: e.memset(Sst, 0.0), writes=[BS])
        for u in range(NSLOT):
            P.op("dve", lambda e, u=u: e.memset(Ssel[u], 0.0), writes=[BSsel[u]])
        p_kv, b_kv = C.ps[0], C.Bps[0]
        p_gl, b_gl = C.ps[1], C.Bps[1]
        p_m, b_m = C.ps[2], C.Bps[2]
        p_cs, b_cs = C.ps[3], C.Bps[3]

        def gate_and_la(xk, Bxk, t0, w, with_kv=True, pe_part=None, act_part=None):
            xs = lambda k: xk(k, t0, t0 + 128)
            proj_tm(C, p_kv[:, 0:384], b_kv, wkv, Bw, xs, Bxk, 384)
            if pe_part is not None:
                pe_part()
            P.op("act", lambda e: e.activation(out=kv, in_=p_kv[:, 0:384], func=AF.Copy), reads=[b_kv], writes=[Bkv])
            if act_part is not None:
                act_part()
            P.op("pe", lambda e: e.matmul(p_m[:, 0:128], lhsT=glrT[0:17, t0:t0 + 128], rhs=w2[0:17, :], start=True, stop=True),
                 reads=[BglrT, Bw], writes=[b_m])
            P.op("act", lambda e: e.activation(out=e1, in_=p_m[:, 0:128], func=AF.Exp, scale=-1.0), reads=[b_m], writes=[Be1])
            P.op("act", lambda e: e.activation(out=lap, in_=e1, func=AF.Ln, bias=1.0), reads=[Be1], writes=[Blap])

        def glr_block(xk, Bxk, ntok):
            proj_fm(C, p_gl[0:16, 0:ntok], b_gl, wglr, Bw, lambda k: xk(k), Bxk, M=16)
            P.op("dve", lambda e: e.tensor_copy(out=glrT[0:16, 0:ntok], in_=p_gl[0:16, 0:ntok]), reads=[b_gl], writes=[BglrT])

        def state_steps(upd_sel_tile=None):
            triU, BtriU = K["triU32"]
            cind, Bcind = K["cind32"]
            P.op("pe", lambda e: e.matmul(p_m[:, 128:256], lhsT=triU, rhs=lap, start=True, stop=True), reads=[BtriU, Blap], writes=[b_m])
            P.op("act", lambda e: e.activation(out=ek, in_=p_m[:, 128:256], func=AF.Exp, scale=-1.0 / 16), reads=[b_m], writes=[Bek])
            if stop == 'G0d1':
                return
            P.op("pe", lambda e: e.matmul(p_m[:, 256:258], lhsT=lap, rhs=cind, start=True, stop=True), reads=[Bcind, Blap], writes=[b_m])
            P.op("act", lambda e: e.activation(out=dec, in_=p_m[:, 256:258], func=AF.Exp, scale=-1.0 / 16), reads=[b_m], writes=[Bdec])
            if stop == 'G0d2':
                return
            for c in range(2):
                P.op("dve", lambda e, c=c: e.scalar_tensor_tensor(out=kend[:, c * 128:(c + 1) * 128], in0=kv[:, 0:128], scalar=cind[:, c:c + 1], in1=ek,
                                                                 op0=ALU.mult, op1=ALU.mult), reads=[Bkv, Bek, Bcind], writes=[Bkend])
            for c in range(2):
                P.op("pe", lambda e, c=c: e.matmul(p_cs[:, c * 256:(c + 1) * 256], lhsT=kend[:, c * 128:(c + 1) * 128], rhs=kv[:, 128:384],
                                                   start=True, stop=True), reads=[Bkend, Bkv], writes=[b_cs])

        snapb = [C.f32(T0 + 26 * KB + i * KB, 256) for i in range(4)]
        Bsn = [Buf("snapb%d" % i) for i in range(4)]
        Bsnapd = Buf("snapd")
        if cached:
            ot_ = own_tiles(vq)
            for u in range(NSLOT):
                P.dma("sp", lambda e, u=u, tl=ot_[u], hd=hd, Ssel=Ssel: e.dma_start(out=Ssel[u], in_=dr["snap"][hd * NTILE + tl]), writes=[BSsel[u]])
        for blk in (range(NBLK) if not cached else []):
            xk = load_xblk(C, dr["xTb"], Bx, blk * 512, 512, XO + (blk % 2) * 16 * KB, Bxb[blk % 2])
            if stop == 'G0b0':
                return Bot
            glr_block(xk, Bxb[blk % 2], 512)
            if stop == 'G0b':
                return Bot
            for tt in range(4):
                tile_i = blk * 4 + tt
                gate_and_la(xk, Bxb[blk % 2], tt * 128, None)
                if stop == 'G0c':
                    return Bot
                state_steps()
                if stop and stop.startswith('G0d'):
                    return Bot
                u = tile_i // 4
                P.op("dve", lambda e, u=u, tile_i=tile_i: e.scalar_tensor_tensor(out=Ssel[u], in0=Sst, scalar=selt[:, tile_i:tile_i + 1], in1=Ssel[u],
                                                                               op0=ALU.mult, op1=ALU.add), reads=[BS, Bselt, BSsel[u]], writes=[BSsel[u]])
                if store:
                    sn, bsn = snapb[tile_i % 4], Bsn[tile_i % 4]
                    P.op("act", lambda e, sn=sn: e.activation(out=sn, in_=Sst, func=AF.Copy), reads=[BS], writes=[bsn])
                    P.dma("pool", lambda e, sn=sn, tile_i=tile_i, hd=hd: e.dma_start(out=dr["snap"][hd * NTILE + tile_i], in_=sn), reads=[bsn], writes=[Bsnapd], sembuf=bsn)
                for c in range(2):
                    P.op("dve", lambda e, c=c: e.scalar_tensor_tensor(out=Sst, in0=Sst, scalar=dec[:, c:c + 1], in1=p_cs[:, c * 256:(c + 1) * 256],
                                                                     op0=ALU.mult, op1=ALU.add), reads=[BS, Bdec, b_cs], writes=[BS])
        if stop == 'G1':
            return Bot
        O0 = T0 + 16 * KB
        eb = C.f32(O0, 128); enb = C.f32(O0 + 512, 128); Beb = Buf("eb")
        qd = C.bf(O0 + 1 * KB, 128); kin = C.bf(O0 + 1 * KB + 256, 128); Bqk = Buf("qk")
        sT = C.bf(O0 + 1 * KB + 512, 128); BsT = Buf("sT")
        Sa = C.bf(O0 + 2 * KB, 256); Sb32 = C.f32(O0 + 3 * KB, 256); Sb = C.bf(O0 + 4 * KB, 256); BSab = Buf("Sab")
        sq = [C.f32(O0 + 5 * KB + i * 512, 128) for i in range(2)]; Bsq = Buf("gsq")
        rs = C.f32(O0 + 6 * KB, 128); Brs = Buf("grs")
        sgr = [C.f32(O0 + 7 * KB + i * 512, 128) for i in range(2)]; Bsgr = Buf("sgr")
        tt_ = C.f32(O0 + 8 * KB, 128); Btt = Buf("gtt")
        triL, BtriL = K["triL32"]
        gmask, Bgmask = K["glamask"]
        ng, Bng = K["g_ng"]
        p_q, b_q = C.ps[4], C.Bps[4]
        p_k, b_k = C.ps[5], C.Bps[5]
        p_o, b_o = C.ps[6], C.Bps[6]
        p_g, b_g = C.ps[7], C.Bps[7]
        for half in range(2):
            xk = load_xblk(C, dr["xTob"], Bxo, half * 512, 512, XO + half * 16 * KB, Bxb[half])
            glr_block(xk, Bxb[half], 512)
            for tt in range(4):
                u = half * 4 + tt
                t0 = tt * 128
                xs = lambda k, xk=xk, t0=t0: xk(k, t0, t0 + 128)

                def pe_part(xs=xs, half=half):
                    proj_fm(C, p_q[:, 0:128], b_q, wq, Bw, xs, Bxb[half])
                    proj_fm(C, p_k[:, 0:128], b_k, wk, Bw, xs, Bxb[half])
                    for vc in range(2):
                        proj_fm(C, p_g[:, vc * 128:(vc + 1) * 128], b_g, wgr[vc], Bw, xs, Bxb[half])

                def act_part():
                    for vc in range(2):
                        P.op("act", lambda e, vc=vc: e.activation(out=sgr[vc], in_=p_g[:, vc * 128:(vc + 1) * 128], func=AF.Silu), reads=[b_g], writes=[Bsgr])
                gate_and_la(xk, Bxb[half], t0, None, pe_part=pe_part, act_part=act_part)
                state_steps()
                P.op("pe", lambda e: e.matmul(p_m[:, 258:386], lhsT=lap, rhs=triL, start=True, stop=True), reads=[Blap, BtriL], writes=[b_m])
                P.op("act", lambda e: e.activation(out=eb, in_=p_m[:, 258:386], func=AF.Exp, scale=-1.0 / 16), reads=[b_m], writes=[Beb])
                P.op("act", lambda e: e.activation(out=enb, in_=p_m[:, 258:386], func=AF.Exp, scale=1.0 / 16), reads=[b_m], writes=[Beb])
                P.op("dve", lambda e: e.scalar_tensor_tensor(out=qd, in0=p_q[:, 0:128], scalar=128.0 ** -0.5, in1=eb, op0=ALU.mult, op1=ALU.mult),
                     reads=[b_q, Beb], writes=[Bqk])
                P.op("dve", lambda e: e.tensor_tensor(out=kin, in0=p_k[:, 0:128], in1=enb, op=ALU.mult), reads=[b_k, Beb], writes=[Bqk])
                P.op("act", lambda e, u=u: e.activation(out=Sa, in_=Ssel[u], func=AF.Copy), reads=[BSsel[u]], writes=[BSab])
                P.op("dve", lambda e, u=u: e.scalar_tensor_tensor(out=Sb32, in0=Ssel[u], scalar=dec[:, 0:1], in1=p_cs[:, 0:256], op0=ALU.mult, op1=ALU.add),
                     reads=[BSsel[u], Bdec, b_cs], writes=[BSab])
                P.op("act", lambda e: e.activation(out=Sb, in_=Sb32, func=AF.Copy), reads=[BSab], writes=[BSab])
                P.op("pe", lambda e: e.matmul(p_o[:, 0:128], lhsT=kin, rhs=qd, start=True, stop=True), reads=[Bqk], writes=[b_o])
                P.op("dve", lambda e: e.tensor_tensor(out=sT, in0=p_o[:, 0:128], in1=gmask, op=ALU.mult), reads=[b_o, Bgmask], writes=[BsT])
                for vc in range(2):
                    po = p_o[:, 128 + vc * 128: 256 + vc * 128]
                    P.op("pe", lambda e, vc=vc, po=po: e.matmul(po, lhsT=kv[:, 128 + vc * 128: 256 + vc * 128], rhs=sT, start=True, stop=False),
                         reads=[Bkv, BsT], writes=[b_o])
                    P.op("pe", lambda e, vc=vc, po=po: e.matmul(po[:, 0:64], lhsT=Sa[:, vc * 128:(vc + 1) * 128], rhs=qd[:, 0:64], start=False, stop=False),
                         reads=[BSab, Bqk], writes=[b_o])
                    P.op("pe", lambda e, vc=vc, po=po: e.matmul(po[:, 64:128], lhsT=Sb[:, vc * 128:(vc + 1) * 128], rhs=qd[:, 64:128], start=False, stop=True),
                         reads=[BSab, Bqk], writes=[b_o])
                    P.op("act", lambda e, vc=vc, po=po: e.activation(out=sq[vc], in_=po, func=AF.Square), reads=[b_o], writes=[Bsq])
                for vc in range(2):
                    P.op("pe", lambda e, vc=vc: e.matmul(p_k[:, 128:256], lhsT=ones32, rhs=sq[vc], start=(vc == 0), stop=(vc == 1)),
                         reads=[Bones, Bsq], writes=[b_k])
                P.op("dve", lambda e: e.tensor_scalar(out=rs, in0=p_k[:, 128:256], scalar1=1.0 / 256, scalar2=EPS, op0=ALU.mult, op1=ALU.add),
                     reads=[b_k], writes=[Brs])
                P.op("act", lambda e: e.activation(out=rs, in_=rs, func=AF.Sqrt), reads=[Brs], writes=[Brs])
                P.op("dve", lambda e: e.reciprocal(out=rs, in_=rs), reads=[Brs], writes=[Brs])
                for vc in range(2):
                    po = p_o[:, 128 + vc * 128: 256 + vc * 128]
                    P.op("dve", lambda e, po=po: e.tensor_tensor(out=tt_, in0=po, in1=rs, op=ALU.mult), reads=[b_o, Brs], writes=[Btt])
                    ch = hd * 2 + vc
                    P.op("dve", lambda e, vc=vc, ch=ch, u=u: e.scalar_tensor_tensor(out=otslot(ch, u), in0=tt_, scalar=ng[:, vc:vc + 1], in1=sgr[vc],
                                                                                  op0=ALU.mult, op1=ALU.mult),
                         reads=[Btt, Bng, Bsgr], writes=[Bot[ch][u // 4]])
    if stop == 'G':
        return Bot
    for pr in range(4):
        P.barrier()
        wq = [C.bf(A0 + i * 4 * KB, 2048) for i in range(2)]
        wk = [C.bf(A0 + 8 * KB + i * 4 * KB, 2048) for i in range(2)]
        wvf = C.bf(A0 + 16 * KB, NCH * 258)
        Bw = Buf("fw")
        for i in range(2):
            stg.load_w(dr["f_wq"][pr * 2 + i], lambda c0, n, i=i: wq[i][:, c0:c0 + n], Bw, 2048)
        if not cached:
            for i in range(2):
                stg.load_w(dr["f_wk"][pr * 2 + i], lambda c0, n, i=i: wk[i][:, c0:c0 + n], Bw, 2048)
            stg.load_w(dr["f_wvf"][pr], lambda c0, n: wvf[:, c0:c0 + n], Bw, NCH * 258)
        XO = A0 + 26 * KB
        Bxb = [Buf("fxblk0"), Buf("fxblk1")]
        KT = [C.bf(A0 + 58 * KB + i * 8 * KB, S) for i in range(2)]
        BKT = [[Buf("KT%d_%d" % (i, b)) for b in range(NBLK)] for i in range(2)]
        VV = C.bf(A0 + 74 * KB, NTILE * 256)
        BVV = [Buf("VV%d" % t) for t in range(NTILE)]
        FB = 128 * KB
        QT = [C.bf(FB + i * 2 * KB, NT) for i in range(2)]
        BQT = [Buf("QT0"), Buf("QT1")]
        ffv = C.f32(FB + 4 * KB, 64); Bffv = Buf("ffv")
        cc = C.f32(FB + 4 * KB + 256, 64); Bcc = Buf("cc")
        cbc = C.f32(FB + 4 * KB + 512, 64); Bcbc = Buf("cbc")
        tot = C.f32(FB + 4 * KB + 768, 64); Btot = Buf("tot")
        cref = C.f32(FB + 5 * KB, 16); Bcref = Buf("cref")
        bias = C.f32(FB + 6 * KB, 2 * NTILE * NSLOT); Bbias = Buf("bias")
        PT = [C.bf(FB + 8 * KB + i * KB, 512) for i in range(4)]; BPT = [Buf("PT%d" % i) for i in range(4)]
        rin = C.f32(FB + 12 * KB, 128); Brin = Buf("rin")
        gb, Bgb = K["f_gb"]
        selt, Bselt = K["selt"]
        pent, Bpent = K["pent"]
        dmask, Bdmask = K["dmask"]
        onesbf, Bonesbf = K["onesbf"]
        triL, BtriL = K["triL32"]
        sel64, Bsel64 = K["sel64"]
        p_a, b_a = C.ps[0], C.Bps[0]
        p_b, b_b = C.ps[1], C.Bps[1]
        Bfsc = Buf("fscr")
        if cached:
            for i in range(2):
                P.dma("sp", lambda e, i=i, pr=pr, KT=KT: e.dma_start(out=KT[i], in_=dr["fk_s"][pr][:, i * S:(i + 1) * S]), writes=BKT[i], sembuf=BKT[i][0])
            P.dma("sp", lambda e, pr=pr, VV=VV: e.dma_start(out=VV, in_=dr["fv_s"][pr]), writes=BVV, sembuf=BVV[0])
            P.dma("sp", lambda e, pr=pr, cc=cc: e.dma_start(out=cc, in_=dr["fc_s"][pr][:, 0:64]), writes=[Bcc])
            P.dma("sp", lambda e, pr=pr, cbc=cbc: e.dma_start(out=cbc, in_=dr["fc_s"][pr][:, 64:128]), writes=[Bcbc])
        for blk in (range(NBLK) if not cached else []):
            xk = load_xblk(C, dr["xTb"], Bx, blk * 512, 512, XO + (blk % 2) * 16 * KB, Bxb[blk % 2])
            for i in range(2):
                proj_fm(C, p_a[:, :], b_a, wk[i], Bw, lambda k: xk(k), Bxb[blk % 2])
                P.op("act", lambda e, i=i, blk=blk: e.activation(out=KT[i][:, blk * 512:(blk + 1) * 512], in_=p_a[:, :], func=AF.Copy),
                     reads=[b_a], writes=[BKT[i][blk]])
            for tt in range(4):
                t = blk * 4 + tt
                proj_tm(C, p_b[:, 0:258], b_b, wvf, Bw, lambda k, tt=tt: xk(k, tt * 128, tt * 128 + 128), Bxb[blk % 2], 258)
                P.op("dve", lambda e, t=t: e.tensor_copy(out=VV[:, t * 256:(t + 1) * 256], in_=p_b[:, 0:256]), reads=[b_b], writes=[BVV[t]])
                for i in range(2):
                    P.op("dve", lambda e, t=t, i=i: e.tensor_copy(out=ffv[:, i * 32 + t: i * 32 + t + 1], in_=p_b[:, 256 + i:257 + i]),
                         reads=[b_b], writes=[Bffv])
        for half in range(2):
            xk = load_xblk(C, dr["xTob"], Bxo, half * 512, 512, XO + half * 16 * KB, Bxb[half])
            for i in range(2):
                proj_fm(C, p_a[:, :], b_a, wq[i], Bw, lambda k: xk(k), Bxb[half])
                P.op("act", lambda e, i=i, half=half: e.activation(out=QT[i][:, half * 512:(half + 1) * 512], in_=p_a[:, :], func=AF.Copy),
                     reads=[b_a], writes=[BQT[i]])
        if stop == 'F0':
            return Bot
        for i in (range(2) if not cached else []):
            hidx = pr * 2 + i
            fs = ffv[:, i * 32:(i + 1) * 32]
            P.op("dve", lambda e, fs=fs, hidx=hidx: e.tensor_scalar(out=fs, in0=fs, scalar1=gb[:, hidx:hidx + 1], scalar2=None, op0=ALU.add),
                 reads=[Bffv, Bgb], writes=[Bffv])
            P.op("act", lambda e, fs=fs: e.activation(out=fs, in_=fs, func=AF.Exp, scale=-1.0), reads=[Bffv], writes=[Bffv])
            P.op("act", lambda e, fs=fs: e.activation(out=fs, in_=fs, func=AF.Ln, bias=1.0), reads=[Bffv], writes=[Bffv])
            P.op("pe", lambda e, fs=fs: e.matmul(p_a[:, 0:32], lhsT=triL_full(K), rhs=fs, start=True, stop=True), reads=[Bffv, K["triF32"][1]], writes=[b_a])
            P.op("pe", lambda e, fs=fs: e.matmul(p_a[:, 32:64], lhsT=ones32, rhs=fs, start=True, stop=True), reads=[Bffv, Bones], writes=[b_a])
            ci = cc[:, i * 32:(i + 1) * 32]
            ti = tot[:, i * 32:(i + 1) * 32]
            P.op("dve", lambda e, ti=ti: e.tensor_copy(out=ti, in_=p_a[:, 32:64]), reads=[b_a], writes=[Btot])
            P.op("dve", lambda e, ci=ci: e.tensor_copy(out=ci, in_=p_a[:, 0:32]), reads=[b_a], writes=[Bcc])
            for j in range(1, NTILE):
                if j >= 2:
                    P.op("dve", lambda e, ti=ti, j=j: e.tensor_tensor(out=ti[:, j - 1:j], in0=ti[:, j - 1:j], in1=ti[:, j - 2:j - 1], op=ALU.add),
                         reads=[Btot], writes=[Btot])
                P.op("dve", lambda e, ci=ci, ti=ti, j=j: e.tensor_tensor(out=ci[:, j:j + 1], in0=ci[:, j:j + 1], in1=ti[:, j - 1:j], op=ALU.add),
                     reads=[Btot, Bcc], writes=[Bcc])
            P.op("pe", lambda e, ci=ci: e.matmul(p_a[:, 64:96], lhsT=sel64, rhs=ci, start=True, stop=True), reads=[Bcc, Bsel64], writes=[b_a])
            cb = cbc[:, i * 32:(i + 1) * 32]
            P.op("dve", lambda e, cb=cb: e.tensor_copy(out=cb, in_=p_a[:, 64:96]), reads=[b_a], writes=[Bcbc])
        if store:
            for i in range(2):
                P.dma("pool", lambda e, i=i, pr=pr, KT=KT: e.dma_start(out=dr["fk_s"][pr][:, i * S:(i + 1) * S], in_=KT[i]), reads=BKT[i], writes=[Bfsc], sembuf=BKT[i][0])
            P.dma("pool", lambda e, pr=pr, VV=VV: e.dma_start(out=dr["fv_s"][pr], in_=VV), reads=BVV, writes=[Bfsc], sembuf=BVV[0])
            P.dma("pool", lambda e, pr=pr, cc=cc: e.dma_start(out=dr["fc_s"][pr][:, 0:64], in_=cc), reads=[Bcc], writes=[Bfsc], sembuf=Bcc)
            P.dma("pool", lambda e, pr=pr, cbc=cbc: e.dma_start(out=dr["fc_s"][pr][:, 64:128], in_=cbc), reads=[Bcbc], writes=[Bfsc], sembuf=Bcbc)
        for i in range(2):
            ci = cc[:, i * 32:(i + 1) * 32]
            cb = cbc[:, i * 32:(i + 1) * 32]
            tsel = tot[:, i * 32:(i + 1) * 32]
            cri = cref[:, i * 8:(i + 1) * 8]
            P.op("dve", lambda e, tsel=tsel, cb=cb: e.tensor_tensor(out=tsel, in0=cb, in1=selt, op=ALU.mult), reads=[Bcbc, Bselt], writes=[Btot])
            P.op("dve", lambda e, tsel=tsel, cri=cri: e.reduce_sum(out=cri, in_=tsel.rearrange("p (u d) -> p u d", d=4), axis=AX.X),
                 reads=[Btot], writes=[Bcref])
            bi3 = bias[:, i * NTILE * NSLOT:(i + 1) * NTILE * NSLOT].rearrange("p (j u) -> p j u", u=NSLOT)
            P.op("dve", lambda e, bi3=bi3, ci=ci, cri=cri: e.tensor_tensor(out=bi3, in0=ci.unsqueeze(2).to_broadcast([128, NTILE, NSLOT]),
                                                                       in1=cri.unsqueeze(1).to_broadcast([128, NTILE, NSLOT]), op=ALU.subtract),
                 reads=[Bcc, Bcref], writes=[Bbias])
            for u in range(NSLOT):
                P.op("dve", lambda e, bi3=bi3, u=u: e.tensor_tensor(out=bi3[:, 4 * u:4 * u + 4, u], in0=bi3[:, 4 * u:4 * u + 4, u],
                                                                 in1=pent[:, 4 * u:4 * u + 4], op=ALU.add),
                     reads=[Bpent, Bbias], writes=[Bbias])
        if stop == 'F1':
            dbg = dr['dbg']
            P.dma('sp', lambda e: e.dma_start(out=dbg[:, 0:64], in_=cc), reads=[Bcc], sembuf=C.Bout)
            P.dma('sp', lambda e: e.dma_start(out=dbg[:, 64:80], in_=cref), reads=[Bcref], sembuf=C.Bout)
            P.dma('sp', lambda e: e.dma_start(out=dbg[:, 128:640], in_=bias), reads=[Bbias], sembuf=C.Bout)
            P.dma('sp', lambda e: e.dma_start(out=dbg[:, 640:704], in_=ffv), reads=[Bffv], sembuf=C.Bout)
            P.dma('sp', lambda e: e.dma_start(out=dbg[:, 704:768], in_=cbc), reads=[Bcbc], sembuf=C.Bout)
            return Bot
        fitems = [(i, u, g) for i in range(2) for u in range(NSLOT) for g in range(u + 1)]

        def emit_Sg(n):
            i, u, g = fitems[n]
            p_s, b_s = C.ps[n % 4], C.Bps[n % 4]
            qs = QT[i][:, u * 128:(u + 1) * 128]
            for jj in range(4):
                j = g * 4 + jj
                P.op("pe", lambda e, p_s=p_s, jj=jj, j=j, i=i, qs=qs: e.matmul(p_s[:, jj * 128:(jj + 1) * 128], lhsT=KT[i][:, j * 128:(j + 1) * 128], rhs=qs,
                                                                           start=True, stop=True), reads=[BKT[i][j // 4], BQT[i]], writes=[b_s])
        for n in range(min(2, len(fitems))):
            emit_Sg(n)
        for n, (i, u, g) in enumerate(fitems):
            hidx = pr * 2 + i
            J = 4 * u + 4
            ngrp = u + 1
            oi = i * NSLOT + u
            p_o, b_o = C.ps[4 + (oi % 2)], C.Bps[4 + (oi % 2)]
            p_r, b_r = C.ps[6 + (oi % 2)], C.Bps[6 + (oi % 2)]
            p_s, b_s = C.ps[n % 4], C.Bps[n % 4]
            pt, bpt = PT[n % 4], BPT[n % 4]
            for jj in range(4):
                j = g * 4 + jj
                bcol = bias[:, (i * NTILE + j) * NSLOT + u:(i * NTILE + j) * NSLOT + u + 1]
                P.op("act", lambda e, p_s=p_s, pt=pt, jj=jj, bcol=bcol: e.activation(out=pt[:, jj * 128:(jj + 1) * 128], in_=p_s[:, jj * 128:(jj + 1) * 128],
                                                                                 func=AF.Exp, scale=128.0 ** -0.5, bias=bcol),
                     reads=[b_s, Bbias], writes=[bpt])
            if g == ngrp - 1:
                P.op("pool", lambda e, pt=pt, u=u: e.tensor_tensor(out=pt, in0=pt, in1=dmask[:, u * 512:(u + 1) * 512], op=ALU.mult),
                     reads=[bpt, Bdmask], writes=[bpt])
            if n + 2 < len(fitems):
                emit_Sg(n + 2)
            for jj in range(4):
                j = g * 4 + jj
                P.op("pe", lambda e, pt=pt, jj=jj, j=j, i=i, p_o=p_o, J=J: e.matmul(p_o[:, 0:128], lhsT=VV[:, j * 256 + i * 128: j * 256 + (i + 1) * 128],
                                                                               rhs=pt[:, jj * 128:(jj + 1) * 128], start=(j == 0), stop=(j == J - 1)),
                     reads=[bpt, BVV[j]], writes=[b_o])
            for jj in range(4):
                j = g * 4 + jj
                P.op("pe", lambda e, pt=pt, jj=jj, j=j, p_r=p_r, J=J: e.matmul(p_r[:, 0:128], lhsT=onesbf, rhs=pt[:, jj * 128:(jj + 1) * 128],
                                                                          start=(j == 0), stop=(j == J - 1)), reads=[bpt, Bonesbf], writes=[b_r])
            if g == ngrp - 1:
                P.op("dve", lambda e, p_r=p_r: e.reciprocal(out=rin, in_=p_r[:, 0:128]), reads=[b_r], writes=[Brin])
                ch = 8 + hidx
                P.op("dve", lambda e, p_o=p_o, ch=ch, u=u: e.tensor_tensor(out=otslot(ch, u), in0=p_o[:, 0:128], in1=rin, op=ALU.mult),
                     reads=[b_o, Brin], writes=[Bot[ch][u // 4]])
    P.barrier()
    return Bot


def triL_full(K):
    return K["triF32"][0]


def own_tiles(cq):
    return [4 * u + (cq if u % 2 == 0 else 3 - cq) for u in range(NSLOT)]


def host_mix_tables(cq):
    import ml_dtypes
    bf = ml_dtypes.bfloat16
    f = np.float32
    own = own_tiles(cq)
    t = np.arange(128)
    same = (t[:, None] // 64) == (t[None, :] // 64)
    T = {}
    T["triL32"] = ((t[:, None] <= t[None, :]) & same).astype(f)
    T["triU32"] = ((t[:, None] > t[None, :]) & same).astype(f)
    T["triF32"] = (t[:, None] <= t[None, :]).astype(f)
    T["cind32"] = (t[:, None] // 64 == np.arange(2)[None, :]).astype(f)
    s64 = np.zeros((128, 128), f); s64[64, :] = 1.0
    T["sel64"] = s64
    T["glamask"] = ((t[:, None] <= t[None, :]) & same).astype(bf)
    selt = np.zeros((128, 32), f); pent = np.zeros((128, 32), f)
    dmask = np.zeros((128, 32, 128), f)
    negm = np.zeros((128, 32, 128), f)
    for u in range(NSLOT):
        for d_ in range(4):
            j = 4 * u + d_
            if j == own[u]:
                selt[:, j] = 1.0
                dmask[:, j, :] = (t[:, None] <= t[None, :])
                negm[:, j, :] = np.where(t[None, :] <= t[:, None], 0.0, -BIG)
            elif j < own[u]:
                dmask[:, j, :] = 1.0
            else:
                pent[:, j] = -BIG
                negm[:, j, :] = -BIG
    T["selt"] = selt
    T["pent"] = pent
    T["dmask"] = dmask.reshape(128, 32 * 128).astype(bf)
    T["negm"] = negm.reshape(128, 32 * 128).astype(bf)
    T["onesbf"] = np.ones((128, 128), bf)
    T["identbf"] = np.eye(128).astype(bf)
    return T


def fm_layout(W):
    n = W.shape[1]
    return np.ascontiguousarray(W.reshape(NCH, 128, n).transpose(1, 0, 2).reshape(128, NCH * n), dtype=np.float32)


def host_M0_inputs(a_w_in, a_gla_gate_w2, a_gla_gate_b, a_gla_norm_g, a_fox_gate_b):
    W = a_w_in[0]
    oq, ok_, ov, ogr, oglr, ofq, ofk, ofv, off_ = 0, 512, 1024, 2048, 3072, 3088, 4112, 5136, 6160
    d = {}
    d["g_wq"] = np.stack([fm_layout(W[:, oq + h * 128: oq + (h + 1) * 128]) for h in range(4)])
    d["g_wk"] = np.stack([fm_layout(W[:, ok_ + h * 128: ok_ + (h + 1) * 128]) for h in range(4)])
    d["g_wkv"] = np.stack([fm_layout(np.concatenate([W[:, ok_ + h * 128: ok_ + (h + 1) * 128], W[:, ov + h * 256: ov + (h + 1) * 256]], 1)) for h in range(4)])
    d["g_wgr"] = np.stack([np.stack([fm_layout(W[:, ogr + h * 256 + i * 128: ogr + h * 256 + (i + 1) * 128]) for i in range(2)]) for h in range(4)])
    d["g_wglr"] = fm_layout(W[:, oglr:oglr + 16])
    d["g_w2"] = np.stack([np.concatenate([a_gla_gate_w2[0][:, h * 128:(h + 1) * 128], a_gla_gate_b[0][None, h * 128:(h + 1) * 128]], 0) for h in range(4)]).astype(np.float32)
    d["g_ng"] = np.ascontiguousarray(a_gla_norm_g[0].reshape(2, 128).T, dtype=np.float32)
    d["f_gb"] = np.ascontiguousarray(np.broadcast_to(a_fox_gate_b[0][None, :], (128, 8)), dtype=np.float32)
    d["f_wq"] = np.stack([fm_layout(W[:, ofq + h * 128: ofq + (h + 1) * 128]) for h in range(8)])
    d["f_wk"] = np.stack([fm_layout(W[:, ofk + h * 128: ofk + (h + 1) * 128]) for h in range(8)])
    d["f_wvf"] = np.stack([fm_layout(np.concatenate([W[:, ofv + 2 * p * 128: ofv + (2 * p + 2) * 128], W[:, off_ + 2 * p: off_ + 2 * p + 2]], 1)) for p in range(4)])
    return d


M0_DRAM = [("g_wq", [4, 128, 2048]), ("g_wk", [4, 128, 2048]), ("g_wkv", [4, 128, 6144]), ("g_wgr", [4, 2, 128, 2048]), ("g_wglr", [128, 256]),
           ("g_w2", [4, 17, 128]), ("g_ng", [128, 2]), ("f_gb", [128, 8]), ("f_wq", [8, 128, 2048]), ("f_wk", [8, 128, 2048]),
           ("f_wvf", [4, 128, NCH * 258]),
           ("triL32", [128, 128]), ("triU32", [128, 128]), ("triF32", [128, 128]), ("cind32", [128, 2]), ("sel64", [128, 128]),
           ("selt", [128, 32]), ("pent", [128, 32])]
M0_DRAM_BF = [("glamask", [128, 128]), ("dmask", [128, 4096]), ("onesbf", [128, 128])]


def declare_mix_dram(nc, dr, f32list, bflist):
    for nm, shp in f32list:
        dr[nm] = nc.dram_tensor(nm, shp, F32, kind="ExternalInput").ap()
    for nm, shp in bflist:
        dr[nm] = nc.dram_tensor(nm, shp, BF16, kind="ExternalInput").ap()
    dr["xTfull"] = nc.dram_tensor("xTfull", [D, S], F32, kind="ExternalInput").ap()
    dr["xTb"] = nc.dram_tensor("xTb", [D, S], BF16, kind="ExternalOutput").ap()
    dr["xTob"] = nc.dram_tensor("xTob", [D, NT], BF16, kind="ExternalOutput").ap()


NEG2 = -3.0e38


def rope_evac(C, py, bpy, pyp, bpyp, cosb, sinb, Bcs, out_ap, Bout, t1, t2, Bt, view=None):
    P = C.P
    P.op("dve", lambda e: e.tensor_tensor(out=t1, in0=py, in1=cosb, op=ALU.mult), reads=[bpy, Bcs], writes=[Bt[0]])
    P.op("dve", lambda e: e.tensor_tensor(out=t2, in0=pyp, in1=sinb, op=ALU.mult), reads=[bpyp, Bcs], writes=[Bt[1]])
    a, b = (t1, t2) if view is None else (view(t1), view(t2))
    P.op("pool", lambda e: e.tensor_tensor(out=out_ap, in0=a, in1=b, op=ALU.add), reads=[Bt[0], Bt[1]], writes=[Bout])


def phase_M1(C, dr, pre=None):
    P = C.P
    stg = Stager(C, "m1")
    Bot = [[Buf("ot%d_%d" % (k, h)) for h in range(2)] for k in range(NCH)]
    K = setup_mix_consts(C, dr, [("negm", 4096, "bf16"), ("onesbf", 128, "bf16"), ("identbf", 128, "bf16")])
    negm, Bnegm = K["negm"]
    onesbf, Bonesbf = K["onesbf"]
    identbf, Bidentbf = K["identbf"]
    SM = OFF_MISC + 12 * KB
    iw = C.f32(SM, 128); Biw = Buf("iw")
    m8 = C.f32(SM + 512, 8); Bm8 = Buf("m8")
    wiw = C.bf(SM + 1024, 256); Bwiw = Buf("wiw")
    Bx, Bxo = make_xbf_scratch(C, dr, stg) if pre is None else pre
    P.barrier()
    stop = getattr(C, 'stop', None)
    maskT = [C.bf(sum(range(1, u + 1)) * KB, (u + 1) * 512) for u in range(NSLOT)]
    BmaskT = [Buf("maskT%d" % u) for u in range(NSLOT)]
    XBLK = 128 * KB
    Bxb = Buf("xblk")
    iqT = C.bf(36 * KB, 8 * NT); BiqT = Buf("iqT")
    ikA = C.bf(52 * KB, S); ikB = C.bf(60 * KB, S); Bik = [Buf("ik%d" % b) for b in range(NBLK)]
    acc = C.f32(68 * KB, S); Bacc = Buf("acc")
    maskq = C.bf(84 * KB, S); Bmq = Buf("maskq")
    t1 = C.f32(92 * KB, 512); t2 = C.f32(94 * KB, 512); Bt = [Buf("t1"), Buf("t2")]
    wsl = [C.bf(144 * KB + i * 4 * KB, 2048) for i in range(2)]; Bwsl = [Buf("wsl0"), Buf("wsl1")]
    tab = [C.f32(152 * KB + i * 2 * KB, 512) for i in range(4)]; Btab = Buf("tab")
    p0, b0 = C.ps[0], C.Bps[0]
    p1, b1 = C.ps[1], C.Bps[1]
    for i in range(2):
        stg.load_w(dr["d_wik"][i], lambda c0, n, i=i: wsl[i][:, c0:c0 + n], Bwsl[i], 2048)
    for blk in range(NBLK):
        xk = load_xblk(C, dr["xTb"], Bx, blk * 512, 512, XBLK, Bxb)
        for i in range(4):
            P.dma("sp", lambda e, i=i, blk=blk, tb=tab[i]: e.dma_start(out=tb, in_=dr["rtik"][i][:, blk * 512:(blk + 1) * 512]), writes=[Btab])
        proj_fm(C, p0[:, :], b0, wsl[0], Bwsl[0], lambda k, xk=xk: xk(k), Bxb)
        proj_fm(C, p1[:, :], b1, wsl[1], Bwsl[1], lambda k, xk=xk: xk(k), Bxb)
        rope_evac(C, p0[:, :], b0, p1[:, :], b1, tab[0], tab[1], Btab, ikA[:, blk * 512:(blk + 1) * 512], Bik[blk], t1, t2, Bt)
        rope_evac(C, p0[:, :], b0, p1[:, :], b1, tab[2], tab[3], Btab, ikB[:, blk * 512:(blk + 1) * 512], Bik[blk], t1, t2, Bt)
    stg.load_cast(dr["d_wiw"], wiw, Bwiw, 256)
    p2, b2 = C.ps[2], C.Bps[2]
    for half in range(2):
        xk = load_xblk(C, dr["xTob"], Bxo, half * 512, 512, XBLK, Bxb)
        for i in range(2):
            P.dma("sp", lambda e, i=i, half=half, tb=tab[i]: e.dma_start(out=tb, in_=dr["rt64o"][i][:, half * 512:(half + 1) * 512]), writes=[Btab])
        for tt in range(4):
            u = half * 4 + tt
            proj_tm(C, p2[:, u * 16:(u + 1) * 16], b2, wiw, Bwiw, lambda k, xk=xk, tt=tt: xk(k, tt * 128, tt * 128 + 128), Bxb, 16)
        for pc in range(8):
            for i in range(2):
                stg.load_w(dr["d_wiq"][pc, i], lambda c0, n, i=i: wsl[i][:, c0:c0 + n], Bwsl[i], 2048)
            proj_fm(C, p0[:, :], b0, wsl[0], Bwsl[0], lambda k, xk=xk: xk(k), Bxb)
            proj_fm(C, p1[:, :], b1, wsl[1], Bwsl[1], lambda k, xk=xk: xk(k), Bxb)
            rope_evac(C, p0[:, :], b0, p1[:, :], b1, tab[0], tab[1], Btab, iqT[:, pc * NT + half * 512: pc * NT + (half + 1) * 512], BiqT, t1, t2, Bt)
    P.op("dve", lambda e: e.tensor_copy(out=iw, in_=p2[:, 0:128]), reads=[b2], writes=[Biw])
    if stop == 'I0':
        return Bot
    rb = [tab[0], tab[1]]
    Brb = [Buf("rb0"), Buf("rb1")]
    vld = C.bf(156 * KB, 512); Bvld = Buf("vld")
    pT = C.ps[4][:].bitcast(BF16)
    bpT = C.Bps[4]
    cnt = 0
    for u in range(NSLOT):
        L = 512 * (u + 1)
        for kb in range(u + 1):
            for h in range(16):
                pc = h // 2
                ik_ = ikA if h % 2 == 0 else ikB
                ps_, bps_ = C.ps[2 + cnt % 2], C.Bps[2 + cnt % 2]
                r_, br_ = rb[cnt % 2], Brb[cnt % 2]
                cnt += 1
                P.op("pe", lambda e, ps_=ps_, pc=pc, u=u, ik_=ik_, kb=kb: e.matmul(ps_[:, :], lhsT=iqT[:, pc * NT + u * 128: pc * NT + (u + 1) * 128],
                                                                            rhs=ik_[:, kb * 512:(kb + 1) * 512], start=True, stop=True),
                     reads=[BiqT, Bik[kb]], writes=[bps_])
                P.op("act", lambda e, ps_=ps_, r_=r_: e.activation(out=r_, in_=ps_[:, :], func=AF.Relu, scale=1.0 / 32), reads=[bps_], writes=[br_])
                a_ = acc[:, kb * 512:(kb + 1) * 512]
                wcol = iw[:, u * 16 + h: u * 16 + h + 1]
                if h == 0:
                    P.op("dve", lambda e, a_=a_, r_=r_, wcol=wcol: e.tensor_scalar(out=a_, in0=r_, scalar1=wcol, scalar2=None, op0=ALU.mult),
                         reads=[br_, Biw], writes=[Bacc])
                else:
                    P.op("dve", lambda e, a_=a_, r_=r_, wcol=wcol: e.scalar_tensor_tensor(out=a_, in0=r_, scalar=wcol, in1=a_, op0=ALU.mult, op1=ALU.add),
                         reads=[br_, Biw, Bacc], writes=[Bacc])
        aw = acc[:, u * 512:(u + 1) * 512]
        nm = negm[:, u * 512:(u + 1) * 512]
        P.op("dve", lambda e, aw=aw, nm=nm: e.tensor_tensor(out=aw, in0=aw, in1=nm, op=ALU.add), reads=[Bacc, Bnegm], writes=[Bacc])
        if stop == 'I1' and u == 1:
            P.dma('sp', lambda e: e.dma_start(out=dr['dbg'][:, 0:1024], in_=acc[:, 0:1024]), reads=[Bacc], sembuf=C.Bout)
            return Bot
        al = acc[:, 0:L]
        for r in range(32):
            P.op("dve", lambda e, al=al: e.max(out=m8, in_=al), reads=[Bacc], writes=[Bm8])
            P.op("dve", lambda e, al=al: e.match_replace(out=al, in_to_replace=m8, in_values=al, imm_value=NEG2), reads=[Bacc, Bm8], writes=[Bacc])
        mq = maskq[:, 0:L]
        P.op("dve", lambda e, al=al, mq=mq: e.tensor_scalar(out=mq, in0=al, scalar1=-2.0e38, scalar2=None, op0=ALU.is_le), reads=[Bacc], writes=[Bmq])
        P.op("pool", lambda e, nm=nm: e.tensor_scalar(out=vld, in0=nm, scalar1=-1.0, scalar2=None, op0=ALU.is_ge), reads=[Bnegm], writes=[Bvld])
        mw = maskq[:, u * 512:(u + 1) * 512]
        P.op("pool", lambda e, mw=mw: e.tensor_tensor(out=mw, in0=mw, in1=vld, op=ALU.mult), reads=[Bmq, Bvld], writes=[Bmq])
        for j4 in range(u + 1):
            for jj in range(4):
                j = j4 * 4 + jj
                P.op("pe", lambda e, jj=jj, j=j: e.transpose(pT[:, jj * 128:(jj + 1) * 128], maskq[:, j * 128:(j + 1) * 128], identbf),
                     reads=[Bmq, Bidentbf], writes=[bpT])
            P.op("act", lambda e, u=u, j4=j4: e.activation(out=maskT[u][:, j4 * 512:(j4 + 1) * 512], in_=pT[:, 0:512], func=AF.Copy),
                 reads=[bpT], writes=[BmaskT[u]])
    if stop == 'I2':
        for u in range(NSLOT):
            P.dma('sp', lambda e, u=u: e.dma_start(out=dr['dbgm'][:, sum(range(1, u + 1)) * 512: sum(range(1, u + 2)) * 512], in_=maskT[u]),
                  reads=[BmaskT[u]], sembuf=C.Bout)
        return Bot
    XB2 = [128 * KB, 144 * KB]
    Bxb2 = [Buf("xblkA0"), Buf("xblkA1")]
    for kvh in range(4):
        P.barrier()
        KT = C.bf(36 * KB, S); BKT = [Buf("KT%d" % b) for b in range(NBLK)]
        VV = C.bf(44 * KB, S); BVV = [Buf("VV%d" % t) for t in range(NTILE)]
        qT4 = C.bf(52 * KB, NSLOT * 512); BqT = [Buf("qT%d" % h) for h in range(2)]
        qT4v = qT4.rearrange("p (u g t) -> p u g t", u=NSLOT, g=4)
        PT = [C.bf(60 * KB + i * KB, 512) for i in range(4)]; BPT = [Buf("PT%d" % i) for i in range(4)]
        t1 = C.f32(64 * KB, 512); t2 = C.f32(66 * KB, 512); Bt = [Buf("t1"), Buf("t2")]
        rin = t1; Brin = Bt[0]
        tabs = [[C.f32(68 * KB + s_ * 4 * KB + i * 2 * KB, 512) for i in range(2)] for s_ in range(2)]
        Btabs = [Buf("tabA"), Buf("tabB")]
        wset = [[C.bf(76 * KB + s_ * 8 * KB + i * 4 * KB, 2048) for i in range(2)] for s_ in range(2)]
        Bwset = [[Buf("ws%d_%d" % (s_, i)) for i in range(2)] for s_ in range(2)]
        wv = C.bf(92 * KB, 2048); Bwv = Buf("wv")
        wk, Bwk = wset[1], Bwset[1]
        for i in range(2):
            stg.load_w(dr["d_wk"][kvh, i], lambda c0, n, i=i, wk=wk: wk[i][:, c0:c0 + n], Bwk[i], 2048)
        stg.load_w(dr["d_wv"][kvh], lambda c0, n, wv=wv: wv[:, c0:c0 + n], Bwv, 2048)
        nt = 0
        for blk in range(NBLK):
            xk = load_xblk(C, dr["xTb"], Bx, blk * 512, 512, XB2[blk % 2], Bxb2[blk % 2])
            tab, Btab = tabs[nt % 2], Btabs[nt % 2]
            nt += 1
            for i in range(2):
                P.dma("sp", lambda e, i=i, blk=blk, tb=tab[i]: e.dma_start(out=tb, in_=dr["rt128"][i][:, blk * 512:(blk + 1) * 512]), writes=[Btab])
            proj_fm(C, p0[:, :], b0, wk[0], Bwk[0], lambda k, xk=xk: xk(k), Bxb2[blk % 2])
            proj_fm(C, p1[:, :], b1, wk[1], Bwk[1], lambda k, xk=xk: xk(k), Bxb2[blk % 2])
            rope_evac(C, p0[:, :], b0, p1[:, :], b1, tab[0], tab[1], Btab, KT[:, blk * 512:(blk + 1) * 512], BKT[blk], t1, t2, Bt)
            for tt in range(4):
                t = blk * 4 + tt
                pv, bv = (p2, b2) if tt % 2 == 0 else (C.ps[3], C.Bps[3])
                proj_tm(C, pv[:, 0:128], bv, wv, Bwv, lambda k, xk=xk, tt=tt: xk(k, tt * 128, tt * 128 + 128), Bxb2[blk % 2], 128)
                P.op("act", lambda e, t=t, VV=VV, pv=pv: e.activation(out=VV[:, t * 128:(t + 1) * 128], in_=pv[:, 0:128], func=AF.Copy), reads=[bv], writes=[BVV[t]])
        ng = 0
        for half in range(2):
            xk = load_xblk(C, dr["xTob"], Bxo, half * 512, 512, XB2[half], Bxb2[half])
            tab, Btab = tabs[nt % 2], Btabs[nt % 2]
            nt += 1
            for i in range(2):
                P.dma("sp", lambda e, i=i, half=half, tb=tab[i]: e.dma_start(out=tb, in_=dr["rt128o"][i][:, half * 512:(half + 1) * 512]), writes=[Btab])
            for g in range(4):
                hq = kvh * 4 + g
                wq, Bwq = wset[ng % 2], Bwset[ng % 2]
                ng += 1
                for i in range(2):
                    stg.load_w(dr["d_wq"][hq, i], lambda c0, n, i=i, wq=wq: wq[i][:, c0:c0 + n], Bwq[i], 2048)
                proj_fm(C, p0[:, :], b0, wq[0], Bwq[0], lambda k, xk=xk: xk(k), Bxb2[half])
                proj_fm(C, p1[:, :], b1, wq[1], Bwq[1], lambda k, xk=xk: xk(k), Bxb2[half])
                v4 = lambda ap: ap.rearrange("p (u t) -> p u t", u=4)
                rope_evac(C, p0[:, :], b0, p1[:, :], b1, tab[0], tab[1], Btab, qT4v[:, half * 4:(half + 1) * 4, g, :], BqT[half], t1, t2, Bt, view=v4)
        items = [(u, j) for u in range(NSLOT) for j in range(4 * (u + 1))]

        def emit_S(idx):
            u, j = items[idx]
            p_s, b_s = C.ps[idx % 4], C.Bps[idx % 4]
            qs = qT4[:, u * 512:(u + 1) * 512]
            P.op("pe", lambda e, p_s=p_s, j=j, qs=qs, KT=KT: e.matmul(p_s[:, :], lhsT=KT[:, j * 128:(j + 1) * 128], rhs=qs, start=True, stop=True),
                 reads=[BKT[j // 4], BqT[u // 4]], writes=[b_s])
        LOOK = 2
        for idx in range(min(LOOK, len(items))):
            emit_S(idx)
        for idx, (u, j) in enumerate(items):
            J = 4 * (u + 1)
            p_o, b_o = C.ps[4 + u % 2], C.Bps[4 + u % 2]
            p_r, b_r = C.ps[6 + u % 2], C.Bps[6 + u % 2]
            p_s, b_s = C.ps[idx % 4], C.Bps[idx % 4]
            pt, bpt = PT[idx % 4], BPT[idx % 4]
            P.op("act", lambda e, p_s=p_s, pt=pt: e.activation(out=pt, in_=p_s[:, :], func=AF.Exp, scale=128.0 ** -0.5), reads=[b_s], writes=[bpt])
            mk = maskT[u][:, j * 128:(j + 1) * 128]
            eng = "pool" if idx % 3 == 2 else "dve"
            P.op(eng, lambda e, pt=pt, mk=mk: e.tensor_tensor(out=pt.rearrange("p (g t) -> p g t", g=4), in0=pt.rearrange("p (g t) -> p g t", g=4),
                                                         in1=mk.unsqueeze(1).to_broadcast([128, 4, 128]), op=ALU.mult),
                 reads=[bpt, BmaskT[u]], writes=[bpt])
            if idx + LOOK < len(items):
                emit_S(idx + LOOK)
            P.op("pe", lambda e, p_o=p_o, j=j, pt=pt, J=J, VV=VV: e.matmul(p_o[:, :], lhsT=VV[:, j * 128:(j + 1) * 128], rhs=pt, start=(j == 0), stop=(j == J - 1)),
                 reads=[BVV[j], bpt], writes=[b_o])
            P.op("pe", lambda e, p_r=p_r, j=j, pt=pt, J=J: e.matmul(p_r[:, :], lhsT=onesbf, rhs=pt, start=(j == 0), stop=(j == J - 1)),
                 reads=[Bonesbf, bpt], writes=[b_r])
            if j == J - 1:
                P.op("dve", lambda e, p_r=p_r, rin=rin: e.reciprocal(out=rin, in_=p_r[:, :]), reads=[b_r], writes=[Brin])
                otv = C.bf(OFF_OT, NCH * NT).rearrange("p (c t) -> p c t", c=NCH)[:, kvh * 4:(kvh + 1) * 4, u * 128:(u + 1) * 128]
                P.op("dve", lambda e, p_o=p_o, otv=otv, rin=rin: e.tensor_tensor(out=otv, in0=p_o[:, :].rearrange("p (g t) -> p g t", g=4),
                                                                               in1=rin.rearrange("p (g t) -> p g t", g=4), op=ALU.mult),
                     reads=[b_o, Brin], writes=[Bot[kvh * 4 + g][u // 4] for g in range(4)])
    P.barrier()
    return Bot


def rope_tables(pos, dh, npieces_heads=1):
    half = dh // 2
    inv = (10000.0 ** (-(np.arange(half, dtype=np.float32)) / np.float32(half))).astype(np.float32)
    ang = pos.astype(np.float32)[None, :] * inv[:, None]
    c = np.cos(ang).astype(np.float32)
    s = np.sin(ang).astype(np.float32)
    d = np.arange(128) % dh
    cosT = c[d % half]
    sinT = np.where((d < half)[:, None], -s[d % half], s[d % half])
    return cosT.astype(np.float32), sinT.astype(np.float32)


def perm_cols(n, dh):
    idx = np.arange(n)
    d = idx % dh
    half = dh // 2
    return np.where(d < half, idx + half, idx - half)


def host_M1_inputs(c_w_in, cq):
    W = c_w_in[0]
    oq, ok_, ov, oiq, oik, oiw = 0, 2048, 2560, 3072, 4096, 4160
    d = {}
    p128 = perm_cols(128, 128)
    p64 = perm_cols(128, 64)
    Wq = W[:, oq:oq + 2048]
    d["d_wq"] = np.stack([np.stack([fm_layout(Wq[:, h * 128:(h + 1) * 128]), fm_layout(Wq[:, h * 128:(h + 1) * 128][:, p128])]) for h in range(16)])
    Wk = W[:, ok_:ok_ + 512]
    d["d_wk"] = np.stack([np.stack([fm_layout(Wk[:, h * 128:(h + 1) * 128]), fm_layout(Wk[:, h * 128:(h + 1) * 128][:, p128])]) for h in range(4)])
    d["d_wv"] = np.stack([fm_layout(W[:, ov + h * 128: ov + (h + 1) * 128]) for h in range(4)])
    Wiq = W[:, oiq:oiq + 1024]
    d["d_wiq"] = np.stack([np.stack([fm_layout(Wiq[:, pc * 128:(pc + 1) * 128]), fm_layout(Wiq[:, pc * 128:(pc + 1) * 128][:, p64])]) for pc in range(8)])
    Wik2 = np.concatenate([W[:, oik:oik + 64], W[:, oik:oik + 64]], 1)
    d["d_wik"] = np.stack([fm_layout(Wik2), fm_layout(Wik2[:, p64])])
    d["d_wiw"] = fm_layout(W[:, oiw:oiw + 16])
    pos = np.arange(S)
    own = own_tiles(cq)
    opos = np.concatenate([np.arange(i * 128, (i + 1) * 128) for i in own])
    c128, s128 = rope_tables(pos, 128)
    c64, s64 = rope_tables(pos, 64)
    d["rt128"] = np.stack([c128, s128])
    d["rt128o"] = np.stack([c128[:, opos], s128[:, opos]])
    d["rt64o"] = np.stack([c64[:, opos], s64[:, opos]])
    mA = (np.arange(128) < 64)[:, None].astype(np.float32)
    d["rtik"] = np.stack([c64 * mA, s64 * mA, c64 * (1 - mA), s64 * (1 - mA)])
    return {k: np.ascontiguousarray(v, dtype=np.float32) for k, v in d.items()}


M1_DRAM = [("d_wq", [16, 2, 128, 2048]), ("d_wk", [4, 2, 128, 2048]), ("d_wv", [4, 128, 2048]), ("d_wiq", [8, 2, 128, 2048]),
           ("d_wik", [2, 128, 2048]), ("d_wiw", [128, 256]), ("rt128", [2, 128, S]), ("rt128o", [2, 128, NT]), ("rt64o", [2, 128, NT]),
           ("rtik", [4, 128, S])]
M1_DRAM_BF = [("negm", [128, 4096]), ("onesbf", [128, 128]), ("identbf", [128, 128])]


_PROG_CACHE = {}
R_NAMES = ("woutp", "moew", "wr", "rb", "lnp")


def prep_layer1(C, dr):
    P = C.P
    P.new_epoch()
    stg = Stager(C, "p1")
    oh = C.f32(OFF_MISC, 4)
    Boh = Buf("oh")
    P.dma("sp", lambda e: e.dma_start(out=oh, in_=dr["onehot"]), writes=[Boh])
    tmp = [C.bf(OFF_XB + i * 2 * KB, NT) for i in range(4)]
    Bt = [Buf("p1t%d" % i) for i in range(4)]
    Bx1, Bxo1 = Buf("xTb1"), Buf("xTob1")
    Bz = [Buf("p1z%d" % k) for k in range(NCH)]
    z = lambda k: C.f32(OFF_ACC + k * NT * 4, NT)
    n = 0
    for vq in range(4):
        for k in range(NCH):
            si = stg.n % NSTAGE
            stg.n += 1
            st, bst = stg.stage[si][:, 0:NT], stg.B[si]
            P.dma("sp", lambda e, st=st, vq=vq, k=k: e.dma_start(out=st, in_=dr["x1sh"][vq][k * 128:(k + 1) * 128, :]), writes=[bst])
            t, bt = tmp[n % 4], Bt[n % 4]
            n += 1
            P.op("act", lambda e, t=t, st=st: e.activation(out=t, in_=st, func=AF.Copy), reads=[bst], writes=[bt])
            dstv = dr["xTb1"][k * 128:(k + 1) * 128, :].rearrange("p (u2 par d t) -> p u2 par d t", u2=4, par=2, d=4)
            tv = t.rearrange("p (u2 par t) -> p u2 par t", u2=4, par=2)
            for par in range(2):
                d_ = vq if par == 0 else 3 - vq
                P.dma("pool", lambda e, dstv=dstv, tv=tv, par=par, d_=d_: e.dma_start(out=dstv[:, :, par, d_, :], in_=tv[:, :, par, :]),
                      reads=[bt], writes=[Bx1], sembuf=bt)
            if vq == 0:
                P.op("dve", lambda e, k=k, st=st: e.tensor_scalar(out=z(k), in0=st, scalar1=oh[:, 0:1], scalar2=None, op0=ALU.mult),
                     reads=[bst, Boh], writes=[Bz[k]])
            else:
                P.op("dve", lambda e, k=k, st=st, vq=vq: e.scalar_tensor_tensor(out=z(k), in0=st, scalar=oh[:, vq:vq + 1], in1=z(k), op0=ALU.mult, op1=ALU.add),
                     reads=[bst, Boh, Bz[k]], writes=[Bz[k]])
    for k in range(NCH):
        P.dma("sp", lambda e, k=k: e.dma_start(out=dr["x1own"][k * 128:(k + 1) * 128, :], in_=z(k)), reads=[Bz[k]], sembuf=C.Bout)
        t, bt = tmp[n % 4], Bt[n % 4]
        n += 1
        P.op("act", lambda e, t=t, k=k: e.activation(out=t, in_=z(k), func=AF.Copy), reads=[Bz[k]], writes=[bt])
        P.dma("pool", lambda e, t=t, k=k: e.dma_start(out=dr["xTob"][k * 128:(k + 1) * 128, :], in_=t), reads=[bt], writes=[Bxo1], sembuf=bt)
    P.barrier()
    return Bx1, Bxo1


M0_TABLES = ("selt", "pent", "dmask")


def build_fused_program():
    if "fused" in _PROG_CACHE:
        return _PROG_CACHE["fused"]
    nc = bass.Bass("TRN2", target_bir_lowering=False)
    dr = {}
    f32in = lambda nm, shp: dr.__setitem__(nm, nc.dram_tensor(nm, shp, F32, kind="ExternalInput").ap())
    bfin = lambda nm, shp: dr.__setitem__(nm, nc.dram_tensor(nm, shp, BF16, kind="ExternalInput").ap())
    f32in("xTfull", [D, S])
    f32in("xTsh", [4, D, NT])
    f32in("ident", [128, 128])
    f32in("onehot", [128, 4])
    for L in range(2):
        f32in("woutp%d" % L, [NCH, 128, 2048])
        f32in("moew%d" % L, [NEXP, 12, 128, 2048])
        f32in("wr%d" % L, [128, NCH * 36])
        f32in("rb%d" % L, [1, 36])
        f32in("lnp%d" % L, [128, 4 * NCH])
    for nm, shp in M0_DRAM:
        if nm in M0_TABLES:
            f32in(nm + "_v", [4] + shp)
        else:
            f32in(nm, shp)
    for nm, shp in M0_DRAM_BF:
        if nm in M0_TABLES:
            bfin(nm + "_v", [4] + shp)
        else:
            bfin(nm, shp)
    for nm, shp in M1_DRAM:
        f32in(nm, shp)
    for nm, shp in M1_DRAM_BF:
        if nm not in dr:
            bfin(nm, shp)
    scr = lambda nm, shp, dt_: dr.__setitem__(nm, nc.dram_tensor(nm, shp, dt_, kind="ExternalOutput").ap())
    scr("xTb", [D, S], BF16)
    scr("xTob", [D, NT], BF16)
    scr("x1sh", [4, D, NT], F32)
    scr("xTb1", [D, S], BF16)
    scr("x1own", [D, NT], F32)
    scr("snap", [4 * NTILE, 128, 256], F32)
    scr("fk_s", [4, 128, 2 * S], BF16)
    scr("fv_s", [4, 128, NTILE * 256], BF16)
    scr("fc_s", [4, 128, 128], F32)
    scr("outT", [D, NT], F32)
    dr["cwd"] = nc.dram_tensor("cwd", [NEXP, NT], F32, kind="Internal").ap()
    with contextlib.ExitStack() as st:
        C = Ctx(nc, st)
        setup_consts(C, dr)
        P = C.P
        Bx = None
        for vq in range(4):
            P.new_epoch()
            drv = dict(dr)
            drv["xT"] = dr["xTsh"][vq]
            for nm in M0_TABLES:
                drv[nm] = dr[nm + "_v"][vq]
            for nm in R_NAMES:
                drv[nm] = dr[nm + "0"]
            drv["outT"] = dr["x1sh"][vq]
            Bot = phase_M0(C, drv, Bx_prev=Bx, vq=vq)
            Bx = C.last_Bx
            P.new_epoch()
            load_lnp(C, drv)
            Bz = load_z(C, drv)
            phase_R(C, drv, Bz, Bot)
        Bx1, Bxo1 = prep_layer1(C, dr)
        P.new_epoch()
        drv = dict(dr)
        drv["xTb"] = dr["xTb1"]
        drv["xT"] = dr["x1own"]
        for nm in R_NAMES:
            drv[nm] = dr[nm + "1"]
        Bot = phase_M1(C, drv, pre=(Bx1, Bxo1))
        P.new_epoch()
        load_lnp(C, drv)
        Bz = load_z(C, drv)
        phase_R(C, drv, Bz, Bot)
        P.emit()
    _PROG_CACHE["fused"] = nc
    return nc


def core_tokens(cq):
    return np.concatenate([np.arange(i * 128, (i + 1) * 128) for i in own_tiles(cq)])


def kernel(x, a_w_in, a_gla_gate_w2, a_gla_gate_b, a_gla_norm_g, a_fox_gate_b, a_w_out,
           c_w_in, c_w_out, ln_mix_g, ln_mix_b, ln_ffn_g, ln_ffn_b,
           moe_group_w, moe_group_b, moe_expert_w, moe_expert_b, moe_w_gate, moe_w_up, moe_w_down):
    A = lambda v: np.asarray(v)
    x = np.asarray(x, dtype=np.float32)
    nc = build_fused_program()
    shared = {}
    for L in range(2):
        r = host_R_inputs(L, A(a_w_out[0] if L == 0 else c_w_out[0]), A(ln_mix_g), A(ln_mix_b), A(ln_ffn_g), A(ln_ffn_b), A(moe_group_w),
                          A(moe_group_b), A(moe_expert_w), A(moe_expert_b), A(moe_w_gate), A(moe_w_up), A(moe_w_down))
        for nm in R_NAMES:
            shared[nm + str(L)] = r[nm]
        shared["ident"] = r["ident"]
    shared.update(host_M0_inputs(A(a_w_in), A(a_gla_gate_w2), A(a_gla_gate_b), A(a_gla_norm_g), A(a_fox_gate_b)))
    Tv = [host_mix_tables(vq) for vq in range(4)]
    for nm, _ in M0_DRAM[11:] + M0_DRAM_BF:
        if nm in M0_TABLES:
            shared[nm + "_v"] = np.stack([Tv[vq][nm] for vq in range(4)])
        else:
            shared[nm] = Tv[0][nm]
    xTb = [np.ascontiguousarray(x[b].T) for b in range(2)]
    xTsh = [np.stack([np.ascontiguousarray(xTb[b][:, core_tokens(vq)]) for vq in range(4)]) for b in range(2)]
    in_maps = []
    for core in range(8):
        b, cq = core // 4, core % 4
        m = dict(shared)
        m.update(host_M1_inputs(A(c_w_in), cq))
        for nm, _ in M1_DRAM_BF:
            if nm not in m:
                m[nm] = Tv[cq][nm]
        oh = np.zeros((128, 4), np.float32)
        oh[:, cq] = 1.0
        m["onehot"] = oh
        m["xTfull"] = xTb[b]
        m["xTsh"] = xTsh[b]
        in_maps.append(m)
    res = run_bass_kernel_spmd(nc, in_maps, core_ids=list(range(8)))
    out = np.empty_like(x)
    for core in range(8):
        b, cq = core // 4, core % 4
        out[b][core_tokens(cq)] = np.asarray(res.results[core]["outT"]).T
    return out
```
